# Optimizing a Trainium2 kernel written in Bass

```python
import math
import jax, jax.numpy as jnp
from jax import lax
import numpy as np

D_MODEL = 1024
BATCH = 2
SEQ = 8192
DEPTH = 1

RET_HEADS = 4
RET_QK_DIM = 64
RET_V_DIM = 128
RET_CHUNK = 128
RET_WIDTH = RET_HEADS * RET_V_DIM
ROPE_BASE = 10000.0
SWA_HEADS = 8
SWA_KV_HEADS = 2
SWA_HEAD_DIM = 64
SWA_WINDOW = 128
SWA_WIDTH = SWA_HEADS * SWA_HEAD_DIM
MIX_WIDTH = RET_WIDTH + SWA_WIDTH
IN_SIZES = (RET_HEADS * RET_QK_DIM, RET_HEADS * RET_QK_DIM, RET_WIDTH, RET_WIDTH,
            SWA_WIDTH, SWA_KV_HEADS * SWA_HEAD_DIM, SWA_KV_HEADS * SWA_HEAD_DIM)
IN_WIDTH = sum(IN_SIZES)
REL_BUCKETS = 32
REL_MAX_DIST = 128
N_EXPERTS = 256
TOP_K = 8
N_GROUPS = 8
TOPK_GROUPS = 4
EXPERT_DIM = 256
SHARED_DIM = 256
ROUTED_SCALE = 2.5
MOE_BLOCK = 128
LN_EPS = 1e-5
GN_EPS = 1e-6
DEEPNORM_ALPHA = (2 * DEPTH) ** 0.25
DEEPNORM_BETA = (8 * DEPTH) ** -0.25

kernel_name = "hybrid_retention_swa_moe_deepnorm"


def layer_norm(x, g, b):
    x32 = x.astype(jnp.float32)
    mu = jnp.mean(x32, axis=-1, keepdims=True)
    var = jnp.mean(jnp.square(x32 - mu), axis=-1, keepdims=True)
    y = (x32 - mu) * lax.rsqrt(var + LN_EPS) * g.astype(jnp.float32) + b.astype(jnp.float32)
    return y.astype(x.dtype)


def rotary(x, pos):
    half = x.shape[-1] // 2
    inv = ROPE_BASE ** (-jnp.arange(half, dtype=jnp.float32) / half)
    ang = pos.astype(jnp.float32)[:, None] * inv[None, :]
    cos = jnp.cos(ang)[None, :, None, :]
    sin = jnp.sin(ang)[None, :, None, :]
    x32 = x.astype(jnp.float32)
    x1, x2 = x32[..., :half], x32[..., half:]
    return jnp.concatenate([x1 * cos - x2 * sin, x1 * sin + x2 * cos], axis=-1).astype(x.dtype)


def retention_chunkwise(q, k, v):
    B, S, H, dk = q.shape
    dv = v.shape[-1]
    C = RET_CHUNK
    NC = S // C
    log_gamma = jnp.log(1.0 - 2.0 ** (-5.0 - jnp.arange(H, dtype=jnp.float32)))
    qc = q.astype(jnp.float32).reshape(B, NC, C, H, dk)
    kc = k.astype(jnp.float32).reshape(B, NC, C, H, dk)
    vc = v.astype(jnp.float32).reshape(B, NC, C, H, dv)
    idx = jnp.arange(C, dtype=jnp.float32)
    diff = idx[:, None] - idx[None, :]
    intra_decay = jnp.where(diff[None] >= 0,
                            jnp.exp(jnp.maximum(diff, 0.0)[None] * log_gamma[:, None, None]), 0.0)
    zeta = jnp.exp((C - 1.0 - idx)[None, :] * log_gamma[:, None])
    xi = jnp.exp((idx + 1.0)[None, :] * log_gamma[:, None])
    chunk_decay = jnp.exp(C * log_gamma)
    scores = jnp.einsum('bnihd,bnjhd->bnhij', qc, kc) * intra_decay
    intra = jnp.einsum('bnhij,bnjhe->bnihe', scores, vc)
    kv = jnp.einsum('bnjhd,bnjhe->bnhde', kc * zeta.T[None, None, :, :, None], vc)

    def step(state, kv_n):
        return state * chunk_decay[None, :, None, None] + kv_n, state

    _, states = lax.scan(step, jnp.zeros((B, H, dk, dv), jnp.float32), jnp.moveaxis(kv, 1, 0))
    states = jnp.moveaxis(states, 0, 1)
    inter = jnp.einsum('bnihd,bnhde->bnihe', qc, states) * xi.T[None, None, :, :, None]
    return (intra + inter).reshape(B, S, H, dv)


def t5_causal_bucket(dist):
    n = jnp.maximum(dist, 0)
    max_exact = REL_BUCKETS // 2
    ratio = jnp.log(jnp.maximum(n, 1).astype(jnp.float32) / max_exact) / math.log(REL_MAX_DIST / max_exact)
    large = max_exact + (ratio * (REL_BUCKETS - max_exact)).astype(jnp.int32)
    large = jnp.minimum(large, REL_BUCKETS - 1)
    return jnp.where(n < max_exact, n, large)


def sliding_window_gqa(q, k, v, rel_bias, sinks):
    B, S, H, d = q.shape
    Hk = k.shape[2]
    G = H // Hk
    W = SWA_WINDOW
    NB = S // W
    qb = q.reshape(B, NB, W, Hk, G, d)
    kb = k.reshape(B, NB, W, Hk, d)
    vb = v.reshape(B, NB, W, Hk, d)
    pad = ((0, 0), (1, 0), (0, 0), (0, 0), (0, 0))
    kcat = jnp.concatenate([jnp.pad(kb, pad)[:, :-1], kb], axis=2)
    vcat = jnp.concatenate([jnp.pad(vb, pad)[:, :-1], vb], axis=2)
    logits = jnp.einsum('bnikgd,bnjkd->bnkgij', qb, kcat).astype(jnp.float32) * (d ** -0.5)
    i = jnp.arange(W)
    j = jnp.arange(2 * W)
    dist = i[:, None] + W - j[None, :]
    band = (dist >= 0) & (dist < W)
    first_ok = (jnp.arange(NB)[:, None] > 0) | (j[None, :] >= W)
    mask = band[None, :, :] & first_ok[:, None, :]
    bias = rel_bias.astype(jnp.float32)[t5_causal_bucket(dist)]
    bias = jnp.transpose(bias, (2, 0, 1)).reshape(Hk, G, W, 2 * W)
    logits = jnp.where(mask[None, :, None, None], logits + bias[None, None], -jnp.inf)
    sink = jnp.broadcast_to(sinks.astype(jnp.float32).reshape(Hk, G)[None, None, :, :, None, None],
                            logits.shape[:-1] + (1,))
    probs = jax.nn.softmax(jnp.concatenate([logits, sink], axis=-1), axis=-1)[..., :-1]
    out = jnp.einsum('bnkgij,bnjkd->bnikgd', probs.astype(v.dtype), vcat)
    return out.reshape(B, S, H * d)


def hybrid_mixer(h, w_in, w_out, rel_bias, sinks):
    B, S, _ = h.shape
    pos = jnp.arange(S)
    split_pts = np.cumsum(IN_SIZES)[:-1].tolist()
    q_r, k_r, v_r, g_r, q_s, k_s, v_s = jnp.split(h @ w_in, split_pts, axis=-1)
    q_r = rotary(q_r.reshape(B, S, RET_HEADS, RET_QK_DIM), pos)
    k_r = rotary(k_r.reshape(B, S, RET_HEADS, RET_QK_DIM), pos) * (RET_QK_DIM ** -0.5)
    ret = retention_chunkwise(q_r, k_r, v_r.reshape(B, S, RET_HEADS, RET_V_DIM))
    mu = jnp.mean(ret, axis=-1, keepdims=True)
    var = jnp.mean(jnp.square(ret - mu), axis=-1, keepdims=True)
    ret = ((ret - mu) * lax.rsqrt(var + GN_EPS)).reshape(B, S, RET_WIDTH).astype(h.dtype)
    ret = jax.nn.silu(g_r) * ret
    swa = sliding_window_gqa(q_s.reshape(B, S, SWA_HEADS, SWA_HEAD_DIM),
                             k_s.reshape(B, S, SWA_KV_HEADS, SWA_HEAD_DIM),
                             v_s.reshape(B, S, SWA_KV_HEADS, SWA_HEAD_DIM), rel_bias, sinks)
    return jnp.concatenate([ret, swa], axis=-1) @ w_out


def moe_ffn(h, w_router, router_bias, w_gate, w_up, w_down, ws_gate, ws_up, ws_down):
    B, S, D = h.shape
    T = B * S
    E = N_EXPERTS
    xf = h.reshape(T, D)
    scores = jax.nn.sigmoid((xf @ w_router).astype(jnp.float32))
    choice = scores + router_bias.astype(jnp.float32)
    grp_score = lax.top_k(choice.reshape(T, N_GROUPS, E // N_GROUPS), 2)[0].sum(-1)
    _, top_grp = lax.top_k(grp_score, TOPK_GROUPS)
    grp_mask = jnp.any(top_grp[:, :, None] == jnp.arange(N_GROUPS)[None, None, :], axis=1)
    masked = jnp.where(jnp.repeat(grp_mask, E // N_GROUPS, axis=1), choice, -jnp.inf)
    _, top_idx = lax.top_k(masked, TOP_K)
    top_w = jnp.take_along_axis(scores, top_idx, axis=1)
    top_w = top_w / jnp.sum(top_w, axis=-1, keepdims=True) * ROUTED_SCALE
    flat_e = top_idx.reshape(-1)
    flat_tok = jnp.repeat(jnp.arange(T, dtype=jnp.int32), TOP_K)
    flat_w = top_w.reshape(-1)
    counts = jnp.bincount(flat_e, length=E)
    padded = (counts + MOE_BLOCK - 1) // MOE_BLOCK * MOE_BLOCK
    pad_end = jnp.cumsum(padded)
    pad_start = pad_end - padded
    start = jnp.cumsum(counts) - counts
    order = jnp.argsort(flat_e)
    se = flat_e[order]
    dest = pad_start[se] + jnp.arange(T * TOP_K) - start[se]
    n_rows = T * TOP_K + E * MOE_BLOCK
    n_blocks = n_rows // MOE_BLOCK
    row_tok = jnp.full((n_rows,), T, jnp.int32).at[dest].set(flat_tok[order])
    row_w = jnp.zeros((n_rows,), jnp.float32).at[dest].set(flat_w[order])
    blk_e = jnp.minimum(jnp.searchsorted(pad_end, jnp.arange(n_blocks) * MOE_BLOCK, side='right'), E - 1)

    def expert_block(args):
        e, tok, wt = args
        xb = xf[jnp.minimum(tok, T - 1)]
        y = (jax.nn.silu(xb @ w_gate[e]) * (xb @ w_up[e])) @ w_down[e]
        return (y * wt[:, None]).astype(xf.dtype)

    y_rows = lax.map(expert_block, (blk_e, row_tok.reshape(n_blocks, MOE_BLOCK),
                                    row_w.reshape(n_blocks, MOE_BLOCK)))
    routed = jax.ops.segment_sum(y_rows.reshape(n_rows, D), row_tok, num_segments=T)
    shared = (jax.nn.silu(xf @ ws_gate) * (xf @ ws_up)) @ ws_down
    return (routed + shared).reshape(B, S, D)


def setup_inputs(seed: int = 0) -> dict:
    key = jax.random.key(seed)
    ks = jax.random.split(key, 24)
    f32 = jnp.float32

    def nrm(k, shape, scale):
        return jax.random.normal(k, shape, f32) * scale

    beta = DEEPNORM_BETA
    col_scale = jnp.concatenate([jnp.full((n,), beta if c in (2, 6) else 1.0, f32)
                                 for c, n in enumerate(IN_SIZES)])
    return {
        "x": nrm(ks[0], (BATCH, SEQ, D_MODEL), 1.0),
        "ln_in_g": 1.0 + nrm(ks[1], (D_MODEL,), 0.02),
        "ln_in_b": nrm(ks[2], (D_MODEL,), 0.02),
        "w_in": nrm(ks[3], (DEPTH, D_MODEL, IN_WIDTH), D_MODEL ** -0.5) * col_scale,
        "w_out": nrm(ks[4], (DEPTH, MIX_WIDTH, D_MODEL), MIX_WIDTH ** -0.5 * beta),
        "rel_bias": nrm(ks[5], (REL_BUCKETS, SWA_HEADS), 0.5),
        "attn_sinks": nrm(ks[6], (DEPTH, SWA_HEADS), 1.0),
        "ln_mix_g": 1.0 + nrm(ks[7], (DEPTH, D_MODEL), 0.02),
        "ln_mix_b": nrm(ks[8], (DEPTH, D_MODEL), 0.02),
        "w_router": nrm(ks[9], (DEPTH, D_MODEL, N_EXPERTS), D_MODEL ** -0.5),
        "router_bias": nrm(ks[10], (DEPTH, N_EXPERTS), 0.01),
        "w_gate": nrm(ks[11], (DEPTH, N_EXPERTS, D_MODEL, EXPERT_DIM), D_MODEL ** -0.5 * beta),
        "w_up": nrm(ks[12], (DEPTH, N_EXPERTS, D_MODEL, EXPERT_DIM), D_MODEL ** -0.5 * beta),
        "w_down": nrm(ks[13], (DEPTH, N_EXPERTS, EXPERT_DIM, D_MODEL), EXPERT_DIM ** -0.5 * beta),
        "ws_gate": nrm(ks[14], (DEPTH, D_MODEL, SHARED_DIM), D_MODEL ** -0.5 * beta),
        "ws_up": nrm(ks[15], (DEPTH, D_MODEL, SHARED_DIM), D_MODEL ** -0.5 * beta),
        "ws_down": nrm(ks[16], (DEPTH, SHARED_DIM, D_MODEL), SHARED_DIM ** -0.5 * beta),
        "ln_ffn_g": 1.0 + nrm(ks[17], (DEPTH, D_MODEL), 0.02),
        "ln_ffn_b": nrm(ks[18], (DEPTH, D_MODEL), 0.02),
    }


def reference(x, ln_in_g, ln_in_b, w_in, w_out, rel_bias, attn_sinks, ln_mix_g, ln_mix_b,
              w_router, router_bias, w_gate, w_up, w_down, ws_gate, ws_up, ws_down,
              ln_ffn_g, ln_ffn_b):
    h = layer_norm(x, ln_in_g, ln_in_b)
    for l in range(DEPTH):
        mix = hybrid_mixer(h, w_in[l], w_out[l], rel_bias, attn_sinks[l])
        h = layer_norm(DEEPNORM_ALPHA * h + mix, ln_mix_g[l], ln_mix_b[l])
        ffn = moe_ffn(h, w_router[l], router_bias[l], w_gate[l], w_up[l], w_down[l],
                      ws_gate[l], ws_up[l], ws_down[l])
        h = layer_norm(DEEPNORM_ALPHA * h + ffn, ln_ffn_g[l], ln_ffn_b[l])
    return h
```

```python
import math
import types
from contextlib import ExitStack

import numpy as np
import concourse.bass as bass
import concourse.mybir as mybir
from concourse.bass_utils import run_bass_kernel_spmd

F32 = mybir.dt.float32
F32R = mybir.dt.float32r
I32 = mybir.dt.int32
U32 = mybir.dt.uint32
AF = mybir.ActivationFunctionType
ALU = mybir.AluOpType

NCORES = 8
NT = 16
NPRE = 48
E = 256
E_RUN = 256
PHASE3 = True
VARIANT = 0
SBUF_ALIGN = 32
STAGE = 7
CAP = 128
ALPHA = 2.0 ** 0.25
NEG = -200.0
CHUNK_DECAY = [float(np.float32((1.0 - 2.0 ** (-5.0 - h)) ** 128)) for h in range(4)]
DEBUG = False


class Res:
    __slots__ = ("name", "w", "rs", "dsem", "dcount")

    def __init__(self, name):
        self.name = name
        self.w = None
        self.rs = []
        self.dsem = None
        self.dcount = 0


def _freeze(fn):
    if fn.__closure__ is None:
        return fn
    cells = []
    for c in fn.__closure__:
        try:
            cells.append(types.CellType(c.cell_contents))
        except ValueError:
            cells.append(c)
    return types.FunctionType(fn.__code__, fn.__globals__, fn.__name__, fn.__defaults__, tuple(cells))


class Prog:
    ENG = ("pe", "dve", "act", "pool", "sp")

    def __init__(self, nc, stack):
        self.nc = nc
        self.stack = stack
        self.stream = {e: [] for e in self.ENG}
        self.seq = {e: 0 for e in self.ENG}
        self.known = {e: {} for e in self.ENG}
        self.esem = {e: stack.enter_context(nc.semaphore("es_" + e)) for e in self.ENG}
        self.used = set()
        self.nd = 0
        self.allres = []
        self._regs = {}

    def reg(self, eng, val):
        key = (id(eng), val)
        if key not in self._regs:
            self._regs[key] = eng.to_reg(val)
        return self._regs[key]

    def res(self, name):
        r = Res(name)
        self.allres.append(r)
        return r

    def _need(self, eng, toks):
        kn = self.known[eng]
        for t in toks:
            if t is None:
                continue
            if t[0] == "e":
                if t[1] == "pe" and eng == "pe":
                    continue
                key = ("e", t[1])
                if kn.get(key, 0) >= t[2]:
                    continue
                kn[key] = t[2]
                self.used.add((t[1], t[2]))
                self.stream[eng].append(("we", t[1], t[2]))
            else:
                key = ("d", id(t[1]))
                if kn.get(key, 0) >= t[2]:
                    continue
                kn[key] = t[2]
                self.stream[eng].append(("wd", t[1], t[2]))

    @staticmethod
    def _deps(R, W):
        toks = []
        for r in R:
            toks.append(r.w)
        for r in W:
            toks.append(r.w)
            toks.extend(r.rs)
        return toks

    def op(self, eng, fn, R=(), W=()):
        self._need(eng, self._deps(R, W))
        self.seq[eng] += 1
        s = self.seq[eng]
        tok = ("e", eng, s)
        self.stream[eng].append(("op", _freeze(fn), s))
        for r in R:
            r.rs.append(tok)
        for r in W:
            r.w = tok
            r.rs = []
        return tok

    def dma(self, q, fn, R=(), W=(), Wn=()):
        self._need(q, self._deps(R, W))
        sr = W[0] if W else Wn[0]
        if sr.dsem is None:
            sr.dsem = self.stack.enter_context(self.nc.semaphore("ds%d" % self.nd))
            self.nd += 1
        sr.dcount += 16
        tok = ("d", sr.dsem, sr.dcount)
        self.stream[q].append(("dma", _freeze(fn), sr.dsem))
        for r in R:
            r.rs.append(tok)
        for r in W:
            r.w = tok
            r.rs = []
        for r in Wn:
            r.w = tok
        return tok

    def wait_all(self, eng, ress):
        best = {}
        for r in ress:
            for t in [r.w] + list(r.rs):
                if t is None:
                    continue
                if t[0] == "e":
                    if t[1] == "pe" and eng == "pe":
                        continue
                    key = ("e", t[1])
                else:
                    key = ("d", id(t[1]))
                if key not in best or best[key][2] < t[2]:
                    best[key] = t
        toks = [t for t in best.values()]
        for i in range(0, len(toks), 3):
            self._need(eng, toks[i:i + 3])
            if i + 3 < len(toks):
                self.seq[eng] += 1
                self.stream[eng].append(("op", (lambda e: e.nop()), self.seq[eng]))

    def barrier(self):
        self.wait_all("sp", self.allres)
        tok = self.op("sp", lambda e: e.nop())
        for e in self.ENG:
            if e != "sp":
                self._need(e, [tok])

    def emit(self):
        nc = self.nc
        val = {}
        for e in self.ENG:
            c = 0
            m = {}
            for it in self.stream[e]:
                if it[0] == "op" and (e, it[2]) in self.used:
                    c += 1
                    m[it[2]] = c
            val[e] = m
        engobj = {"pe": "tensor", "dve": "vector", "act": "scalar", "pool": "gpsimd", "sp": "sync"}
        esem = self.esem
        used = self.used
        with nc.Block() as block:
            for e in self.ENG:
                items = self.stream[e]

                def body(eng, items=items, e=e):
                    for it in items:
                        k = it[0]
                        if k == "we":
                            eng.wait_ge(esem[it[1]], val[it[1]][it[2]])
                        elif k == "wd":
                            eng.wait_ge(it[1], it[2])
                        elif k == "op":
                            ins = it[1](eng)
                            if (e, it[2]) in used:
                                ins.then_inc(esem[e], 1)
                        else:
                            it[1](eng).then_inc(it[2], 16)
                getattr(block, engobj[e])(body)


class TPool:
    def __init__(self, P, alloc, name, shape, dt, n):
        self.tiles = [alloc("%s_%d" % (name, i), shape, dt) for i in range(n)]
        self.ress = [P.res("%s_%d" % (name, i)) for i in range(n)]
        self.i = 0

    def next(self):
        k = self.i % len(self.tiles)
        self.i += 1
        return self.tiles[k], self.ress[k]


def build_program():
    nc = bass.Bass("TRN2", target_bir_lowering=False)

    def din(name, shape, dt=F32):
        return nc.dram_tensor(name, list(shape), dt, kind="ExternalInput").ap()

    x_own = din("x_own", [NT * 128, 1024])
    x_pre = din("x_pre", [NPRE * 128, 1024])
    rope = din("rope", [NPRE + NT, 128, 64])
    pmask = din("pmask", [128, NPRE])
    hflag = din("hflag", [128, 1])
    ln_g = [din("ln_g%d" % i, [1, 1024]) for i in range(3)]
    ln_b = [din("ln_b%d" % i, [1, 1024]) for i in range(3)]
    w_in = din("w_in", [1024, 2304])
    w_out = din("w_out", [1024, 1024])
    w_router = din("w_router", [1024, 256])
    rbias = din("rbias", [1, 256])
    w_gate = din("w_gate", [max(E_RUN, 1), 1024, 256])
    w_up = din("w_up", [max(E_RUN, 1), 1024, 256])
    w_down = din("w_down", [max(E_RUN, 1), 256, 1024])
    ws_gate = din("ws_gate", [1024, 256])
    ws_up = din("ws_up", [1024, 256])
    ws_down = din("ws_down", [256, 1024])
    sinks = din("sinks", [1, 8])
    c_ident = din("c_ident", [128, 128])
    c_decay = din("c_decay", [128, 512])
    c_zeta = din("c_zeta", [128, 4])
    c_pz = din("c_pz", [128, NPRE * 4])
    c_xi = din("c_xi", [128, 256])
    c_cd = din("c_cd", [128, 512])
    c_rb = din("c_rb", [128, 2048])
    c_mask = din("c_mask", [128, 2048])
    c_ltri = din("c_ltri", [128, 128])
    c_iota = din("c_iota", [128, 256])
    c_tok = din("c_tok", [128, NT * 2], I32)

    out = nc.dram_tensor("out", [NT * 128, 1024], F32, kind="ExternalOutput").ap()
    h2_dram = nc.dram_tensor("h2_dram", [NT * 128, 1024], F32, kind="Internal").ap()
    ys_dram = nc.dram_tensor("ys_dram", [(E + NT) * CAP, 1024], F32, kind="Internal").ap()
    list_dram = nc.dram_tensor("list_dram", [E * CAP, 2], I32, kind="Internal").ap()
    if DEBUG:
        dbg_h2 = nc.dram_tensor("dbg_h2", [NT * 128, 1024], F32, kind="ExternalOutput").ap()
        dbg_dest = nc.dram_tensor("dbg_dest", [128, NT * 8], I32, kind="ExternalOutput").ap()
        dbg_wk = nc.dram_tensor("dbg_wk", [128, NT * 8], F32, kind="ExternalOutput").ap()

    with ExitStack() as st0:
        P = Prog(nc, st0)

        cur = [16481]
        npad = [0]
        stack_marks = []

        def mk_alloc(st):
            base_at_entry = cur[0]
            st.callback(lambda: cur.__setitem__(0, base_at_entry))

            def sb(name, shape, dt=F32):
                a32 = (cur[0] + 31) // 32 * 32
                if a32 % SBUF_ALIGN:
                    npad[0] += 1
                    st.enter_context(nc.sbuf_tensor("pad%d" % npad[0], [128, 1], mybir.dt.uint8))
                    a32 += 32
                nbytes = int(np.prod(shape[1:])) * mybir.dt.size(dt)
                cur[0] = a32 + nbytes
                return st.enter_context(nc.sbuf_tensor(name, list(shape), dt))
            return sb

        sb0 = mk_alloc(st0)
        banks = [st0.enter_context(nc.psum_tensor("bank%d" % i, [128, 512], F32)) for i in range(8)]
        bres = [P.res("bank%d" % i) for i in range(8)]
        bctr = [0]

        def bank():
            k = bctr[0] % 7
            bctr[0] += 1
            return banks[k], bres[k]

        sbank, r_sbank = banks[7], bres[7]

        ident = sb0("ident", [128, 128]); r_ident = P.res("ident")
        destI = sb0("destI", [128, NT * 8], I32); r_destI = P.res("destI")
        wk_all = sb0("wk_all", [128, NT * 8]); r_wk = P.res("wk_all")
        r_h2d = P.res("h2_dram"); r_ys = P.res("ys_dram"); r_list = P.res("list_dram"); r_out = P.res("out")
        r_dbg = P.res("dbg")
        P.dma("sp", lambda e: e.dma_start(out=ident[:], in_=c_ident), W=[r_ident])

        mhalf = sb0("mhalf", [128, 1]); r_mhalf = P.res("mhalf")
        P.op("pool", lambda e: e.memset(mhalf[:], -0.5), W=[r_mhalf])

        def ln_stats(sbp, src, r_src, eps, width=1024, rs_eng="act"):
            stats, r_st = sbp["stats"].next()
            mv, r_mv = sbp["mv"].next()
            rstd, r_rstd = sbp["rstd"].next()
            nchunk = width // 512
            for c in range(nchunk):
                P.op("dve", lambda e, c=c: e.bn_stats(out=stats[:, c, :], in_=src[:, c * 512:(c + 1) * 512]),
                     R=[r_src], W=[r_st])
            P.op("dve", lambda e: e.bn_aggr(out=mv[:], in_=stats[:, 0:nchunk, :].rearrange("p a b -> p (a b)")),
                 R=[r_st], W=[r_mv])
            if rs_eng == "pool":
                P.op("pool", lambda e: e.tensor_scalar(out=rstd[:], in0=mv[:, 1:2], scalar1=eps, scalar2=None, op0=ALU.add),
                     R=[r_mv], W=[r_rstd])
                P.op("pool", lambda e: e.tensor_tensor(out=rstd[:], in0=rstd[:], in1=mhalf[:], op=ALU.pow),
                     R=[r_rstd, r_mhalf], W=[r_rstd])
                return mv, r_mv, rstd, r_rstd
            P.op("act", lambda e: e.activation(out=rstd[:], in_=mv[:, 1:2], func=AF.Sqrt, bias=eps, scale=1.0),
                 R=[r_mv], W=[r_rstd])
            P.op("dve", lambda e: e.reciprocal(out=rstd[:], in_=rstd[:]), R=[r_rstd], W=[r_rstd])
            return mv, r_mv, rstd, r_rstd

        def layer_norm(sbp, src, r_src, dst, r_dst, g_t, r_g, b_t, r_b, g_eng="pool", rs_eng="act"):
            mv, r_mv, rstd, r_rstd = ln_stats(sbp, src, r_src, 1e-5, rs_eng=rs_eng)
            P.op("dve", lambda e: e.tensor_scalar(out=dst[:], in0=src[:], scalar1=mv[:, 0:1], scalar2=rstd[:, 0:1],
                                                  op0=ALU.subtract, op1=ALU.mult),
                 R=[r_src, r_mv, r_rstd], W=[r_dst])
            P.op(g_eng, lambda e: e.tensor_tensor(out=dst[:], in0=dst[:], in1=g_t[:], op=ALU.mult),
                 R=[r_dst, r_g], W=[r_dst])
            P.op("pool", lambda e: e.tensor_tensor(out=dst[:], in0=dst[:], in1=b_t[:], op=ALU.add),
                 R=[r_dst, r_b], W=[r_dst])

        def transpose_to(src_fn, nchunks, r_src, dstT, r_dstT, rows=128, evac="mix"):
            for b0 in range(0, nchunks, 4):
                bk, r_bk = bank()
                nb = min(4, nchunks - b0)
                for c in range(b0, b0 + nb):
                    P.op("pe", lambda e, c=c, bk=bk, b0=b0: e.transpose(
                        out=bk[0:rows, (c - b0) * 128:(c - b0 + 1) * 128], in_=src_fn(c), identity=ident[:]),
                        R=[r_src, r_ident], W=[r_bk])
                eng = "act" if ((b0 // 4) % 2 == 0 or evac == "act") else "dve"
                if eng == "act":
                    P.op("act", lambda e, bk=bk, b0=b0, nb=nb: e.copy(
                        out=dstT[0:rows, b0:b0 + nb, :].rearrange("p a b -> p (a b)"), in_=bk[0:rows, 0:nb * 128]),
                        R=[r_bk], W=[r_dstT])
                else:
                    P.op("dve", lambda e, bk=bk, b0=b0, nb=nb: e.tensor_copy(
                        out=dstT[0:rows, b0:b0 + nb, :].rearrange("p a b -> p (a b)"), in_=bk[0:rows, 0:nb * 128]),
                        R=[r_bk], W=[r_dstT])

        with ExitStack() as stA:
            sb = mk_alloc(stA)
            w_in_t = sb("w_in_t", [128, 8, 2304], F32R); r_w_in = P.res("w_in")
            w_out_t = sb("w_out_t", [128, 8, 1024], F32R); r_w_out = P.res("w_out")
            w_r_t = sb("w_r_t", [128, 8, 256], F32R); r_w_r = P.res("w_r")
            lng = [sb("lng%d" % i, [128, 1024]) for i in range(2)]
            lnb = [sb("lnb%d" % i, [128, 1024]) for i in range(2)]
            r_lng = [P.res("lng%d" % i) for i in range(2)]
            r_lnb = [P.res("lnb%d" % i) for i in range(2)]
            decay_t = sb("decay_t", [128, 512]); r_decay = P.res("decay")
            zeta_t = sb("zeta_t", [128, 4]); r_zeta = P.res("zeta")
            pz_t = sb("pz_t", [128, NPRE * 4]); r_pz = P.res("pz")
            xi_t = sb("xi_t", [128, 256]); r_xi = P.res("xi")
            btab = sb("btab", [128, 2048]); r_btab = P.res("btab")
            ltri = sb("ltri", [128, 128]); r_ltri = P.res("ltri")
            ones_t = sb("ones_t", [128, 128]); r_ones = P.res("ones")
            iota_t = sb("iota_t", [128, 256]); r_iota = P.res("iota")
            rb_t = sb("rb_t", [128, 256]); r_rb = P.res("rbias")
            tok_t = sb("tok_t", [128, NT * 2], I32); r_tok = P.res("tok")
            pm_t = sb("pm_t", [128, NPRE]); r_pm = P.res("pm")
            hf_t = sb("hf_t", [128, 1]); r_hf = P.res("hf")
            sexp = sb("sexp", [128, 8]); r_sexp = P.res("sexp")
            state = sb("state", [128, 512]); r_state = P.res("state")
            selsum = sb("selsum", [128, 256]); r_selsum = P.res("selsum")

            sbp = {
                "stats": TPool(P, sb, "stats", [128, 4, 6], F32, 2),
                "mv": TPool(P, sb, "mv", [128, 2], F32, 2),
                "rstd": TPool(P, sb, "rstd", [128, 1], F32, 2),
            }
            p_xt = TPool(P, sb, "xt", [128, 1024], F32, 2)
            p_rp = TPool(P, sb, "rp", [128, 64], F32, 2)
            hN, r_hN = sb("hN", [128, 1024]), P.res("hN")
            hT, r_hT = sb("hT", [128, 8, 128], F32R), P.res("hT")
            qk_raw, r_qk_raw = sb("qk_raw", [128, 512]), P.res("qk_raw")
            qk_rot, r_qk_rot = sb("qk_rot", [128, 512]), P.res("qk_rot")
            rt = [sb("rt%d" % i, [128, 256]) for i in range(2)]
            r_rt = [P.res("rt%d" % i) for i in range(2)]
            v_sb, r_v = sb("v_sb", [128, 512]), P.res("v_sb")
            sg, r_sg = sb("sg", [128, 512]), P.res("sg")
            qs_pad, r_qs = sb("qs_pad", [128, 8, 128]), P.res("qs_pad")
            kv_sb, r_ks = sb("kv_sb", [128, 256]), P.res("kv_sb")
            ks_sb = kv_sb[:, 0:128]
            if VARIANT >= 100:
                spacer = sb("spacer", [128, 64])
            p_v1 = TPool(P, sb, "v1", [128, 2, 128], F32, 2)
            qT, r_qT = sb("qT", [128, 2, 128]), P.res("qT")
            krm, r_krm = sb("krm", [128, 4, 128]), P.res("krm")
            kTm, r_kTm = sb("kTm", [128, 4, 128]), P.res("kTm")
            qxT, r_qxT = sb("qxT", [128, 256]), P.res("qxT")
            scT, r_scT = qk_raw, r_qk_raw
            kz, r_kz = sb("kz", [128, 4, 128]), P.res("kz")
            gmv, r_gmv = sb("gmv", [128, 4, 2]), P.res("gmv")
            grstd, r_grstd = sb("grstd", [128, 4]), P.res("grstd")
            concat, r_cc = sb("concat", [128, 1024]), P.res("concat")
            qsT, r_qsT = sb("qsT", [128, 8, 128]), P.res("qsT")
            p_ksT = TPool(P, sb, "ksT", [128, 128], F32, 2)
            p_psb = TPool(P, sb, "psb", [128, 512], F32, 2)
            den, r_den = sb("den", [128, 8]), P.res("den")
            h2, r_h2 = hN, r_hN
            sc, r_sc = sb("sc", [128, 256]), P.res("sc")
            choice, r_choice = sb("choice", [128, 256]), P.res("choice")
            g8, r_g8 = sb("g8", [128, 8, 8]), P.res("g8")
            gs, r_gs = sb("gs", [128, 8]), P.res("gs")
            s8, r_s8 = sb("s8", [128, 8]), P.res("s8")
            pen, r_pen = sb("pen", [128, 8]), P.res("pen")
            m8, r_m8 = sb("m8", [128, 8]), P.res("m8")
            i8, r_i8 = sb("i8", [128, 8], U32), P.res("i8")
            ekf, r_ekf = sb("ekf", [128, 8]), P.res("ekf")
            sel, r_sel = sb("sel", [128, 256]), P.res("sel")
            wn, r_wn = sc, r_sc
            dsum, r_dsum = sb("dsum", [128, 1]), P.res("dsum")
            posd, r_posd = sel, r_sel
            junk, r_junk = choice, r_choice
            destf, r_destf = sb("destf", [128, 8]), P.res("destf")
            lfill, r_lfill = qk_raw[:].bitcast(I32), r_qk_raw

            for c8 in range(8):
                P.dma("pool", lambda e, c8=c8: e.dma_start(out=w_in_t[:, c8, :], in_=w_in[c8 * 128:(c8 + 1) * 128, :], max_dma_last_dim=4096), W=[r_w_in])
            P.dma("pool", lambda e: e.dma_start(out=w_out_t[:], in_=w_out.rearrange("(c p) n -> p c n", p=128)), W=[r_w_out])
            P.dma("pool", lambda e: e.dma_start(out=w_r_t[:], in_=w_router.rearrange("(c p) n -> p c n", p=128)), W=[r_w_r])
            for i in range(2):
                P.dma("sp", lambda e, i=i: e.dma_start(out=lng[i][:], in_=ln_g[i].partition_broadcast(128)), W=[r_lng[i]])
                P.dma("sp", lambda e, i=i: e.dma_start(out=lnb[i][:], in_=ln_b[i].partition_broadcast(128)), W=[r_lnb[i]])
            for (t_, r_, src) in ((decay_t, r_decay, c_decay), (zeta_t, r_zeta, c_zeta), (pz_t, r_pz, c_pz), (xi_t, r_xi, c_xi),
                                  (btab, r_btab, c_rb), (ltri, r_ltri, c_ltri),
                                  (iota_t, r_iota, c_iota), (tok_t, r_tok, c_tok),
                                  (pm_t, r_pm, pmask), (hf_t, r_hf, hflag)):
                P.dma("act", lambda e, t_=t_, src=src: e.dma_start(out=t_[:], in_=src), W=[r_])
            P.dma("act", lambda e: e.dma_start(out=rb_t[:], in_=rbias.partition_broadcast(128)), W=[r_rb])
            P.dma("act", lambda e: e.dma_start(out=sexp[:], in_=sinks.partition_broadcast(128)), W=[r_sexp])
            P.op("act", lambda e: e.activation(out=sexp[:], in_=sexp[:], func=AF.Exp), R=[r_sexp], W=[r_sexp])
            for q4 in range(2):
                mt, r_mt = p_xt.next()
                P.dma("sp", lambda e, mt=mt, q4=q4: e.dma_start(out=mt[:], in_=c_mask[:, q4 * 1024:(q4 + 1) * 1024]), W=[r_mt])
                P.op("pool", lambda e, mt=mt, q4=q4: e.tensor_tensor(out=btab[:, q4 * 1024:(q4 + 1) * 1024],
                                                                     in0=btab[:, q4 * 1024:(q4 + 1) * 1024], in1=mt[:], op=ALU.add),
                     R=[r_mt, r_btab], W=[r_btab])
            P.op("pool", lambda e: e.memset(ones_t[:], 1.0), W=[r_ones])
            P.op("pool", lambda e: e.memset(state[:], 0.0), W=[r_state])
            P.op("pool", lambda e: e.memset(selsum[:], 0.0), W=[r_selsum])
            P.op("pool", lambda e: e.memset(krm[:], 0.0), W=[r_krm])
            P.op("pool", lambda e: e.memset(kz[:], 0.0), W=[r_kz])
            P.op("pool", lambda e: e.memset(qs_pad[:], 0.0), W=[r_qs])
            for i in range(2):
                P.op("pool", lambda e, i=i: e.memset(p_v1.tiles[i][:], 0.0), W=[p_v1.ress[i]])
                P.op("pool", lambda e, i=i: e.memset(p_v1.tiles[i][:, :, 64:65], 1.0), W=[p_v1.ress[i]])
            P.op("pool", lambda e: e.memset(lfill, NT * 128), W=[r_lfill])
            P.dma("pool", lambda e: e.dma_start(out=list_dram.rearrange("(p r) c -> p (r c)", p=128), in_=lfill),
                  R=[r_lfill], W=[r_list])
            P.wait_all("pool", [r_list])

            prev_ksT = [None, None]
            prev_v1 = [None, None]

            def mixer_tile(gt, is_own, i_own, pending=None):
                xsrc = x_own if is_own else x_pre
                ti = i_own if is_own else gt
                xt, r_xt = p_xt.next()
                rp, r_rp = p_rp.next()
                P.dma("sp", lambda e: e.dma_start(out=xt[:], in_=xsrc[ti * 128:(ti + 1) * 128, :]), W=[r_xt])
                P.dma("act", lambda e: e.dma_start(out=rp[:], in_=rope[gt]), W=[r_rp])
                layer_norm(sbp, xt, r_xt, hN, r_hN, lng[0], r_lng[0], lnb[0], r_lnb[0])
                transpose_to(lambda c: hN[:, c * 128:(c + 1) * 128], 8, r_hN, hT, r_hT)
                if STAGE < 2:
                    return
                need_swa_kv = is_own or gt == NPRE - 1

                def proj(col0, ncols):
                    bk, r_bk = bank()
                    for c in range(8):
                        P.op("pe", lambda e, c=c: e.matmul(out=bk[:, 0:ncols], lhsT=hT[:, c, :],
                                                           rhs=w_in_t[:, c, col0:col0 + ncols], start=(c == 0), stop=(c == 7)),
                             R=[r_hT, r_w_in], W=[r_bk])
                    return bk, r_bk

                if is_own:
                    bk, r_bk = proj(0, 512)
                    P.op("act", lambda e: e.copy(out=qk_raw[:], in_=bk[:]), R=[r_bk], W=[r_qk_raw])
                    lo, nh = 0, 8
                else:
                    bk, r_bk = proj(256, 256)
                    P.op("act", lambda e: e.copy(out=qk_raw[:, 256:512], in_=bk[:, 0:256]), R=[r_bk], W=[r_qk_raw])
                    lo, nh = 256, 4
                src4 = qk_raw[:, lo:512].rearrange("p (h t d) -> p h t d", t=2, d=32)
                dst4 = qk_rot[:, lo:512].rearrange("p (h t d) -> p h t d", t=2, d=32)
                cosb = rp[:, 0:32].unsqueeze(1).to_broadcast([128, nh, 32])
                sinb = rp[:, 32:64].unsqueeze(1).to_broadcast([128, nh, 32])
                ta = rt[0][:, 0:nh * 32].rearrange("p (h d) -> p h d", d=32)
                tb = rt[1][:, 0:nh * 32].rearrange("p (h d) -> p h d", d=32)
                P.op("pool", lambda e: e.tensor_tensor(out=ta, in0=src4[:, :, 0, :], in1=cosb, op=ALU.mult), R=[r_qk_raw, r_rp], W=[r_rt[0]])
                P.op("dve", lambda e: e.tensor_tensor(out=tb, in0=src4[:, :, 1, :], in1=sinb, op=ALU.mult), R=[r_qk_raw, r_rp], W=[r_rt[1]])
                P.op("dve", lambda e: e.tensor_tensor(out=dst4[:, :, 0, :], in0=ta, in1=tb, op=ALU.subtract), R=[r_rt[0], r_rt[1]], W=[r_qk_rot])
                P.op("pool", lambda e: e.tensor_tensor(out=ta, in0=src4[:, :, 0, :], in1=sinb, op=ALU.mult), R=[r_qk_raw, r_rp], W=[r_rt[0]])
                P.op("dve", lambda e: e.tensor_tensor(out=tb, in0=src4[:, :, 1, :], in1=cosb, op=ALU.mult), R=[r_qk_raw, r_rp], W=[r_rt[1]])
                P.op("dve", lambda e: e.tensor_tensor(out=dst4[:, :, 1, :], in0=ta, in1=tb, op=ALU.add), R=[r_rt[0], r_rt[1]], W=[r_qk_rot])
                bk, r_bk = proj(512, 512)
                P.op("act", lambda e: e.copy(out=v_sb[:], in_=bk[:]), R=[r_bk], W=[r_v])
                for h in range(4):
                    if is_own:
                        P.op("pool", lambda e, h=h: e.tensor_scalar(out=kz[:, h, (h % 2) * 64:(h % 2 + 1) * 64], in0=qk_rot[:, 256 + h * 64:256 + (h + 1) * 64],
                                                                    scalar1=zeta_t[:, h:h + 1], scalar2=None, op0=ALU.mult),
                             R=[r_qk_rot, r_zeta], W=[r_kz])
                    else:
                        P.op("pool", lambda e, h=h: e.tensor_scalar(out=kz[:, h, (h % 2) * 64:(h % 2 + 1) * 64], in0=qk_rot[:, 256 + h * 64:256 + (h + 1) * 64],
                                                                    scalar1=zeta_t[:, h:h + 1], scalar2=pm_t[:, gt:gt + 1], op0=ALU.mult, op1=ALU.mult),
                             R=[r_qk_rot, r_zeta, r_pm], W=[r_kz])
                if STAGE < 2.1:
                    return
                if is_own:
                    bk, r_bk = proj(1024, 512)
                    P.op("act", lambda e: e.activation(out=sg[:], in_=bk[:], func=AF.Silu), R=[r_bk], W=[r_sg])
                    if STAGE < 2.12:
                        return
                    bk, r_bk = proj(1536, 512)
                    for kv in range(2):
                        P.op("act", lambda e, kv=kv: e.copy(out=qs_pad[:, kv * 4:(kv + 1) * 4, kv * 64:(kv + 1) * 64],
                                                            in_=bk[:, kv * 256:(kv + 1) * 256].rearrange("p (g d) -> p g d", d=64)),
                             R=[r_bk], W=[r_qs])
                if STAGE < 2.13:
                    return
                if need_swa_kv and STAGE >= 2.2:
                    bk, r_bk = proj(2048, 256)
                    v1, r_v1 = p_v1.next()
                    P.op("act", lambda e: e.copy(out=kv_sb[:], in_=bk[:, 0:256]), R=[r_bk], W=[r_ks])
                    P.op("pool", lambda e: e.tensor_copy(out=v1[:, :, 0:64], in_=kv_sb[:, 128:256].rearrange("p (k d) -> p k d", d=64)),
                         R=[r_ks], W=[r_v1])
                    ksT, r_ksT = p_ksT.next()
                    bk, r_bk = bank()
                    if VARIANT == 1:
                        bk, r_bk = bank()
                    if VARIANT != 2:
                        P.op("pe", lambda e: e.transpose(out=bk[:, 0:128], in_=ks_sb, identity=ident[:]), R=[r_ks, r_ident], W=[r_bk])
                    if VARIANT == 3:
                        P.op("dve", lambda e: e.tensor_copy(out=ksT[:], in_=bk[:, 0:128]), R=[r_bk], W=[r_ksT])
                    elif VARIANT != 4:
                        P.op("act", lambda e: e.copy(out=ksT[:], in_=bk[:, 0:128]), R=[r_bk], W=[r_ksT])
                if pending is not None:
                    pending()
                if is_own and STAGE >= 2.3:
                    for hp in range(2):
                        P.op("pool", lambda e, hp=hp: e.tensor_copy(
                            out=krm[:].rearrange("p (a b) c -> p a b c", b=2)[:, :, hp, hp * 64:(hp + 1) * 64],
                            in_=qk_rot[:, 256:512].rearrange("p (a b d) -> p a b d", b=2, d=64)[:, :, hp, :]),
                            R=[r_qk_rot], W=[r_krm])
                    transpose_to(lambda c: qk_rot[:, c * 128:(c + 1) * 128], 2, r_qk_rot, qT, r_qT)
                    transpose_to(lambda c: krm[:, c, :], 4, r_krm, kTm, r_kTm)
                    P.op("pool", lambda e: e.tensor_tensor(out=qxT[:], in0=qT[:].rearrange("p a b -> p (a b)"), in1=xi_t[:], op=ALU.mult),
                         R=[r_qT, r_xi], W=[r_qxT])
                    bk, r_bk = bank()
                    for h in range(4):
                        P.op("pe", lambda e, h=h: e.matmul(out=bk[:, h * 128:(h + 1) * 128], lhsT=kTm[:, h, :],
                                                           rhs=qT[:, h // 2, :], start=True, stop=True),
                             R=[r_kTm, r_qT], W=[r_bk])
                    P.op("dve", lambda e: e.tensor_tensor(out=scT[:], in0=bk[:], in1=decay_t[:], op=ALU.mult), R=[r_bk, r_decay], W=[r_scT])
                    rbk, r_rbk = bank()
                    for h in range(4):
                        P.op("pe", lambda e, h=h: e.matmul(out=rbk[:, h * 128:(h + 1) * 128], lhsT=scT[:, h * 128:(h + 1) * 128],
                                                           rhs=v_sb[:, h * 128:(h + 1) * 128], start=True, stop=False),
                             R=[r_scT, r_v], W=[r_rbk])
                        P.op("pe", lambda e, h=h: e.matmul(out=rbk[:, h * 128:(h + 1) * 128], lhsT=qxT[:, (h // 2) * 128:(h // 2 + 1) * 128],
                                                           rhs=state[:, h * 128:(h + 1) * 128], start=False, stop=True),
                             R=[r_qxT, r_state], W=[r_rbk])
                if STAGE < 2.4:
                    return
                kbk, r_kbk = bank()
                for h in range(4):
                    P.op("pe", lambda e, h=h: e.matmul(out=kbk[:, h * 128:(h + 1) * 128], lhsT=kz[:, h, :],
                                                       rhs=v_sb[:, h * 128:(h + 1) * 128], start=True, stop=True),
                         R=[r_kz, r_v], W=[r_kbk])
                for h in range(4):
                    P.op("pool", lambda e, h=h: e.tensor_scalar(out=state[:, h * 128:(h + 1) * 128], in0=state[:, h * 128:(h + 1) * 128],
                                                                scalar1=CHUNK_DECAY[h], scalar2=None, op0=ALU.mult), R=[r_state], W=[r_state])
                P.op("dve", lambda e: e.tensor_tensor(out=state[:], in0=kbk[:], in1=state[:], op=ALU.add), R=[r_kbk, r_state], W=[r_state])
                if not is_own:
                    if need_swa_kv:
                        prev_ksT[0], prev_ksT[1] = ksT, r_ksT
                        prev_v1[0], prev_v1[1] = v1, r_v1
                    return
                if STAGE < 3:
                    return
                stats, r_st = sbp["stats"].next()
                for h in range(4):
                    P.op("dve", lambda e, h=h: e.bn_stats(out=stats[:, h, :], in_=rbk[:, h * 128:(h + 1) * 128]), R=[r_rbk], W=[r_st])
                for h in range(4):
                    P.op("dve", lambda e, h=h: e.bn_aggr(out=gmv[:, h, :], in_=stats[:, h, :]), R=[r_st], W=[r_gmv])
                P.op("act", lambda e: e.activation(out=grstd[:], in_=gmv[:, :, 1], func=AF.Sqrt, bias=1e-6, scale=1.0), R=[r_gmv], W=[r_grstd])
                P.op("dve", lambda e: e.reciprocal(out=grstd[:], in_=grstd[:]), R=[r_grstd], W=[r_grstd])
                for h in range(4):
                    P.op("dve", lambda e, h=h: e.tensor_scalar(out=concat[:, h * 128:(h + 1) * 128], in0=rbk[:, h * 128:(h + 1) * 128],
                                                               scalar1=gmv[:, h, 0:1], scalar2=grstd[:, h:h + 1], op0=ALU.subtract, op1=ALU.mult),
                         R=[r_rbk, r_gmv, r_grstd], W=[r_cc])
                P.op("pool", lambda e: e.tensor_tensor(out=concat[:, 0:512], in0=concat[:, 0:512], in1=sg[:], op=ALU.mult), R=[r_cc, r_sg], W=[r_cc])
                if STAGE < 4:
                    return
                for b0 in range(2):
                    bk, r_bk = bank()
                    for hh in range(4):
                        hq = b0 * 4 + hh
                        P.op("pe", lambda e, hq=hq, hh=hh, bk=bk: e.transpose(out=bk[:, hh * 128:(hh + 1) * 128], in_=qs_pad[:, hq, :], identity=ident[:]),
                             R=[r_qs, r_ident], W=[r_bk])
                    if b0 == 0:
                        P.op("act", lambda e, bk=bk: e.copy(out=qsT[:, 0:4, :].rearrange("p a b -> p (a b)"), in_=bk[:]), R=[r_bk], W=[r_qsT])
                    else:
                        P.op("dve", lambda e, bk=bk: e.tensor_copy(out=qsT[:, 4:8, :].rearrange("p a b -> p (a b)"), in_=bk[:]), R=[r_bk], W=[r_qsT])
                obk = [bank(), bank()]
                for kv in range(2):
                    psbs = []
                    for half in range(2):
                        kT_, r_kT_ = (prev_ksT[0], prev_ksT[1]) if half == 0 else (ksT, r_ksT)
                        bk, r_bk = bank()
                        P.op("pe", lambda e, kv=kv, kT_=kT_, bk=bk: e.matmul(out=bk[:], lhsT=kT_[:],
                                                                           rhs=qsT[:, kv * 4:(kv + 1) * 4, :].rearrange("p a b -> p (a b)"), start=True, stop=True),
                             R=[r_kT_, r_qsT], W=[r_bk])
                        psb, r_psb = p_psb.next()
                        o0 = (kv * 2 + half) * 512
                        P.op("dve", lambda e, bk=bk, psb=psb, o0=o0: e.scalar_tensor_tensor(out=psb[:], in0=bk[:], scalar=0.125, in1=btab[:, o0:o0 + 512],
                                                                                         op0=ALU.mult, op1=ALU.add),
                             R=[r_bk, r_btab], W=[r_psb])
                        P.op("act", lambda e, psb=psb: e.activation(out=psb[:], in_=psb[:], func=AF.Exp), R=[r_psb], W=[r_psb])
                        if half == 0 and i_own == 0:
                            P.op("pool", lambda e, psb=psb: e.tensor_scalar(out=psb[:], in0=psb[:], scalar1=hf_t[:, 0:1], scalar2=None, op0=ALU.mult),
                                 R=[r_psb, r_hf], W=[r_psb])
                        psbs.append((psb, r_psb))
                    ob, r_ob = obk[kv]
                    for g in range(4):
                        for half in range(2):
                            v1_, r_v1_ = (prev_v1[0], prev_v1[1]) if half == 0 else (v1, r_v1)
                            psb, r_psb = psbs[half]
                            P.op("pe", lambda e, g=g, kv=kv, half=half, v1_=v1_, psb=psb, ob=ob: e.matmul(
                                out=ob[:, g * 128:g * 128 + 66], lhsT=psb[:, g * 128:(g + 1) * 128], rhs=v1_[:, kv, 0:66],
                                start=(half == 0), stop=(half == 1)),
                                R=[r_psb, r_v1_], W=[r_ob])
                for kv in range(2):
                    ob, r_ob = obk[kv]
                    ob3 = ob[:].rearrange("p (g c) -> p g c", c=128)
                    P.op("dve", lambda e, kv=kv, ob3=ob3: e.tensor_tensor(out=den[:, kv * 4:(kv + 1) * 4], in0=ob3[:, :, 64], in1=sexp[:, kv * 4:(kv + 1) * 4], op=ALU.add),
                         R=[r_ob, r_sexp], W=[r_den])
                P.op("dve", lambda e: e.reciprocal(out=den[:], in_=den[:]), R=[r_den], W=[r_den])
                for kv in range(2):
                    ob, r_ob = obk[kv]
                    ob3 = ob[:].rearrange("p (g c) -> p g c", c=128)
                    P.op("dve", lambda e, kv=kv, ob3=ob3: e.tensor_tensor(
                        out=concat[:, 512 + kv * 256:512 + (kv + 1) * 256].rearrange("p (g d) -> p g d", d=64),
                        in0=ob3[:, :, 0:64], in1=den[:, kv * 4:(kv + 1) * 4].unsqueeze(2).to_broadcast([128, 4, 64]), op=ALU.mult),
                        R=[r_ob, r_den], W=[r_cc])
                prev_ksT[0], prev_ksT[1] = ksT, r_ksT
                prev_v1[0], prev_v1[1] = v1, r_v1
                if STAGE < 5:
                    return
                transpose_to(lambda c: concat[:, c * 128:(c + 1) * 128], 8, r_cc, hT, r_hT)
                mb = [bank(), bank()]
                for nb_ in range(2):
                    bk, r_bk = mb[nb_]
                    for c in range(8):
                        P.op("pe", lambda e, c=c, nb_=nb_, bk=bk: e.matmul(out=bk[:], lhsT=hT[:, c, :], rhs=w_out_t[:, c, nb_ * 512:(nb_ + 1) * 512],
                                                                         start=(c == 0), stop=(c == 7)),
                             R=[r_hT, r_w_out], W=[r_bk])
                for nb_ in range(2):
                    bk, r_bk = mb[nb_]
                    P.op("dve", lambda e, nb_=nb_, bk=bk: e.scalar_tensor_tensor(out=concat[:, nb_ * 512:(nb_ + 1) * 512], in0=hN[:, nb_ * 512:(nb_ + 1) * 512],
                                                                               scalar=ALPHA, in1=bk[:], op0=ALU.mult, op1=ALU.add),
                         R=[r_hN, r_bk], W=[r_cc])
                layer_norm(sbp, concat, r_cc, h2, r_h2, lng[1], r_lng[1], lnb[1], r_lnb[1])
                P.dma("sp", lambda e: e.dma_start(out=h2_dram[i_own * 128:(i_own + 1) * 128, :], in_=h2[:]), R=[r_h2], Wn=[r_h2d])
                if DEBUG:
                    P.dma("sp", lambda e: e.dma_start(out=dbg_h2[i_own * 128:(i_own + 1) * 128, :], in_=h2[:]), R=[r_h2], Wn=[r_dbg])
                if STAGE < 6:
                    return
                transpose_to(lambda c: h2[:, c * 128:(c + 1) * 128], 8, r_h2, hT, r_hT)
                lb, r_lb = bank()
                for c in range(8):
                    P.op("pe", lambda e, c=c: e.matmul(out=lb[:, 0:256], lhsT=hT[:, c, :], rhs=w_r_t[:, c, :], start=(c == 0), stop=(c == 7)),
                         R=[r_hT, r_w_r], W=[r_lb])
                P.op("act", lambda e: e.activation(out=sc[:], in_=lb[:, 0:256], func=AF.Sigmoid), R=[r_lb], W=[r_sc])
                P.op("pool", lambda e: e.tensor_tensor(out=choice[:], in0=sc[:], in1=rb_t[:], op=ALU.add), R=[r_sc, r_rb], W=[r_choice])
                def tail():
                    for g in range(8):
                        P.op("dve", lambda e, g=g: e.max(out=g8[:, g, :], in_=choice[:, g * 32:(g + 1) * 32]), R=[r_choice], W=[r_g8])
                    P.op("dve", lambda e: e.tensor_tensor(out=gs[:], in0=g8[:, :, 0], in1=g8[:, :, 1], op=ALU.add), R=[r_g8], W=[r_gs])
                    P.op("dve", lambda e: e.max(out=s8[:], in_=gs[:]), R=[r_gs], W=[r_s8])
                    P.op("dve", lambda e: e.tensor_scalar(out=pen[:], in0=gs[:], scalar1=s8[:, 3:4], scalar2=1e9, op0=ALU.is_ge, op1=ALU.mult), R=[r_gs, r_s8], W=[r_pen])
                    P.op("dve", lambda e: e.tensor_scalar(out=pen[:], in0=pen[:], scalar1=-1e9, scalar2=None, op0=ALU.add), R=[r_pen], W=[r_pen])
                    P.op("dve", lambda e: e.tensor_tensor(out=choice[:].rearrange("p (g j) -> p g j", j=32), in0=choice[:].rearrange("p (g j) -> p g j", j=32),
                                                          in1=pen[:].unsqueeze(2).to_broadcast([128, 8, 32]), op=ALU.add), R=[r_choice, r_pen], W=[r_choice])
                    P.op("dve", lambda e: e.max(out=m8[:], in_=choice[:]), R=[r_choice], W=[r_m8])
                    P.op("dve", lambda e: e.max_index(out=i8[:], in_max=m8[:], in_values=choice[:]), R=[r_choice, r_m8], W=[r_i8])
                    P.op("dve", lambda e: e.tensor_copy(out=ekf[:], in_=i8[:]), R=[r_i8], W=[r_ekf])
                    P.op("dve", lambda e: e.tensor_scalar(out=sel[:], in0=choice[:], scalar1=m8[:, 7:8], scalar2=None, op0=ALU.is_ge), R=[r_choice, r_m8], W=[r_sel])
                    P.op("dve", lambda e: e.tensor_tensor(out=wn[:], in0=sel[:], in1=sc[:], op=ALU.mult), R=[r_sel, r_sc], W=[r_wn])
                    P.op("dve", lambda e: e.reduce_sum(out=dsum[:], in_=wn[:], axis=mybir.AxisListType.X), R=[r_wn], W=[r_dsum])
                    P.op("dve", lambda e: e.reciprocal(out=dsum[:], in_=dsum[:]), R=[r_dsum], W=[r_dsum])
                    P.op("dve", lambda e: e.tensor_scalar(out=wn[:], in0=wn[:], scalar1=dsum[:, 0:1], scalar2=2.5, op0=ALU.mult, op1=ALU.mult), R=[r_wn, r_dsum], W=[r_wn])
                    pb, r_pb = bank()
                    P.op("pe", lambda e: e.matmul(out=pb[:, 0:256], lhsT=ltri[:], rhs=sel[:], start=True, stop=False), R=[r_ltri, r_sel], W=[r_pb])
                    P.op("pe", lambda e: e.matmul(out=pb[:, 0:256], lhsT=ones_t[:], rhs=selsum[:], start=False, stop=True), R=[r_ones, r_selsum], W=[r_pb])
                    P.op("pool", lambda e: e.tensor_tensor(out=selsum[:], in0=selsum[:], in1=sel[:], op=ALU.add), R=[r_selsum, r_sel], W=[r_selsum])
                    P.op("dve", lambda e: e.scalar_tensor_tensor(out=posd[:], in0=iota_t[:], scalar=float(CAP), in1=pb[:, 0:256], op0=ALU.mult, op1=ALU.add), R=[r_pb, r_iota], W=[r_posd])
                    for k in range(8):
                        P.op("dve", lambda e, k=k: e.scalar_tensor_tensor(out=junk[:], in0=iota_t[:], scalar=ekf[:, k:k + 1], in1=wn[:], op0=ALU.is_equal, op1=ALU.mult,
                                                                         accum_out=wk_all[:, i_own * 8 + k:i_own * 8 + k + 1]),
                             R=[r_iota, r_ekf, r_wn], W=[r_junk, r_wk])
                        P.op("dve", lambda e, k=k: e.scalar_tensor_tensor(out=junk[:], in0=iota_t[:], scalar=ekf[:, k:k + 1], in1=posd[:], op0=ALU.is_equal, op1=ALU.mult,
                                                                         accum_out=destf[:, k:k + 1]),
                             R=[r_iota, r_ekf, r_posd], W=[r_junk, r_destf])
                    P.op("dve", lambda e: e.tensor_copy(out=destI[:, i_own * 8:(i_own + 1) * 8], in_=destf[:]), R=[r_destf], W=[r_destI])
                    if STAGE < 7:
                        return
                    for k in range(8):
                        P.dma("pool", lambda e, k=k: e.indirect_dma_start(
                            out=list_dram, out_offset=bass.IndirectOffsetOnAxis(ap=destI[:, i_own * 8 + k:i_own * 8 + k + 1], axis=0),
                            in_=tok_t[:, i_own * 2:i_own * 2 + 2], in_offset=None, bounds_check=P.reg(e, E * CAP - 1), oob_is_err=False),
                            R=[r_destI, r_tok], Wn=[r_list])

                return tail

            def prefix_F(gt):
                xt, r_xt = p_xt.next()
                rp, r_rp = p_rp.next()
                P.dma("sp", lambda e: e.dma_start(out=xt[:], in_=x_pre[gt * 128:(gt + 1) * 128, :]), W=[r_xt])
                P.dma("act", lambda e: e.dma_start(out=rp[:], in_=rope[gt]), W=[r_rp])
                layer_norm(sbp, xt, r_xt, hN, r_hN, lng[0], r_lng[0], lnb[0], r_lnb[0], g_eng="dve", rs_eng="pool")
                transpose_to(lambda c: hN[:, c * 128:(c + 1) * 128], 8, r_hN, hT, r_hT, evac="act")
                if gt % 2 == 0:
                    kraw, r_kraw, vbuf, r_vbuf = qk_raw, r_qk_raw, v_sb, r_v
                else:
                    kraw, r_kraw, vbuf, r_vbuf = sg, r_sg, p_psb.tiles[0], p_psb.ress[0]

                def proj(col0, ncols):
                    bk, r_bk = bank()
                    for c in range(8):
                        P.op("pe", lambda e, c=c: e.matmul(out=bk[:, 0:ncols], lhsT=hT[:, c, :],
                                                           rhs=w_in_t[:, c, col0:col0 + ncols], start=(c == 0), stop=(c == 7)),
                             R=[r_hT, r_w_in], W=[r_bk])
                    return bk, r_bk

                bk, r_bk = proj(256, 256)
                P.op("act", lambda e: e.copy(out=kraw[:, 256:512], in_=bk[:, 0:256]), R=[r_bk], W=[r_kraw])
                bk, r_bk = proj(512, 512)
                P.op("act", lambda e: e.copy(out=vbuf[:], in_=bk[:]), R=[r_bk], W=[r_vbuf])
                if gt == NPRE - 1:
                    bk, r_bk = proj(2048, 256)
                    v1, r_v1 = p_v1.next()
                    P.op("act", lambda e: e.copy(out=kv_sb[:], in_=bk[:, 0:256]), R=[r_bk], W=[r_ks])
                    P.op("pool", lambda e: e.tensor_copy(out=v1[:, :, 0:64], in_=kv_sb[:, 128:256].rearrange("p (k d) -> p k d", d=64)),
                         R=[r_ks], W=[r_v1])
                    ksT, r_ksT = p_ksT.next()
                    bk, r_bk = bank()
                    P.op("pe", lambda e: e.transpose(out=bk[:, 0:128], in_=ks_sb, identity=ident[:]), R=[r_ks, r_ident], W=[r_bk])
                    P.op("act", lambda e: e.copy(out=ksT[:], in_=bk[:, 0:128]), R=[r_bk], W=[r_ksT])
                    prev_ksT[0], prev_ksT[1] = ksT, r_ksT
                    prev_v1[0], prev_v1[1] = v1, r_v1
                return (gt, rp, r_rp, kraw, r_kraw, vbuf, r_vbuf)

            def prefix_G(ctx):
                gt, rp, r_rp, kraw, r_kraw, vbuf, r_vbuf = ctx
                src4 = kraw[:, 256:512].rearrange("p (h t d) -> p h t d", t=2, d=32)
                dst4 = qk_rot[:, 256:512].rearrange("p (h t d) -> p h t d", t=2, d=32)
                cosb = rp[:, 0:32].unsqueeze(1).to_broadcast([128, 4, 32])
                sinb = rp[:, 32:64].unsqueeze(1).to_broadcast([128, 4, 32])
                ta = rt[0][:, 0:128].rearrange("p (h d) -> p h d", d=32)
                tb = rt[1][:, 0:128].rearrange("p (h d) -> p h d", d=32)
                P.op("dve", lambda e: e.tensor_tensor(out=ta, in0=src4[:, :, 0, :], in1=cosb, op=ALU.mult), R=[r_kraw, r_rp], W=[r_rt[0]])
                P.op("dve", lambda e: e.tensor_tensor(out=tb, in0=src4[:, :, 1, :], in1=sinb, op=ALU.mult), R=[r_kraw, r_rp], W=[r_rt[1]])
                P.op("dve", lambda e: e.tensor_tensor(out=dst4[:, :, 0, :], in0=ta, in1=tb, op=ALU.subtract), R=[r_rt[0], r_rt[1]], W=[r_qk_rot])
                P.op("dve", lambda e: e.tensor_tensor(out=ta, in0=src4[:, :, 0, :], in1=sinb, op=ALU.mult), R=[r_kraw, r_rp], W=[r_rt[0]])
                P.op("dve", lambda e: e.tensor_tensor(out=tb, in0=src4[:, :, 1, :], in1=cosb, op=ALU.mult), R=[r_kraw, r_rp], W=[r_rt[1]])
                P.op("dve", lambda e: e.tensor_tensor(out=dst4[:, :, 1, :], in0=ta, in1=tb, op=ALU.add), R=[r_rt[0], r_rt[1]], W=[r_qk_rot])
                for h in range(4):
                    P.op("pool", lambda e, h=h: e.tensor_scalar(out=kz[:, h, (h % 2) * 64:(h % 2 + 1) * 64], in0=qk_rot[:, 256 + h * 64:256 + (h + 1) * 64],
                                                                scalar1=pz_t[:, gt * 4 + h:gt * 4 + h + 1], scalar2=pm_t[:, gt:gt + 1], op0=ALU.mult, op1=ALU.mult),
                         R=[r_qk_rot, r_pz, r_pm], W=[r_kz])
                for h in range(4):
                    P.op("pe", lambda e, h=h: e.matmul(out=sbank[:, h * 128:(h + 1) * 128], lhsT=kz[:, h, :],
                                                       rhs=vbuf[:, h * 128:(h + 1) * 128], start=(gt == 0 and h == 0), stop=(gt == NPRE - 1)),
                         R=[r_kz, r_vbuf], W=[r_sbank])

            ctx_prev = None
            for gt in range(NPRE):
                ctx = prefix_F(gt)
                if ctx_prev is not None:
                    prefix_G(ctx_prev)
                ctx_prev = ctx
            if ctx_prev is not None:
                prefix_G(ctx_prev)
                P.op("act", lambda e: e.copy(out=state[:], in_=sbank[:]), R=[r_sbank], W=[r_state])
            pending = None
            for i in range(NT):
                pending = mixer_tile(NPRE + i, True, i, pending)
            if pending is not None:
                pending()
            if DEBUG:
                P.dma("sp", lambda e: e.dma_start(out=dbg_dest, in_=destI[:]), R=[r_destI], Wn=[r_dbg])
                P.dma("sp", lambda e: e.dma_start(out=dbg_wk, in_=wk_all[:]), R=[r_wk], Wn=[r_dbg])
            P.barrier()

        with ExitStack() as stB:
            sb = mk_alloc(stB)
            p_sgu = TPool(P, sb, "sgu", [128, 2, 2048], F32, 3)
            p_sdn = TPool(P, sb, "sdn", [128, 2, 1024], F32, 3)
            p_gu = TPool(P, sb, "wgu", [128, 8, 512], F32R, 2)
            p_dn = TPool(P, sb, "wdn", [128, 2, 1024], F32R, 4)
            p_idx = TPool(P, sb, "idx", [128, 2], I32, 3)
            p_xg = TPool(P, sb, "xg", [128, 1024], F32, 3)
            p_xgT = TPool(P, sb, "xgT", [128, 8, 128], F32R, 2)
            p_sg2 = TPool(P, sb, "sg2", [128, 256], F32, 2)
            p_hh = TPool(P, sb, "hh", [128, 256], F32, 2)
            p_hhT = TPool(P, sb, "hhT", [128, 2, 128], F32R, 2)
            p_y = TPool(P, sb, "ysb", [128, 1024], F32, 2)
            lng2 = sb("lng2", [128, 1024]); r_lng2 = P.res("lng2")
            lnb2 = sb("lnb2", [128, 1024]); r_lnb2 = P.res("lnb2")
            sbp = {
                "stats": TPool(P, sb, "statsB", [128, 4, 6], F32, 2),
                "mv": TPool(P, sb, "mvB", [128, 2], F32, 2),
                "rstd": TPool(P, sb, "rstdB", [128, 1], F32, 2),
            }
            P.dma("act", lambda e: e.dma_start(out=lng2[:], in_=ln_g[2].partition_broadcast(128)), W=[r_lng2])
            P.dma("act", lambda e: e.dma_start(out=lnb2[:], in_=ln_b[2].partition_broadcast(128)), W=[r_lnb2])

            njobs = E_RUN + (NT if PHASE3 else 0)
            jobs = {}
            for i in range(3):
                P.op("pool", lambda e, i=i: e.memset(p_xg.tiles[i][:], 0.0), W=[p_xg.ress[i]])

            def st_L(j):
                c = {}
                jobs[j] = c
                xg, r_xg = p_xg.next()
                c["xg"] = (xg, r_xg)
                sgu, r_sgu = p_sgu.next()
                sdn, r_sdn = p_sdn.next()
                c["sgu"] = (sgu, r_sgu)
                c["sdn"] = (sdn, r_sdn)
                if j < E_RUN:
                    wg_, wu_, wd_ = w_gate[j], w_up[j], w_down[j]
                else:
                    wg_, wu_, wd_ = ws_gate, ws_up, ws_down
                P.dma("sp", lambda e: e.dma_start(out=sgu[:, 0, :], in_=wg_.rearrange("(p c) n -> p (c n)", c=8)), W=[r_sgu])
                P.dma("sp", lambda e: e.dma_start(out=sgu[:, 1, :], in_=wu_.rearrange("(p c) n -> p (c n)", c=8)), W=[r_sgu])
                P.dma("sp", lambda e: e.dma_start(out=sdn[:].rearrange("p c n -> p (c n)"), in_=wd_.rearrange("(p c) n -> p (c n)", c=2)), W=[r_sdn])
                if j < E_RUN:
                    idx, r_idx = p_idx.next()
                    P.dma("pool", lambda e: e.dma_start(out=idx[:], in_=list_dram[j * CAP:(j + 1) * CAP, :]), R=[r_list], W=[r_idx])
                    P.dma("pool", lambda e: e.indirect_dma_start(out=xg[:], out_offset=None, in_=h2_dram,
                                                                 in_offset=bass.IndirectOffsetOnAxis(ap=idx[:, 0:1], axis=0),
                                                                 bounds_check=P.reg(e, NT * 128 - 1), oob_is_err=False),
                          R=[r_h2d, r_idx], W=[r_xg])
                else:
                    i = j - E_RUN
                    P.dma("act", lambda e: e.dma_start(out=xg[:], in_=h2_dram[i * 128:(i + 1) * 128, :]), R=[r_h2d], W=[r_xg])

            def st_R(j):
                c = jobs[j]
                sgu, r_sgu = c["sgu"]
                sdn, r_sdn = c["sdn"]
                gu, r_gu = p_gu.next()
                dn, r_dn = p_dn.next()
                c["gu"] = (gu, r_gu)
                c["dn"] = (dn, r_dn)
                P.op("act", lambda e: e.copy(out=gu[:, :, 0:256], in_=sgu[:, 0, :].rearrange("p (c n) -> p c n", n=256)), R=[r_sgu], W=[r_gu])
                P.op("dve", lambda e: e.tensor_copy(out=gu[:, :, 256:512], in_=sgu[:, 1, :].rearrange("p (c n) -> p c n", n=256)), R=[r_sgu], W=[r_gu])
                P.op("act", lambda e: e.copy(out=dn[:, 0, :], in_=sdn[:, 0, :]), R=[r_sdn], W=[r_dn])
                P.op("dve", lambda e: e.tensor_copy(out=dn[:, 1, :], in_=sdn[:, 1, :]), R=[r_sdn], W=[r_dn])

            def st_A(j):
                c = jobs[j]
                xg, r_xg = c["xg"]
                xgT, r_xgT = p_xgT.next()
                c["xgT"] = (xgT, r_xgT)
                xv = xg[:].rearrange("s (p c) -> s c p", c=8)
                transpose_to(lambda cc: xv[:, cc, :], 8, r_xg, xgT, r_xgT)

            def st_B(j):
                c = jobs[j]
                xgT, r_xgT = c["xgT"]
                gu, r_gu = c["gu"]
                gb, r_gb = bank()
                for cc in range(8):
                    P.op("pe", lambda e, cc=cc: e.matmul(out=gb[:], lhsT=xgT[:, cc, :], rhs=gu[:, cc, :], start=(cc == 0), stop=(cc == 7)),
                         R=[r_xgT, r_gu], W=[r_gb])
                sg2, r_sg2 = p_sg2.next()
                hh, r_hh = p_hh.next()
                P.op("act", lambda e: e.activation(out=sg2[:], in_=gb[:, 0:256], func=AF.Silu), R=[r_gb], W=[r_sg2])
                P.op("dve", lambda e: e.tensor_tensor(out=hh[:], in0=gb[:, 256:512], in1=sg2[:], op=ALU.mult), R=[r_gb, r_sg2], W=[r_hh])
                c["hh"] = (hh, r_hh)

            def st_C(j):
                c = jobs[j]
                hh, r_hh = c["hh"]
                hhT, r_hhT = p_hhT.next()
                c["hhT"] = (hhT, r_hhT)
                hv = hh[:].rearrange("s (p c) -> s c p", c=2)
                transpose_to(lambda cc: hv[:, cc, :], 2, r_hh, hhT, r_hhT)

            def st_D(j):
                c = jobs[j]
                hhT, r_hhT = c["hhT"]
                dn, r_dn = c["dn"]
                yb = [bank(), bank()]
                for nb_ in range(2):
                    bk, r_bk = yb[nb_]
                    for cc in range(2):
                        P.op("pe", lambda e, cc=cc, nb_=nb_, bk=bk: e.matmul(out=bk[:], lhsT=hhT[:, cc, :], rhs=dn[:, cc, nb_ * 512:(nb_ + 1) * 512],
                                                                           start=(cc == 0), stop=(cc == 1)),
                             R=[r_hhT, r_dn], W=[r_bk])
                ysb, r_ysb = p_y.next()
                P.op("act", lambda e: e.copy(out=ysb[:, 0:512], in_=yb[0][0][:]), R=[yb[0][1]], W=[r_ysb])
                P.op("dve", lambda e: e.tensor_copy(out=ysb[:, 512:1024], in_=yb[1][0][:]), R=[yb[1][1]], W=[r_ysb])
                P.dma("pool", lambda e: e.dma_start(out=ys_dram[j * CAP:(j + 1) * CAP, :], in_=ysb[:]), R=[r_ysb], Wn=[r_ys])
                del jobs[j]

            for it in range(-4, njobs):
                for (fn, off) in ((st_L, 4), (st_R, 3), (st_A, 3), (st_B, 2), (st_C, 1), (st_D, 0)):
                    j = it + off
                    if 0 <= j < njobs:
                        fn(j)

            p_acc = TPool(P, sb, "acc", [128, 1024], F32, 2)
            p_yk = TPool(P, sb, "yk", [128, 1024], F32, 3)
            for i in range(NT if PHASE3 else 0):
                xg, r_xg = p_xg.next()
                ysh, r_ysh = p_xg.next()
                P.dma("act", lambda e: e.dma_start(out=xg[:], in_=h2_dram[i * 128:(i + 1) * 128, :]), R=[r_h2d], W=[r_xg])
                P.dma("act", lambda e: e.dma_start(out=ysh[:], in_=ys_dram[(E_RUN + i) * CAP:(E_RUN + i + 1) * CAP, :]), R=[r_ys], W=[r_ysh])
                acc, r_acc = p_acc.next()
                P.op("dve", lambda e: e.scalar_tensor_tensor(out=acc[:], in0=xg[:], scalar=ALPHA, in1=ysh[:], op0=ALU.mult, op1=ALU.add),
                     R=[r_xg, r_ysh], W=[r_acc])
                for k in range(8):
                    yk, r_yk = p_yk.next()
                    P.dma("pool", lambda e, k=k: e.indirect_dma_start(
                        out=yk[:], out_offset=None, in_=ys_dram,
                        in_offset=bass.IndirectOffsetOnAxis(ap=destI[:, i * 8 + k:i * 8 + k + 1], axis=0),
                        bounds_check=P.reg(e, E * CAP - 1), oob_is_err=False),
                        R=[r_ys, r_destI], W=[r_yk])
                    P.op("dve", lambda e, k=k: e.scalar_tensor_tensor(
                        out=acc[:], in0=yk[:], scalar=wk_all[:, i * 8 + k:i * 8 + k + 1], in1=acc[:], op0=ALU.mult, op1=ALU.add),
                        R=[r_yk, r_wk, r_acc], W=[r_acc])
                ysb, r_ysb = p_y.next()
                layer_norm(sbp, acc, r_acc, ysb, r_ysb, lng2, r_lng2, lnb2, r_lnb2)
                P.dma("sp", lambda e: e.dma_start(out=out[i * 128:(i + 1) * 128, :], in_=ysb[:]), R=[r_ysb], Wn=[r_out])
            P.wait_all("sp", [r_out, r_dbg])
            P.barrier()
        P.emit()
    return nc


def _t5_bucket(n):
    n = np.maximum(n, 0)
    ratio = np.log(np.maximum(n, 1).astype(np.float32) / np.float32(16)) / np.float32(math.log(8.0))
    large = 16 + (ratio * np.float32(16)).astype(np.int32)
    large = np.minimum(large, 31)
    return np.where(n < 16, n, large)


def _constants():
    c = {}
    c["c_ident"] = np.eye(128, dtype=np.float32)
    H = 4
    lg = np.log(1.0 - 2.0 ** (-5.0 - np.arange(H, dtype=np.float64)))
    idx = np.arange(128, dtype=np.float64)
    qk_scale = 64.0 ** -0.5
    dec = np.zeros((128, H, 128), np.float64)
    for h in range(H):
        d = idx[None, :] - idx[:, None]
        dec[:, h, :] = np.where(d >= 0, np.exp(np.maximum(d, 0) * lg[h]), 0.0) * qk_scale
    c["c_decay"] = dec.reshape(128, 512).astype(np.float32)
    zeta = np.exp((127.0 - idx)[:, None] * lg[None, :]) * qk_scale
    c["c_zeta"] = zeta.astype(np.float32)
    pz = np.zeros((128, NPRE, 4), np.float64)
    for t in range(NPRE):
        pz[:, t, :] = zeta * np.exp(128.0 * lg[None, :] * (NPRE - 1 - t))
    c["c_pz"] = pz.reshape(128, NPRE * 4).astype(np.float32)
    xi = np.exp((idx + 1.0)[None, :] * lg[:, None])
    xil = np.zeros((128, 2, 128), np.float64)
    cdl = np.zeros((128, 4, 128), np.float64)
    for h in range(H):
        po = (h % 2) * 64
        xil[po:po + 64, h // 2, :] = xi[h][None, :]
        cdl[:, h, :] = np.exp(128.0 * lg[h])
    c["c_xi"] = xil.reshape(128, 256).astype(np.float32)
    c["c_cd"] = cdl.reshape(128, 512).astype(np.float32)
    c["c_ltri"] = (np.arange(128)[:, None] < np.arange(128)[None, :]).astype(np.float32)
    c["c_iota"] = np.tile(np.arange(256, dtype=np.float32)[None, :], (128, 1))
    tok = np.zeros((128, NT, 2), np.int32)
    for i in range(NT):
        tok[:, i, :] = (i * 128 + np.arange(128))[:, None]
    c["c_tok"] = tok.reshape(128, NT * 2)
    i_ = np.arange(128)[None, :]
    j_ = np.arange(128)[:, None]
    mask = np.zeros((128, 2, 2, 4, 128), np.float32)
    bidx = np.zeros((128, 2, 128), np.int64)
    for half in range(2):
        dist = i_ + 128 - (j_ + half * 128)
        valid = (dist >= 0) & (dist < 128)
        mask[:, :, half, :, :] = np.where(valid, 0.0, NEG)[:, None, None, :]
        bidx[:, half, :] = _t5_bucket(np.clip(dist, 0, 127))
    c["c_mask"] = mask.reshape(128, 2048)
    return c, bidx


def _prepare_inputs(inp):
    consts, bidx = _constants()
    x = np.ascontiguousarray(inp["x"], dtype=np.float32)
    rel_bias = np.asarray(inp["rel_bias"], np.float32)
    rb = np.zeros((128, 2, 2, 4, 128), np.float32)
    for kv in range(2):
        for g in range(4):
            for half in range(2):
                rb[:, kv, half, g, :] = rel_bias[bidx[:, half, :], kv * 4 + g]
    consts["c_rb"] = rb.reshape(128, 2048)
    inv = 10000.0 ** (-np.arange(32, dtype=np.float32) / np.float32(32))
    shared = {
        "ln_g0": inp["ln_in_g"].reshape(1, 1024), "ln_b0": inp["ln_in_b"].reshape(1, 1024),
        "ln_g1": inp["ln_mix_g"].reshape(1, 1024), "ln_b1": inp["ln_mix_b"].reshape(1, 1024),
        "ln_g2": inp["ln_ffn_g"].reshape(1, 1024), "ln_b2": inp["ln_ffn_b"].reshape(1, 1024),
        "w_in": inp["w_in"][0], "w_out": inp["w_out"][0], "w_router": inp["w_router"][0],
        "rbias": inp["router_bias"].reshape(1, 256),
        "w_gate": inp["w_gate"][0][:max(E_RUN, 1)], "w_up": inp["w_up"][0][:max(E_RUN, 1)], "w_down": inp["w_down"][0][:max(E_RUN, 1)],
        "ws_gate": inp["ws_gate"][0], "ws_up": inp["ws_up"][0], "ws_down": inp["ws_down"][0],
        "sinks": inp["attn_sinks"].reshape(1, 8),
    }
    shared = {k: np.ascontiguousarray(v, dtype=np.float32) for k, v in shared.items()}
    shared.update(consts)
    in_maps = []
    for c in range(NCORES):
        b, q = c // 4, c % 4
        t0 = q * NT * 128
        m = dict(shared)
        m["x_own"] = x[b, t0:t0 + NT * 128]
        xp = np.zeros((NPRE * 128, 1024), np.float32)
        npre = min(t0, NPRE * 128)
        if npre:
            xp[NPRE * 128 - npre:] = x[b, t0 - npre:t0]
        m["x_pre"] = xp
        pm = np.zeros((128, NPRE), np.float32)
        pm[:, NPRE - npre // 128:] = 1.0 if npre else 0.0
        if not npre:
            pm[:] = 0.0
        m["pmask"] = pm
        m["hflag"] = np.full((128, 1), 1.0 if q > 0 else 0.0, np.float32)
        pos = (t0 - NPRE * 128 + np.arange((NPRE + NT) * 128)).astype(np.float32)
        ang = pos[:, None] * inv[None, :]
        rp = np.concatenate([np.cos(ang), np.sin(ang)], axis=1).astype(np.float32)
        m["rope"] = rp.reshape(NPRE + NT, 128, 64)
        in_maps.append(m)
    return in_maps


_NC_CACHE = {}


def kernel(**inputs):
    in_maps = _prepare_inputs(inputs)
    if "nc" not in _NC_CACHE:
        _NC_CACHE["nc"] = build_program()
    res = run_bass_kernel_spmd(_NC_CACHE["nc"], in_maps, core_ids=list(range(NCORES)))
    outs = [np.asarray(r["out"], dtype=np.float32) for r in res.results]
    full = np.stack(outs, 0).reshape(2, 4 * NT * 128, 1024)
    if DEBUG:
        kernel.dbg = res.results
    return full
```

```python
import math
import types
from contextlib import ExitStack

import numpy as np
import concourse.bass as bass
import concourse.mybir as mybir
from concourse.bass_utils import run_bass_kernel_spmd

F32 = mybir.dt.float32
F32R = mybir.dt.float32r
I32 = mybir.dt.int32
U32 = mybir.dt.uint32
AF = mybir.ActivationFunctionType
ALU = mybir.AluOpType

NCORES = 8
NT = 16
NPRE = 48
E = 256
E_RUN = 256
PHASE3 = True
VARIANT = 0
SBUF_ALIGN = 32
STAGE = 7
CAP = 128
ALPHA = 2.0 ** 0.25
NEG = -200.0
CHUNK_DECAY = [float(np.float32((1.0 - 2.0 ** (-5.0 - h)) ** 128)) for h in range(4)]
DEBUG = False


class Res:
    __slots__ = ("name", "w", "rs", "dsem", "dcount")

    def __init__(self, name):
        self.name = name
        self.w = None
        self.rs = []
        self.dsem = None
        self.dcount = 0


def _freeze(fn):
    if fn.__closure__ is None:
        return fn
    cells = []
    for c in fn.__closure__:
        try:
            cells.append(types.CellType(c.cell_contents))
        except ValueError:
            cells.append(c)
    return types.FunctionType(fn.__code__, fn.__globals__, fn.__name__, fn.__defaults__, tuple(cells))


class Prog:
    ENG = ("pe", "dve", "act", "pool", "sp")

    def __init__(self, nc, stack):
        self.nc = nc
        self.stack = stack
        self.stream = {e: [] for e in self.ENG}
        self.seq = {e: 0 for e in self.ENG}
        self.known = {e: {} for e in self.ENG}
        self.esem = {e: stack.enter_context(nc.semaphore("es_" + e)) for e in self.ENG}
        self.used = set()
        self.nd = 0
        self.allres = []
        self._regs = {}

    def reg(self, eng, val):
        key = (id(eng), val)
        if key not in self._regs:
            self._regs[key] = eng.to_reg(val)
        return self._regs[key]

    def res(self, name):
        r = Res(name)
        self.allres.append(r)
        return r

    def _need(self, eng, toks):
        kn = self.known[eng]
        for t in toks:
            if t is None:
                continue
            if t[0] == "e":
                if t[1] == "pe" and eng == "pe":
                    continue
                key = ("e", t[1])
                if kn.get(key, 0) >= t[2]:
                    continue
                kn[key] = t[2]
                self.used.add((t[1], t[2]))
                self.stream[eng].append(("we", t[1], t[2]))
            else:
                key = ("d", id(t[1]))
                if kn.get(key, 0) >= t[2]:
                    continue
                kn[key] = t[2]
                self.stream[eng].append(("wd", t[1], t[2]))

    @staticmethod
    def _deps(R, W):
        toks = []
        for r in R:
            toks.append(r.w)
        for r in W:
            toks.append(r.w)
            toks.extend(r.rs)
        return toks

    def op(self, eng, fn, R=(), W=()):
        self._need(eng, self._deps(R, W))
        self.seq[eng] += 1
        s = self.seq[eng]
        tok = ("e", eng, s)
        self.stream[eng].append(("op", _freeze(fn), s))
        for r in R:
            r.rs.append(tok)
        for r in W:
            r.w = tok
            r.rs = []
        return tok

    def dma(self, q, fn, R=(), W=(), Wn=()):
        self._need(q, self._deps(R, W))
        sr = W[0] if W else Wn[0]
        if sr.dsem is None:
            sr.dsem = self.stack.enter_context(self.nc.semaphore("ds%d" % self.nd))
            self.nd += 1
        sr.dcount += 16
        tok = ("d", sr.dsem, sr.dcount)
        self.stream[q].append(("dma", _freeze(fn), sr.dsem))
        for r in R:
            r.rs.append(tok)
        for r in W:
            r.w = tok
            r.rs = []
        for r in Wn:
            r.w = tok
        return tok

    def wait_all(self, eng, ress):
        best = {}
        for r in ress:
            for t in [r.w] + list(r.rs):
                if t is None:
                    continue
                if t[0] == "e":
                    if t[1] == "pe" and eng == "pe":
                        continue
                    key = ("e", t[1])
                else:
                    key = ("d", id(t[1]))
                if key not in best or best[key][2] < t[2]:
                    best[key] = t
        toks = [t for t in best.values()]
        for i in range(0, len(toks), 3):
            self._need(eng, toks[i:i + 3])
            if i + 3 < len(toks):
                self.seq[eng] += 1
                self.stream[eng].append(("op", (lambda e: e.nop()), self.seq[eng]))

    def barrier(self):
        self.wait_all("sp", self.allres)
        tok = self.op("sp", lambda e: e.nop())
        for e in self.ENG:
            if e != "sp":
                self._need(e, [tok])

    def emit(self):
        nc = self.nc
        val = {}
        for e in self.ENG:
            c = 0
            m = {}
            for it in self.stream[e]:
                if it[0] == "op" and (e, it[2]) in self.used:
                    c += 1
                    m[it[2]] = c
            val[e] = m
        engobj = {"pe": "tensor", "dve": "vector", "act": "scalar", "pool": "gpsimd", "sp": "sync"}
        esem = self.esem
        used = self.used
        with nc.Block() as block:
            for e in self.ENG:
                items = self.stream[e]

                def body(eng, items=items, e=e):
                    for it in items:
                        k = it[0]
                        if k == "we":
                            eng.wait_ge(esem[it[1]], val[it[1]][it[2]])
                        elif k == "wd":
                            eng.wait_ge(it[1], it[2])
                        elif k == "op":
                            ins = it[1](eng)
                            if (e, it[2]) in used:
                                ins.then_inc(esem[e], 1)
                        else:
                            it[1](eng).then_inc(it[2], 16)
                getattr(block, engobj[e])(body)


class TPool:
    def __init__(self, P, alloc, name, shape, dt, n):
        self.tiles = [alloc("%s_%d" % (name, i), shape, dt) for i in range(n)]
        self.ress = [P.res("%s_%d" % (name, i)) for i in range(n)]
        self.i = 0

    def next(self):
        k = self.i % len(self.tiles)
        self.i += 1
        return self.tiles[k], self.ress[k]


def build_program():
    nc = bass.Bass("TRN2", target_bir_lowering=False)

    def din(name, shape, dt=F32):
        return nc.dram_tensor(name, list(shape), dt, kind="ExternalInput").ap()

    x_own = din("x_own", [NT * 128, 1024])
    x_pre = din("x_pre", [NPRE * 128, 1024])
    rope = din("rope", [NPRE + NT, 128, 64])
    pmask = din("pmask", [128, NPRE])
    hflag = din("hflag", [128, 1])
    ln_g = [din("ln_g%d" % i, [1, 1024]) for i in range(3)]
    ln_b = [din("ln_b%d" % i, [1, 1024]) for i in range(3)]
    w_in = din("w_in", [1024, 2304])
    w_out = din("w_out", [1024, 1024])
    w_router = din("w_router", [1024, 256])
    rbias = din("rbias", [1, 256])
    w_gate = din("w_gate", [max(E_RUN, 1), 1024, 256])
    w_up = din("w_up", [max(E_RUN, 1), 1024, 256])
    w_down = din("w_down", [max(E_RUN, 1), 256, 1024])
    ws_gate = din("ws_gate", [1024, 256])
    ws_up = din("ws_up", [1024, 256])
    ws_down = din("ws_down", [256, 1024])
    sinks = din("sinks", [1, 8])
    c_ident = din("c_ident", [128, 128])
    c_decay = din("c_decay", [128, 512])
    c_zeta = din("c_zeta", [128, 4])
    c_pz = din("c_pz", [128, NPRE * 4])
    c_xi = din("c_xi", [128, 256])
    c_cd = din("c_cd", [128, 512])
    c_rb = din("c_rb", [128, 2048])
    c_mask = din("c_mask", [128, 2048])
    c_ltri = din("c_ltri", [128, 128])
    c_iota = din("c_iota", [128, 256])
    c_tok = din("c_tok", [128, NT * 2], I32)

    out = nc.dram_tensor("out", [NT * 128, 1024], F32, kind="ExternalOutput").ap()
    h2_dram = nc.dram_tensor("h2_dram", [NT * 128, 1024], F32, kind="Internal").ap()
    ys_dram = nc.dram_tensor("ys_dram", [(E + NT) * CAP, 1024], F32, kind="Internal").ap()
    list_dram = nc.dram_tensor("list_dram", [E * CAP, 2], I32, kind="Internal").ap()
    if DEBUG:
        dbg_h2 = nc.dram_tensor("dbg_h2", [NT * 128, 1024], F32, kind="ExternalOutput").ap()
        dbg_dest = nc.dram_tensor("dbg_dest", [128, NT * 8], I32, kind="ExternalOutput").ap()
        dbg_wk = nc.dram_tensor("dbg_wk", [128, NT * 8], F32, kind="ExternalOutput").ap()

    with ExitStack() as st0:
        P = Prog(nc, st0)

        cur = [16481]
        npad = [0]
        stack_marks = []

        def mk_alloc(st):
            base_at_entry = cur[0]
            st.callback(lambda: cur.__setitem__(0, base_at_entry))

            def sb(name, shape, dt=F32):
                a32 = (cur[0] + 31) // 32 * 32
                if a32 % SBUF_ALIGN:
                    npad[0] += 1
                    st.enter_context(nc.sbuf_tensor("pad%d" % npad[0], [128, 1], mybir.dt.uint8))
                    a32 += 32
                nbytes = int(np.prod(shape[1:])) * mybir.dt.size(dt)
                cur[0] = a32 + nbytes
                return st.enter_context(nc.sbuf_tensor(name, list(shape), dt))
            return sb

        sb0 = mk_alloc(st0)
        banks = [st0.enter_context(nc.psum_tensor("bank%d" % i, [128, 512], F32)) for i in range(8)]
        bres = [P.res("bank%d" % i) for i in range(8)]
        bctr = [0]

        nbank = [7]

        def bank():
            k = bctr[0] % nbank[0]
            bctr[0] += 1
            return banks[k], bres[k]

        sbank, r_sbank = banks[7], bres[7]

        ident = sb0("ident", [128, 128]); r_ident = P.res("ident")
        destI = sb0("destI", [128, NT * 8], I32); r_destI = P.res("destI")
        wk_all = sb0("wk_all", [128, NT * 8]); r_wk = P.res("wk_all")
        r_h2d = P.res("h2_dram"); r_ys = P.res("ys_dram"); r_list = P.res("list_dram"); r_out = P.res("out")
        r_dbg = P.res("dbg")
        P.dma("sp", lambda e: e.dma_start(out=ident[:], in_=c_ident), W=[r_ident])

        mhalf = sb0("mhalf", [128, 1]); r_mhalf = P.res("mhalf")
        P.op("pool", lambda e: e.memset(mhalf[:], -0.5), W=[r_mhalf])

        def ln_stats(sbp, src, r_src, eps, width=1024, rs_eng="act"):
            stats, r_st = sbp["stats"].next()
            mv, r_mv = sbp["mv"].next()
            rstd, r_rstd = sbp["rstd"].next()
            nchunk = width // 512
            for c in range(nchunk):
                P.op("dve", lambda e, c=c: e.bn_stats(out=stats[:, c, :], in_=src[:, c * 512:(c + 1) * 512]),
                     R=[r_src], W=[r_st])
            P.op("dve", lambda e: e.bn_aggr(out=mv[:], in_=stats[:, 0:nchunk, :].rearrange("p a b -> p (a b)")),
                 R=[r_st], W=[r_mv])
            if rs_eng == "pool":
                P.op("pool", lambda e: e.tensor_scalar(out=rstd[:], in0=mv[:, 1:2], scalar1=eps, scalar2=None, op0=ALU.add),
                     R=[r_mv], W=[r_rstd])
                P.op("pool", lambda e: e.tensor_tensor(out=rstd[:], in0=rstd[:], in1=mhalf[:], op=ALU.pow),
                     R=[r_rstd, r_mhalf], W=[r_rstd])
                return mv, r_mv, rstd, r_rstd
            P.op("act", lambda e: e.activation(out=rstd[:], in_=mv[:, 1:2], func=AF.Sqrt, bias=eps, scale=1.0),
                 R=[r_mv], W=[r_rstd])
            P.op("dve", lambda e: e.reciprocal(out=rstd[:], in_=rstd[:]), R=[r_rstd], W=[r_rstd])
            return mv, r_mv, rstd, r_rstd

        def layer_norm(sbp, src, r_src, dst, r_dst, g_t, r_g, b_t, r_b, g_eng="pool", rs_eng="act"):
            mv, r_mv, rstd, r_rstd = ln_stats(sbp, src, r_src, 1e-5, rs_eng=rs_eng)
            P.op("dve", lambda e: e.tensor_scalar(out=dst[:], in0=src[:], scalar1=mv[:, 0:1], scalar2=rstd[:, 0:1],
                                                  op0=ALU.subtract, op1=ALU.mult),
                 R=[r_src, r_mv, r_rstd], W=[r_dst])
            P.op(g_eng, lambda e: e.tensor_tensor(out=dst[:], in0=dst[:], in1=g_t[:], op=ALU.mult),
                 R=[r_dst, r_g], W=[r_dst])
            P.op("pool", lambda e: e.tensor_tensor(out=dst[:], in0=dst[:], in1=b_t[:], op=ALU.add),
                 R=[r_dst, r_b], W=[r_dst])

        def transpose_to(src_fn, nchunks, r_src, dstT, r_dstT, rows=128, evac="mix"):
            for b0 in range(0, nchunks, 4):
                bk, r_bk = bank()
                nb = min(4, nchunks - b0)
                for c in range(b0, b0 + nb):
                    P.op("pe", lambda e, c=c, bk=bk, b0=b0: e.transpose(
                        out=bk[0:rows, (c - b0) * 128:(c - b0 + 1) * 128], in_=src_fn(c), identity=ident[:]),
                        R=[r_src, r_ident], W=[r_bk])
                eng = "act" if ((b0 // 4) % 2 == 0 or evac == "act") else "dve"
                if eng == "act":
                    P.op("act", lambda e, bk=bk, b0=b0, nb=nb: e.copy(
                        out=dstT[0:rows, b0:b0 + nb, :].rearrange("p a b -> p (a b)"), in_=bk[0:rows, 0:nb * 128]),
                        R=[r_bk], W=[r_dstT])
                else:
                    P.op("dve", lambda e, bk=bk, b0=b0, nb=nb: e.tensor_copy(
                        out=dstT[0:rows, b0:b0 + nb, :].rearrange("p a b -> p (a b)"), in_=bk[0:rows, 0:nb * 128]),
                        R=[r_bk], W=[r_dstT])

        with ExitStack() as stA:
            sb = mk_alloc(stA)
            w_in_t = sb("w_in_t", [128, 8, 2304], F32R); r_w_in = P.res("w_in")
            w_out_t = sb("w_out_t", [128, 8, 1024], F32R); r_w_out = P.res("w_out")
            w_r_t = sb("w_r_t", [128, 8, 256], F32R); r_w_r = P.res("w_r")
            lng = [sb("lng%d" % i, [128, 1024]) for i in range(2)]
            lnb = [sb("lnb%d" % i, [128, 1024]) for i in range(2)]
            r_lng = [P.res("lng%d" % i) for i in range(2)]
            r_lnb = [P.res("lnb%d" % i) for i in range(2)]
            decay_t = sb("decay_t", [128, 512]); r_decay = P.res("decay")
            zeta_t = sb("zeta_t", [128, 4]); r_zeta = P.res("zeta")
            pz_t = sb("pz_t", [128, NPRE * 4]); r_pz = P.res("pz")
            xi_t = sb("xi_t", [128, 256]); r_xi = P.res("xi")
            btab = sb("btab", [128, 2048]); r_btab = P.res("btab")
            ltri = sb("ltri", [128, 128]); r_ltri = P.res("ltri")
            ones_t = sb("ones_t", [128, 128]); r_ones = P.res("ones")
            iota_t = sb("iota_t", [128, 256]); r_iota = P.res("iota")
            rb_t = sb("rb_t", [128, 256]); r_rb = P.res("rbias")
            tok_t = sb("tok_t", [128, NT * 2], I32); r_tok = P.res("tok")
            pm_t = sb("pm_t", [128, NPRE]); r_pm = P.res("pm")
            hf_t = sb("hf_t", [128, 1]); r_hf = P.res("hf")
            sexp = sb("sexp", [128, 8]); r_sexp = P.res("sexp")
            state = sb("state", [128, 512]); r_state = P.res("state")
            selsum = sb("selsum", [128, 256]); r_selsum = P.res("selsum")

            sbp = {
                "stats": TPool(P, sb, "stats", [128, 4, 6], F32, 2),
                "mv": TPool(P, sb, "mv", [128, 2], F32, 2),
                "rstd": TPool(P, sb, "rstd", [128, 1], F32, 2),
            }
            p_xt = TPool(P, sb, "xt", [128, 1024], F32, 2)
            p_rp = TPool(P, sb, "rp", [128, 64], F32, 2)
            hN, r_hN = sb("hN", [128, 1024]), P.res("hN")
            hT, r_hT = sb("hT", [128, 8, 128], F32R), P.res("hT")
            qk_raw, r_qk_raw = sb("qk_raw", [128, 512]), P.res("qk_raw")
            qk_rot, r_qk_rot = sb("qk_rot", [128, 512]), P.res("qk_rot")
            rt = [sb("rt%d" % i, [128, 256]) for i in range(2)]
            r_rt = [P.res("rt%d" % i) for i in range(2)]
            v_sb, r_v = sb("v_sb", [128, 512]), P.res("v_sb")
            sg, r_sg = sb("sg", [128, 512]), P.res("sg")
            qs_pad, r_qs = sb("qs_pad", [128, 8, 128]), P.res("qs_pad")
            kv_sb, r_ks = sb("kv_sb", [128, 256]), P.res("kv_sb")
            ks_sb = kv_sb[:, 0:128]
            if VARIANT >= 100:
                spacer = sb("spacer", [128, 64])
            p_v1 = TPool(P, sb, "v1", [128, 2, 128], F32, 2)
            qT, r_qT = sb("qT", [128, 2, 128]), P.res("qT")
            krm, r_krm = sb("krm", [128, 4, 128]), P.res("krm")
            kTm, r_kTm = sb("kTm", [128, 4, 128]), P.res("kTm")
            qxT, r_qxT = sb("qxT", [128, 256]), P.res("qxT")
            scT, r_scT = qk_raw, r_qk_raw
            kz, r_kz = sb("kz", [128, 4, 128]), P.res("kz")
            gmv, r_gmv = sb("gmv", [128, 4, 2]), P.res("gmv")
            grstd, r_grstd = sb("grstd", [128, 4]), P.res("grstd")
            concat, r_cc = sb("concat", [128, 1024]), P.res("concat")
            qsT, r_qsT = sb("qsT", [128, 8, 128]), P.res("qsT")
            p_ksT = TPool(P, sb, "ksT", [128, 128], F32, 2)
            p_psb = TPool(P, sb, "psb", [128, 512], F32, 2)
            den, r_den = sb("den", [128, 8]), P.res("den")
            h2, r_h2 = hN, r_hN
            sc, r_sc = sb("sc", [128, 256]), P.res("sc")
            choice, r_choice = sb("choice", [128, 256]), P.res("choice")
            g8, r_g8 = sb("g8", [128, 8, 8]), P.res("g8")
            gs, r_gs = sb("gs", [128, 8]), P.res("gs")
            s8, r_s8 = sb("s8", [128, 8]), P.res("s8")
            pen, r_pen = sb("pen", [128, 8]), P.res("pen")
            m8, r_m8 = sb("m8", [128, 8]), P.res("m8")
            i8, r_i8 = sb("i8", [128, 8], U32), P.res("i8")
            ekf, r_ekf = sb("ekf", [128, 8]), P.res("ekf")
            sel, r_sel = sb("sel", [128, 256]), P.res("sel")
            wn, r_wn = sc, r_sc
            dsum, r_dsum = sb("dsum", [128, 1]), P.res("dsum")
            posd, r_posd = sel, r_sel
            junk, r_junk = choice, r_choice
            destf, r_destf = sb("destf", [128, 8]), P.res("destf")
            lfill, r_lfill = qk_raw[:].bitcast(I32), r_qk_raw

            for c8 in range(8):
                P.dma("pool", lambda e, c8=c8: e.dma_start(out=w_in_t[:, c8, :], in_=w_in[c8 * 128:(c8 + 1) * 128, :], max_dma_last_dim=4096), W=[r_w_in])
            P.dma("pool", lambda e: e.dma_start(out=w_out_t[:], in_=w_out.rearrange("(c p) n -> p c n", p=128)), W=[r_w_out])
            P.dma("pool", lambda e: e.dma_start(out=w_r_t[:], in_=w_router.rearrange("(c p) n -> p c n", p=128)), W=[r_w_r])
            for i in range(2):
                P.dma("sp", lambda e, i=i: e.dma_start(out=lng[i][:], in_=ln_g[i].partition_broadcast(128)), W=[r_lng[i]])
                P.dma("sp", lambda e, i=i: e.dma_start(out=lnb[i][:], in_=ln_b[i].partition_broadcast(128)), W=[r_lnb[i]])
            for (t_, r_, src) in ((decay_t, r_decay, c_decay), (zeta_t, r_zeta, c_zeta), (pz_t, r_pz, c_pz), (xi_t, r_xi, c_xi),
                                  (btab, r_btab, c_rb), (ltri, r_ltri, c_ltri),
                                  (iota_t, r_iota, c_iota), (tok_t, r_tok, c_tok),
                                  (pm_t, r_pm, pmask), (hf_t, r_hf, hflag)):
                P.dma("act", lambda e, t_=t_, src=src: e.dma_start(out=t_[:], in_=src), W=[r_])
            P.dma("act", lambda e: e.dma_start(out=rb_t[:], in_=rbias.partition_broadcast(128)), W=[r_rb])
            P.dma("act", lambda e: e.dma_start(out=sexp[:], in_=sinks.partition_broadcast(128)), W=[r_sexp])
            P.op("act", lambda e: e.activation(out=sexp[:], in_=sexp[:], func=AF.Exp), R=[r_sexp], W=[r_sexp])
            for q4 in range(2):
                mt, r_mt = p_xt.next()
                P.dma("sp", lambda e, mt=mt, q4=q4: e.dma_start(out=mt[:], in_=c_mask[:, q4 * 1024:(q4 + 1) * 1024]), W=[r_mt])
                P.op("pool", lambda e, mt=mt, q4=q4: e.tensor_tensor(out=btab[:, q4 * 1024:(q4 + 1) * 1024],
                                                                     in0=btab[:, q4 * 1024:(q4 + 1) * 1024], in1=mt[:], op=ALU.add),
                     R=[r_mt, r_btab], W=[r_btab])
            P.op("pool", lambda e: e.memset(ones_t[:], 1.0), W=[r_ones])
            P.op("pool", lambda e: e.memset(state[:], 0.0), W=[r_state])
            P.op("pool", lambda e: e.memset(selsum[:], 0.0), W=[r_selsum])
            P.op("pool", lambda e: e.memset(krm[:], 0.0), W=[r_krm])
            P.op("pool", lambda e: e.memset(kz[:], 0.0), W=[r_kz])
            P.op("pool", lambda e: e.memset(qs_pad[:], 0.0), W=[r_qs])
            for i in range(2):
                P.op("pool", lambda e, i=i: e.memset(p_v1.tiles[i][:], 0.0), W=[p_v1.ress[i]])
                P.op("pool", lambda e, i=i: e.memset(p_v1.tiles[i][:, :, 64:65], 1.0), W=[p_v1.ress[i]])
            P.op("pool", lambda e: e.memset(lfill, NT * 128), W=[r_lfill])
            P.dma("pool", lambda e: e.dma_start(out=list_dram.rearrange("(p r) c -> p (r c)", p=128), in_=lfill),
                  R=[r_lfill], W=[r_list])
            P.wait_all("pool", [r_list])

            prev_ksT = [None, None]
            prev_v1 = [None, None]

            def mixer_tile(gt, is_own, i_own, pending=None):
                xsrc = x_own if is_own else x_pre
                ti = i_own if is_own else gt
                xt, r_xt = p_xt.next()
                rp, r_rp = p_rp.next()
                P.dma("sp", lambda e: e.dma_start(out=xt[:], in_=xsrc[ti * 128:(ti + 1) * 128, :]), W=[r_xt])
                P.dma("act", lambda e: e.dma_start(out=rp[:], in_=rope[gt]), W=[r_rp])
                layer_norm(sbp, xt, r_xt, hN, r_hN, lng[0], r_lng[0], lnb[0], r_lnb[0])
                transpose_to(lambda c: hN[:, c * 128:(c + 1) * 128], 8, r_hN, hT, r_hT)
                if STAGE < 2:
                    return
                need_swa_kv = is_own or gt == NPRE - 1

                def proj(col0, ncols):
                    bk, r_bk = bank()
                    for c in range(8):
                        P.op("pe", lambda e, c=c: e.matmul(out=bk[:, 0:ncols], lhsT=hT[:, c, :],
                                                           rhs=w_in_t[:, c, col0:col0 + ncols], start=(c == 0), stop=(c == 7)),
                             R=[r_hT, r_w_in], W=[r_bk])
                    return bk, r_bk

                if is_own:
                    bk, r_bk = proj(0, 512)
                    P.op("act", lambda e: e.copy(out=qk_raw[:], in_=bk[:]), R=[r_bk], W=[r_qk_raw])
                    lo, nh = 0, 8
                else:
                    bk, r_bk = proj(256, 256)
                    P.op("act", lambda e: e.copy(out=qk_raw[:, 256:512], in_=bk[:, 0:256]), R=[r_bk], W=[r_qk_raw])
                    lo, nh = 256, 4
                src4 = qk_raw[:, lo:512].rearrange("p (h t d) -> p h t d", t=2, d=32)
                dst4 = qk_rot[:, lo:512].rearrange("p (h t d) -> p h t d", t=2, d=32)
                cosb = rp[:, 0:32].unsqueeze(1).to_broadcast([128, nh, 32])
                sinb = rp[:, 32:64].unsqueeze(1).to_broadcast([128, nh, 32])
                ta = rt[0][:, 0:nh * 32].rearrange("p (h d) -> p h d", d=32)
                tb = rt[1][:, 0:nh * 32].rearrange("p (h d) -> p h d", d=32)
                P.op("pool", lambda e: e.tensor_tensor(out=ta, in0=src4[:, :, 0, :], in1=cosb, op=ALU.mult), R=[r_qk_raw, r_rp], W=[r_rt[0]])
                P.op("dve", lambda e: e.tensor_tensor(out=tb, in0=src4[:, :, 1, :], in1=sinb, op=ALU.mult), R=[r_qk_raw, r_rp], W=[r_rt[1]])
                P.op("dve", lambda e: e.tensor_tensor(out=dst4[:, :, 0, :], in0=ta, in1=tb, op=ALU.subtract), R=[r_rt[0], r_rt[1]], W=[r_qk_rot])
                P.op("pool", lambda e: e.tensor_tensor(out=ta, in0=src4[:, :, 0, :], in1=sinb, op=ALU.mult), R=[r_qk_raw, r_rp], W=[r_rt[0]])
                P.op("dve", lambda e: e.tensor_tensor(out=tb, in0=src4[:, :, 1, :], in1=cosb, op=ALU.mult), R=[r_qk_raw, r_rp], W=[r_rt[1]])
                P.op("dve", lambda e: e.tensor_tensor(out=dst4[:, :, 1, :], in0=ta, in1=tb, op=ALU.add), R=[r_rt[0], r_rt[1]], W=[r_qk_rot])
                bk, r_bk = proj(512, 512)
                P.op("act", lambda e: e.copy(out=v_sb[:], in_=bk[:]), R=[r_bk], W=[r_v])
                for h in range(4):
                    if is_own:
                        P.op("pool", lambda e, h=h: e.tensor_scalar(out=kz[:, h, (h % 2) * 64:(h % 2 + 1) * 64], in0=qk_rot[:, 256 + h * 64:256 + (h + 1) * 64],
                                                                    scalar1=zeta_t[:, h:h + 1], scalar2=None, op0=ALU.mult),
                             R=[r_qk_rot, r_zeta], W=[r_kz])
                    else:
                        P.op("pool", lambda e, h=h: e.tensor_scalar(out=kz[:, h, (h % 2) * 64:(h % 2 + 1) * 64], in0=qk_rot[:, 256 + h * 64:256 + (h + 1) * 64],
                                                                    scalar1=zeta_t[:, h:h + 1], scalar2=pm_t[:, gt:gt + 1], op0=ALU.mult, op1=ALU.mult),
                             R=[r_qk_rot, r_zeta, r_pm], W=[r_kz])
                if STAGE < 2.1:
                    return
                if is_own:
                    bk, r_bk = proj(1024, 512)
                    P.op("act", lambda e: e.activation(out=sg[:], in_=bk[:], func=AF.Silu), R=[r_bk], W=[r_sg])
                    if STAGE < 2.12:
                        return
                    bk, r_bk = proj(1536, 512)
                    for kv in range(2):
                        P.op("act", lambda e, kv=kv: e.copy(out=qs_pad[:, kv * 4:(kv + 1) * 4, kv * 64:(kv + 1) * 64],
                                                            in_=bk[:, kv * 256:(kv + 1) * 256].rearrange("p (g d) -> p g d", d=64)),
                             R=[r_bk], W=[r_qs])
                if STAGE < 2.13:
                    return
                if need_swa_kv and STAGE >= 2.2:
                    bk, r_bk = proj(2048, 256)
                    v1, r_v1 = p_v1.next()
                    P.op("act", lambda e: e.copy(out=kv_sb[:], in_=bk[:, 0:256]), R=[r_bk], W=[r_ks])
                    P.op("pool", lambda e: e.tensor_copy(out=v1[:, :, 0:64], in_=kv_sb[:, 128:256].rearrange("p (k d) -> p k d", d=64)),
                         R=[r_ks], W=[r_v1])
                    ksT, r_ksT = p_ksT.next()
                    bk, r_bk = bank()
                    if VARIANT == 1:
                        bk, r_bk = bank()
                    if VARIANT != 2:
                        P.op("pe", lambda e: e.transpose(out=bk[:, 0:128], in_=ks_sb, identity=ident[:]), R=[r_ks, r_ident], W=[r_bk])
                    if VARIANT == 3:
                        P.op("dve", lambda e: e.tensor_copy(out=ksT[:], in_=bk[:, 0:128]), R=[r_bk], W=[r_ksT])
                    elif VARIANT != 4:
                        P.op("act", lambda e: e.copy(out=ksT[:], in_=bk[:, 0:128]), R=[r_bk], W=[r_ksT])
                if pending is not None:
                    pending()
                if is_own and STAGE >= 2.3:
                    for hp in range(2):
                        P.op("pool", lambda e, hp=hp: e.tensor_copy(
                            out=krm[:].rearrange("p (a b) c -> p a b c", b=2)[:, :, hp, hp * 64:(hp + 1) * 64],
                            in_=qk_rot[:, 256:512].rearrange("p (a b d) -> p a b d", b=2, d=64)[:, :, hp, :]),
                            R=[r_qk_rot], W=[r_krm])
                    transpose_to(lambda c: qk_rot[:, c * 128:(c + 1) * 128], 2, r_qk_rot, qT, r_qT)
                    transpose_to(lambda c: krm[:, c, :], 4, r_krm, kTm, r_kTm)
                    P.op("pool", lambda e: e.tensor_tensor(out=qxT[:], in0=qT[:].rearrange("p a b -> p (a b)"), in1=xi_t[:], op=ALU.mult),
                         R=[r_qT, r_xi], W=[r_qxT])
                    bk, r_bk = bank()
                    for h in range(4):
                        P.op("pe", lambda e, h=h: e.matmul(out=bk[:, h * 128:(h + 1) * 128], lhsT=kTm[:, h, :],
                                                           rhs=qT[:, h // 2, :], start=True, stop=True),
                             R=[r_kTm, r_qT], W=[r_bk])
                    P.op("dve", lambda e: e.tensor_tensor(out=scT[:], in0=bk[:], in1=decay_t[:], op=ALU.mult), R=[r_bk, r_decay], W=[r_scT])
                    rbk, r_rbk = bank()
                    for h in range(4):
                        P.op("pe", lambda e, h=h: e.matmul(out=rbk[:, h * 128:(h + 1) * 128], lhsT=scT[:, h * 128:(h + 1) * 128],
                                                           rhs=v_sb[:, h * 128:(h + 1) * 128], start=True, stop=False),
                             R=[r_scT, r_v], W=[r_rbk])
                        P.op("pe", lambda e, h=h: e.matmul(out=rbk[:, h * 128:(h + 1) * 128], lhsT=qxT[:, (h // 2) * 128:(h // 2 + 1) * 128],
                                                           rhs=state[:, h * 128:(h + 1) * 128], start=False, stop=True),
                             R=[r_qxT, r_state], W=[r_rbk])
                if STAGE < 2.4:
                    return
                kbk, r_kbk = bank()
                for h in range(4):
                    P.op("pe", lambda e, h=h: e.matmul(out=kbk[:, h * 128:(h + 1) * 128], lhsT=kz[:, h, :],
                                                       rhs=v_sb[:, h * 128:(h + 1) * 128], start=True, stop=True),
                         R=[r_kz, r_v], W=[r_kbk])
                for h in range(4):
                    P.op("pool", lambda e, h=h: e.tensor_scalar(out=state[:, h * 128:(h + 1) * 128], in0=state[:, h * 128:(h + 1) * 128],
                                                                scalar1=CHUNK_DECAY[h], scalar2=None, op0=ALU.mult), R=[r_state], W=[r_state])
                P.op("dve", lambda e: e.tensor_tensor(out=state[:], in0=kbk[:], in1=state[:], op=ALU.add), R=[r_kbk, r_state], W=[r_state])
                if not is_own:
                    if need_swa_kv:
                        prev_ksT[0], prev_ksT[1] = ksT, r_ksT
                        prev_v1[0], prev_v1[1] = v1, r_v1
                    return
                if STAGE < 3:
                    return
                stats, r_st = sbp["stats"].next()
                for h in range(4):
                    P.op("dve", lambda e, h=h: e.bn_stats(out=stats[:, h, :], in_=rbk[:, h * 128:(h + 1) * 128]), R=[r_rbk], W=[r_st])
                for h in range(4):
                    P.op("dve", lambda e, h=h: e.bn_aggr(out=gmv[:, h, :], in_=stats[:, h, :]), R=[r_st], W=[r_gmv])
                P.op("act", lambda e: e.activation(out=grstd[:], in_=gmv[:, :, 1], func=AF.Sqrt, bias=1e-6, scale=1.0), R=[r_gmv], W=[r_grstd])
                P.op("dve", lambda e: e.reciprocal(out=grstd[:], in_=grstd[:]), R=[r_grstd], W=[r_grstd])
                for h in range(4):
                    P.op("dve", lambda e, h=h: e.tensor_scalar(out=concat[:, h * 128:(h + 1) * 128], in0=rbk[:, h * 128:(h + 1) * 128],
                                                               scalar1=gmv[:, h, 0:1], scalar2=grstd[:, h:h + 1], op0=ALU.subtract, op1=ALU.mult),
                         R=[r_rbk, r_gmv, r_grstd], W=[r_cc])
                P.op("pool", lambda e: e.tensor_tensor(out=concat[:, 0:512], in0=concat[:, 0:512], in1=sg[:], op=ALU.mult), R=[r_cc, r_sg], W=[r_cc])
                if STAGE < 4:
                    return
                for b0 in range(2):
                    bk, r_bk = bank()
                    for hh in range(4):
                        hq = b0 * 4 + hh
                        P.op("pe", lambda e, hq=hq, hh=hh, bk=bk: e.transpose(out=bk[:, hh * 128:(hh + 1) * 128], in_=qs_pad[:, hq, :], identity=ident[:]),
                             R=[r_qs, r_ident], W=[r_bk])
                    if b0 == 0:
                        P.op("act", lambda e, bk=bk: e.copy(out=qsT[:, 0:4, :].rearrange("p a b -> p (a b)"), in_=bk[:]), R=[r_bk], W=[r_qsT])
                    else:
                        P.op("dve", lambda e, bk=bk: e.tensor_copy(out=qsT[:, 4:8, :].rearrange("p a b -> p (a b)"), in_=bk[:]), R=[r_bk], W=[r_qsT])
                obk = [bank(), bank()]
                for kv in range(2):
                    psbs = []
                    for half in range(2):
                        kT_, r_kT_ = (prev_ksT[0], prev_ksT[1]) if half == 0 else (ksT, r_ksT)
                        bk, r_bk = bank()
                        P.op("pe", lambda e, kv=kv, kT_=kT_, bk=bk: e.matmul(out=bk[:], lhsT=kT_[:],
                                                                           rhs=qsT[:, kv * 4:(kv + 1) * 4, :].rearrange("p a b -> p (a b)"), start=True, stop=True),
                             R=[r_kT_, r_qsT], W=[r_bk])
                        psb, r_psb = p_psb.next()
                        o0 = (kv * 2 + half) * 512
                        P.op("dve", lambda e, bk=bk, psb=psb, o0=o0: e.scalar_tensor_tensor(out=psb[:], in0=bk[:], scalar=0.125, in1=btab[:, o0:o0 + 512],
                                                                                         op0=ALU.mult, op1=ALU.add),
                             R=[r_bk, r_btab], W=[r_psb])
                        P.op("act", lambda e, psb=psb: e.activation(out=psb[:], in_=psb[:], func=AF.Exp), R=[r_psb], W=[r_psb])
                        if half == 0 and i_own == 0:
                            P.op("pool", lambda e, psb=psb: e.tensor_scalar(out=psb[:], in0=psb[:], scalar1=hf_t[:, 0:1], scalar2=None, op0=ALU.mult),
                                 R=[r_psb, r_hf], W=[r_psb])
                        psbs.append((psb, r_psb))
                    ob, r_ob = obk[kv]
                    for g in range(4):
                        for half in range(2):
                            v1_, r_v1_ = (prev_v1[0], prev_v1[1]) if half == 0 else (v1, r_v1)
                            psb, r_psb = psbs[half]
                            P.op("pe", lambda e, g=g, kv=kv, half=half, v1_=v1_, psb=psb, ob=ob: e.matmul(
                                out=ob[:, g * 128:g * 128 + 66], lhsT=psb[:, g * 128:(g + 1) * 128], rhs=v1_[:, kv, 0:66],
                                start=(half == 0), stop=(half == 1)),
                                R=[r_psb, r_v1_], W=[r_ob])
                for kv in range(2):
                    ob, r_ob = obk[kv]
                    ob3 = ob[:].rearrange("p (g c) -> p g c", c=128)
                    P.op("dve", lambda e, kv=kv, ob3=ob3: e.tensor_tensor(out=den[:, kv * 4:(kv + 1) * 4], in0=ob3[:, :, 64], in1=sexp[:, kv * 4:(kv + 1) * 4], op=ALU.add),
                         R=[r_ob, r_sexp], W=[r_den])
                P.op("dve", lambda e: e.reciprocal(out=den[:], in_=den[:]), R=[r_den], W=[r_den])
                for kv in range(2):
                    ob, r_ob = obk[kv]
                    ob3 = ob[:].rearrange("p (g c) -> p g c", c=128)
                    P.op("dve", lambda e, kv=kv, ob3=ob3: e.tensor_tensor(
                        out=concat[:, 512 + kv * 256:512 + (kv + 1) * 256].rearrange("p (g d) -> p g d", d=64),
                        in0=ob3[:, :, 0:64], in1=den[:, kv * 4:(kv + 1) * 4].unsqueeze(2).to_broadcast([128, 4, 64]), op=ALU.mult),
                        R=[r_ob, r_den], W=[r_cc])
                prev_ksT[0], prev_ksT[1] = ksT, r_ksT
                prev_v1[0], prev_v1[1] = v1, r_v1
                if STAGE < 5:
                    return
                transpose_to(lambda c: concat[:, c * 128:(c + 1) * 128], 8, r_cc, hT, r_hT)
                mb = [bank(), bank()]
                for nb_ in range(2):
                    bk, r_bk = mb[nb_]
                    for c in range(8):
                        P.op("pe", lambda e, c=c, nb_=nb_, bk=bk: e.matmul(out=bk[:], lhsT=hT[:, c, :], rhs=w_out_t[:, c, nb_ * 512:(nb_ + 1) * 512],
                                                                         start=(c == 0), stop=(c == 7)),
                             R=[r_hT, r_w_out], W=[r_bk])
                for nb_ in range(2):
                    bk, r_bk = mb[nb_]
                    P.op("dve", lambda e, nb_=nb_, bk=bk: e.scalar_tensor_tensor(out=concat[:, nb_ * 512:(nb_ + 1) * 512], in0=hN[:, nb_ * 512:(nb_ + 1) * 512],
                                                                               scalar=ALPHA, in1=bk[:], op0=ALU.mult, op1=ALU.add),
                         R=[r_hN, r_bk], W=[r_cc])
                layer_norm(sbp, concat, r_cc, h2, r_h2, lng[1], r_lng[1], lnb[1], r_lnb[1])
                P.dma("sp", lambda e: e.dma_start(out=h2_dram[i_own * 128:(i_own + 1) * 128, :], in_=h2[:]), R=[r_h2], Wn=[r_h2d])
                if DEBUG:
                    P.dma("sp", lambda e: e.dma_start(out=dbg_h2[i_own * 128:(i_own + 1) * 128, :], in_=h2[:]), R=[r_h2], Wn=[r_dbg])
                if STAGE < 6:
                    return
                transpose_to(lambda c: h2[:, c * 128:(c + 1) * 128], 8, r_h2, hT, r_hT)
                lb, r_lb = bank()
                for c in range(8):
                    P.op("pe", lambda e, c=c: e.matmul(out=lb[:, 0:256], lhsT=hT[:, c, :], rhs=w_r_t[:, c, :], start=(c == 0), stop=(c == 7)),
                         R=[r_hT, r_w_r], W=[r_lb])
                P.op("act", lambda e: e.activation(out=sc[:], in_=lb[:, 0:256], func=AF.Sigmoid), R=[r_lb], W=[r_sc])
                P.op("pool", lambda e: e.tensor_tensor(out=choice[:], in0=sc[:], in1=rb_t[:], op=ALU.add), R=[r_sc, r_rb], W=[r_choice])
                def tail():
                    for g in range(8):
                        P.op("dve", lambda e, g=g: e.max(out=g8[:, g, :], in_=choice[:, g * 32:(g + 1) * 32]), R=[r_choice], W=[r_g8])
                    P.op("dve", lambda e: e.tensor_tensor(out=gs[:], in0=g8[:, :, 0], in1=g8[:, :, 1], op=ALU.add), R=[r_g8], W=[r_gs])
                    P.op("dve", lambda e: e.max(out=s8[:], in_=gs[:]), R=[r_gs], W=[r_s8])
                    P.op("dve", lambda e: e.tensor_scalar(out=pen[:], in0=gs[:], scalar1=s8[:, 3:4], scalar2=1e9, op0=ALU.is_ge, op1=ALU.mult), R=[r_gs, r_s8], W=[r_pen])
                    P.op("dve", lambda e: e.tensor_scalar(out=pen[:], in0=pen[:], scalar1=-1e9, scalar2=None, op0=ALU.add), R=[r_pen], W=[r_pen])
                    P.op("dve", lambda e: e.tensor_tensor(out=choice[:].rearrange("p (g j) -> p g j", j=32), in0=choice[:].rearrange("p (g j) -> p g j", j=32),
                                                          in1=pen[:].unsqueeze(2).to_broadcast([128, 8, 32]), op=ALU.add), R=[r_choice, r_pen], W=[r_choice])
                    P.op("dve", lambda e: e.max(out=m8[:], in_=choice[:]), R=[r_choice], W=[r_m8])
                    P.op("dve", lambda e: e.max_index(out=i8[:], in_max=m8[:], in_values=choice[:]), R=[r_choice, r_m8], W=[r_i8])
                    P.op("dve", lambda e: e.tensor_copy(out=ekf[:], in_=i8[:]), R=[r_i8], W=[r_ekf])
                    P.op("dve", lambda e: e.tensor_scalar(out=sel[:], in0=choice[:], scalar1=m8[:, 7:8], scalar2=None, op0=ALU.is_ge), R=[r_choice, r_m8], W=[r_sel])
                    P.op("dve", lambda e: e.tensor_tensor(out=wn[:], in0=sel[:], in1=sc[:], op=ALU.mult), R=[r_sel, r_sc], W=[r_wn])
                    P.op("dve", lambda e: e.reduce_sum(out=dsum[:], in_=wn[:], axis=mybir.AxisListType.X), R=[r_wn], W=[r_dsum])
                    P.op("dve", lambda e: e.reciprocal(out=dsum[:], in_=dsum[:]), R=[r_dsum], W=[r_dsum])
                    P.op("dve", lambda e: e.tensor_scalar(out=wn[:], in0=wn[:], scalar1=dsum[:, 0:1], scalar2=2.5, op0=ALU.mult, op1=ALU.mult), R=[r_wn, r_dsum], W=[r_wn])
                    pb, r_pb = bank()
                    P.op("pe", lambda e: e.matmul(out=pb[:, 0:256], lhsT=ltri[:], rhs=sel[:], start=True, stop=False), R=[r_ltri, r_sel], W=[r_pb])
                    P.op("pe", lambda e: e.matmul(out=pb[:, 0:256], lhsT=ones_t[:], rhs=selsum[:], start=False, stop=True), R=[r_ones, r_selsum], W=[r_pb])
                    P.op("pool", lambda e: e.tensor_tensor(out=selsum[:], in0=selsum[:], in1=sel[:], op=ALU.add), R=[r_selsum, r_sel], W=[r_selsum])
                    P.op("dve", lambda e: e.scalar_tensor_tensor(out=posd[:], in0=iota_t[:], scalar=float(CAP), in1=pb[:, 0:256], op0=ALU.mult, op1=ALU.add), R=[r_pb, r_iota], W=[r_posd])
                    for k in range(8):
                        P.op("dve", lambda e, k=k: e.scalar_tensor_tensor(out=junk[:], in0=iota_t[:], scalar=ekf[:, k:k + 1], in1=wn[:], op0=ALU.is_equal, op1=ALU.mult,
                                                                         accum_out=wk_all[:, i_own * 8 + k:i_own * 8 + k + 1]),
                             R=[r_iota, r_ekf, r_wn], W=[r_junk, r_wk])
                        P.op("dve", lambda e, k=k: e.scalar_tensor_tensor(out=junk[:], in0=iota_t[:], scalar=ekf[:, k:k + 1], in1=posd[:], op0=ALU.is_equal, op1=ALU.mult,
                                                                         accum_out=destf[:, k:k + 1]),
                             R=[r_iota, r_ekf, r_posd], W=[r_junk, r_destf])
                    P.op("dve", lambda e: e.tensor_copy(out=destI[:, i_own * 8:(i_own + 1) * 8], in_=destf[:]), R=[r_destf], W=[r_destI])
                    if STAGE < 7:
                        return
                    for k in range(8):
                        P.dma("pool", lambda e, k=k: e.indirect_dma_start(
                            out=list_dram, out_offset=bass.IndirectOffsetOnAxis(ap=destI[:, i_own * 8 + k:i_own * 8 + k + 1], axis=0),
                            in_=tok_t[:, i_own * 2:i_own * 2 + 2], in_offset=None, bounds_check=P.reg(e, E * CAP - 1), oob_is_err=False),
                            R=[r_destI, r_tok], Wn=[r_list])

                return tail

            def prefix_F(gt):
                xt, r_xt = p_xt.next()
                rp, r_rp = p_rp.next()
                P.dma("sp", lambda e: e.dma_start(out=xt[:], in_=x_pre[gt * 128:(gt + 1) * 128, :]), W=[r_xt])
                P.dma("act", lambda e: e.dma_start(out=rp[:], in_=rope[gt]), W=[r_rp])
                layer_norm(sbp, xt, r_xt, hN, r_hN, lng[0], r_lng[0], lnb[0], r_lnb[0], g_eng="dve", rs_eng="pool")
                transpose_to(lambda c: hN[:, c * 128:(c + 1) * 128], 8, r_hN, hT, r_hT, evac="act")
                if gt % 2 == 0:
                    kraw, r_kraw, vbuf, r_vbuf = qk_raw, r_qk_raw, v_sb, r_v
                else:
                    kraw, r_kraw, vbuf, r_vbuf = sg, r_sg, p_psb.tiles[0], p_psb.ress[0]

                def proj(col0, ncols):
                    bk, r_bk = bank()
                    for c in range(8):
                        P.op("pe", lambda e, c=c: e.matmul(out=bk[:, 0:ncols], lhsT=hT[:, c, :],
                                                           rhs=w_in_t[:, c, col0:col0 + ncols], start=(c == 0), stop=(c == 7)),
                             R=[r_hT, r_w_in], W=[r_bk])
                    return bk, r_bk

                bk, r_bk = proj(256, 256)
                P.op("act", lambda e: e.copy(out=kraw[:, 256:512], in_=bk[:, 0:256]), R=[r_bk], W=[r_kraw])
                bk, r_bk = proj(512, 512)
                P.op("act", lambda e: e.copy(out=vbuf[:], in_=bk[:]), R=[r_bk], W=[r_vbuf])
                if gt == NPRE - 1:
                    bk, r_bk = proj(2048, 256)
                    v1, r_v1 = p_v1.next()
                    P.op("act", lambda e: e.copy(out=kv_sb[:], in_=bk[:, 0:256]), R=[r_bk], W=[r_ks])
                    P.op("pool", lambda e: e.tensor_copy(out=v1[:, :, 0:64], in_=kv_sb[:, 128:256].rearrange("p (k d) -> p k d", d=64)),
                         R=[r_ks], W=[r_v1])
                    ksT, r_ksT = p_ksT.next()
                    bk, r_bk = bank()
                    P.op("pe", lambda e: e.transpose(out=bk[:, 0:128], in_=ks_sb, identity=ident[:]), R=[r_ks, r_ident], W=[r_bk])
                    P.op("act", lambda e: e.copy(out=ksT[:], in_=bk[:, 0:128]), R=[r_bk], W=[r_ksT])
                    prev_ksT[0], prev_ksT[1] = ksT, r_ksT
                    prev_v1[0], prev_v1[1] = v1, r_v1
                return (gt, rp, r_rp, kraw, r_kraw, vbuf, r_vbuf)

            def prefix_G(ctx):
                gt, rp, r_rp, kraw, r_kraw, vbuf, r_vbuf = ctx
                src4 = kraw[:, 256:512].rearrange("p (h t d) -> p h t d", t=2, d=32)
                dst4 = qk_rot[:, 256:512].rearrange("p (h t d) -> p h t d", t=2, d=32)
                cosb = rp[:, 0:32].unsqueeze(1).to_broadcast([128, 4, 32])
                sinb = rp[:, 32:64].unsqueeze(1).to_broadcast([128, 4, 32])
                ta = rt[0][:, 0:128].rearrange("p (h d) -> p h d", d=32)
                tb = rt[1][:, 0:128].rearrange("p (h d) -> p h d", d=32)
                P.op("dve", lambda e: e.tensor_tensor(out=ta, in0=src4[:, :, 0, :], in1=cosb, op=ALU.mult), R=[r_kraw, r_rp], W=[r_rt[0]])
                P.op("dve", lambda e: e.tensor_tensor(out=tb, in0=src4[:, :, 1, :], in1=sinb, op=ALU.mult), R=[r_kraw, r_rp], W=[r_rt[1]])
                P.op("dve", lambda e: e.tensor_tensor(out=dst4[:, :, 0, :], in0=ta, in1=tb, op=ALU.subtract), R=[r_rt[0], r_rt[1]], W=[r_qk_rot])
                P.op("dve", lambda e: e.tensor_tensor(out=ta, in0=src4[:, :, 0, :], in1=sinb, op=ALU.mult), R=[r_kraw, r_rp], W=[r_rt[0]])
                P.op("dve", lambda e: e.tensor_tensor(out=tb, in0=src4[:, :, 1, :], in1=cosb, op=ALU.mult), R=[r_kraw, r_rp], W=[r_rt[1]])
                P.op("dve", lambda e: e.tensor_tensor(out=dst4[:, :, 1, :], in0=ta, in1=tb, op=ALU.add), R=[r_rt[0], r_rt[1]], W=[r_qk_rot])
                for h in range(4):
                    P.op("pool", lambda e, h=h: e.tensor_scalar(out=kz[:, h, (h % 2) * 64:(h % 2 + 1) * 64], in0=qk_rot[:, 256 + h * 64:256 + (h + 1) * 64],
                                                                scalar1=pz_t[:, gt * 4 + h:gt * 4 + h + 1], scalar2=pm_t[:, gt:gt + 1], op0=ALU.mult, op1=ALU.mult),
                         R=[r_qk_rot, r_pz, r_pm], W=[r_kz])
                for h in range(4):
                    P.op("pe", lambda e, h=h: e.matmul(out=sbank[:, h * 128:(h + 1) * 128], lhsT=kz[:, h, :],
                                                       rhs=vbuf[:, h * 128:(h + 1) * 128], start=(gt == 0 and h == 0), stop=(gt == NPRE - 1)),
                         R=[r_kz, r_vbuf], W=[r_sbank])

            ctx_prev = None
            for gt in range(NPRE):
                ctx = prefix_F(gt)
                if ctx_prev is not None:
                    prefix_G(ctx_prev)
                ctx_prev = ctx
            if ctx_prev is not None:
                prefix_G(ctx_prev)
                P.op("act", lambda e: e.copy(out=state[:], in_=sbank[:]), R=[r_sbank], W=[r_state])
            nbank[0] = 8
            pending = None
            for i in range(NT):
                pending = mixer_tile(NPRE + i, True, i, pending)
            if pending is not None:
                pending()
            if DEBUG:
                P.dma("sp", lambda e: e.dma_start(out=dbg_dest, in_=destI[:]), R=[r_destI], Wn=[r_dbg])
                P.dma("sp", lambda e: e.dma_start(out=dbg_wk, in_=wk_all[:]), R=[r_wk], Wn=[r_dbg])
            P.barrier()

        with ExitStack() as stB:
            sb = mk_alloc(stB)
            p_sgu = TPool(P, sb, "sgu", [128, 2, 2048], F32, 3)
            p_sdn = TPool(P, sb, "sdn", [128, 2, 1024], F32, 3)
            p_gu = TPool(P, sb, "wgu", [128, 8, 512], F32R, 2)
            p_dn = TPool(P, sb, "wdn", [128, 2, 1024], F32R, 4)
            p_idx = TPool(P, sb, "idx", [128, 2], I32, 3)
            p_xg = TPool(P, sb, "xg", [128, 1024], F32, 3)
            p_xgT = TPool(P, sb, "xgT", [128, 8, 128], F32R, 2)
            p_sg2 = TPool(P, sb, "sg2", [128, 256], F32, 2)
            p_hh = TPool(P, sb, "hh", [128, 256], F32, 2)
            p_hhT = TPool(P, sb, "hhT", [128, 2, 128], F32R, 2)
            p_y = TPool(P, sb, "ysb", [128, 1024], F32, 2)
            lng2 = sb("lng2", [128, 1024]); r_lng2 = P.res("lng2")
            lnb2 = sb("lnb2", [128, 1024]); r_lnb2 = P.res("lnb2")
            sbp = {
                "stats": TPool(P, sb, "statsB", [128, 4, 6], F32, 2),
                "mv": TPool(P, sb, "mvB", [128, 2], F32, 2),
                "rstd": TPool(P, sb, "rstdB", [128, 1], F32, 2),
            }
            P.dma("act", lambda e: e.dma_start(out=lng2[:], in_=ln_g[2].partition_broadcast(128)), W=[r_lng2])
            P.dma("act", lambda e: e.dma_start(out=lnb2[:], in_=ln_b[2].partition_broadcast(128)), W=[r_lnb2])

            njobs = E_RUN + (NT if PHASE3 else 0)
            jobs = {}
            for i in range(3):
                P.op("pool", lambda e, i=i: e.memset(p_xg.tiles[i][:], 0.0), W=[p_xg.ress[i]])

            def st_L(j):
                c = {}
                jobs[j] = c
                xg, r_xg = p_xg.next()
                c["xg"] = (xg, r_xg)
                sgu, r_sgu = p_sgu.next()
                sdn, r_sdn = p_sdn.next()
                c["sgu"] = (sgu, r_sgu)
                c["sdn"] = (sdn, r_sdn)
                if j < E_RUN:
                    wg_, wu_, wd_ = w_gate[j], w_up[j], w_down[j]
                else:
                    wg_, wu_, wd_ = ws_gate, ws_up, ws_down
                P.dma("sp", lambda e: e.dma_start(out=sgu[:, 0, :], in_=wg_.rearrange("(p c) n -> p (c n)", c=8)), W=[r_sgu])
                P.dma("sp", lambda e: e.dma_start(out=sgu[:, 1, :], in_=wu_.rearrange("(p c) n -> p (c n)", c=8)), W=[r_sgu])
                P.dma("sp", lambda e: e.dma_start(out=sdn[:].rearrange("p c n -> p (c n)"), in_=wd_.rearrange("(p c) n -> p (c n)", c=2)), W=[r_sdn])
                if j < E_RUN:
                    idx, r_idx = p_idx.next()
                    P.dma("pool", lambda e: e.dma_start(out=idx[:], in_=list_dram[j * CAP:(j + 1) * CAP, :]), R=[r_list], W=[r_idx])
                    P.dma("pool", lambda e: e.indirect_dma_start(out=xg[:], out_offset=None, in_=h2_dram,
                                                                 in_offset=bass.IndirectOffsetOnAxis(ap=idx[:, 0:1], axis=0),
                                                                 bounds_check=P.reg(e, NT * 128 - 1), oob_is_err=False),
                          R=[r_h2d, r_idx], W=[r_xg])
                else:
                    i = j - E_RUN
                    P.dma("act", lambda e: e.dma_start(out=xg[:], in_=h2_dram[i * 128:(i + 1) * 128, :]), R=[r_h2d], W=[r_xg])

            def st_R(j):
                c = jobs[j]
                sgu, r_sgu = c["sgu"]
                sdn, r_sdn = c["sdn"]
                gu, r_gu = p_gu.next()
                dn, r_dn = p_dn.next()
                c["gu"] = (gu, r_gu)
                c["dn"] = (dn, r_dn)
                P.op("act", lambda e: e.copy(out=gu[:, :, 0:256], in_=sgu[:, 0, :].rearrange("p (c n) -> p c n", n=256)), R=[r_sgu], W=[r_gu])
                P.op("dve", lambda e: e.tensor_copy(out=gu[:, :, 256:512], in_=sgu[:, 1, :].rearrange("p (c n) -> p c n", n=256)), R=[r_sgu], W=[r_gu])
                P.op("act", lambda e: e.copy(out=dn[:, 0, :], in_=sdn[:, 0, :]), R=[r_sdn], W=[r_dn])
                P.op("dve", lambda e: e.tensor_copy(out=dn[:, 1, :], in_=sdn[:, 1, :]), R=[r_sdn], W=[r_dn])

            def st_A(j):
                c = jobs[j]
                xg, r_xg = c["xg"]
                xgT, r_xgT = p_xgT.next()
                c["xgT"] = (xgT, r_xgT)
                xv = xg[:].rearrange("s (p c) -> s c p", c=8)
                transpose_to(lambda cc: xv[:, cc, :], 8, r_xg, xgT, r_xgT)

            def st_B(j):
                c = jobs[j]
                xgT, r_xgT = c["xgT"]
                gu, r_gu = c["gu"]
                gb, r_gb = bank()
                for cc in range(8):
                    P.op("pe", lambda e, cc=cc: e.matmul(out=gb[:], lhsT=xgT[:, cc, :], rhs=gu[:, cc, :], start=(cc == 0), stop=(cc == 7)),
                         R=[r_xgT, r_gu], W=[r_gb])
                sg2, r_sg2 = p_sg2.next()
                hh, r_hh = p_hh.next()
                P.op("act", lambda e: e.activation(out=sg2[:], in_=gb[:, 0:256], func=AF.Silu), R=[r_gb], W=[r_sg2])
                P.op("dve", lambda e: e.tensor_tensor(out=hh[:], in0=gb[:, 256:512], in1=sg2[:], op=ALU.mult), R=[r_gb, r_sg2], W=[r_hh])
                c["hh"] = (hh, r_hh)

            def st_C(j):
                c = jobs[j]
                hh, r_hh = c["hh"]
                hhT, r_hhT = p_hhT.next()
                c["hhT"] = (hhT, r_hhT)
                hv = hh[:].rearrange("s (p c) -> s c p", c=2)
                transpose_to(lambda cc: hv[:, cc, :], 2, r_hh, hhT, r_hhT)

            def st_D(j):
                c = jobs[j]
                hhT, r_hhT = c["hhT"]
                dn, r_dn = c["dn"]
                yb = [bank(), bank()]
                for nb_ in range(2):
                    bk, r_bk = yb[nb_]
                    for cc in range(2):
                        P.op("pe", lambda e, cc=cc, nb_=nb_, bk=bk: e.matmul(out=bk[:], lhsT=hhT[:, cc, :], rhs=dn[:, cc, nb_ * 512:(nb_ + 1) * 512],
                                                                           start=(cc == 0), stop=(cc == 1)),
                             R=[r_hhT, r_dn], W=[r_bk])
                ysb, r_ysb = p_y.next()
                P.op("act", lambda e: e.copy(out=ysb[:, 0:512], in_=yb[0][0][:]), R=[yb[0][1]], W=[r_ysb])
                P.op("dve", lambda e: e.tensor_copy(out=ysb[:, 512:1024], in_=yb[1][0][:]), R=[yb[1][1]], W=[r_ysb])
                P.dma("pool", lambda e: e.dma_start(out=ys_dram[j * CAP:(j + 1) * CAP, :], in_=ysb[:]), R=[r_ysb], Wn=[r_ys])
                del jobs[j]

            for it in range(-4, njobs):
                for (fn, off) in ((st_L, 4), (st_R, 3), (st_A, 3), (st_B, 2), (st_C, 1), (st_D, 0)):
                    j = it + off
                    if 0 <= j < njobs:
                        fn(j)

            p_acc = TPool(P, sb, "acc", [128, 1024], F32, 2)
            p_yk = TPool(P, sb, "yk", [128, 1024], F32, 3)
            for i in range(NT if PHASE3 else 0):
                xg, r_xg = p_xg.next()
                ysh, r_ysh = p_xg.next()
                P.dma("act", lambda e: e.dma_start(out=xg[:], in_=h2_dram[i * 128:(i + 1) * 128, :]), R=[r_h2d], W=[r_xg])
                P.dma("act", lambda e: e.dma_start(out=ysh[:], in_=ys_dram[(E_RUN + i) * CAP:(E_RUN + i + 1) * CAP, :]), R=[r_ys], W=[r_ysh])
                acc, r_acc = p_acc.next()
                P.op("dve", lambda e: e.scalar_tensor_tensor(out=acc[:], in0=xg[:], scalar=ALPHA, in1=ysh[:], op0=ALU.mult, op1=ALU.add),
                     R=[r_xg, r_ysh], W=[r_acc])
                for k in range(8):
                    yk, r_yk = p_yk.next()
                    P.dma("pool", lambda e, k=k: e.indirect_dma_start(
                        out=yk[:], out_offset=None, in_=ys_dram,
                        in_offset=bass.IndirectOffsetOnAxis(ap=destI[:, i * 8 + k:i * 8 + k + 1], axis=0),
                        bounds_check=P.reg(e, E * CAP - 1), oob_is_err=False),
                        R=[r_ys, r_destI], W=[r_yk])
                    P.op("dve", lambda e, k=k: e.scalar_tensor_tensor(
                        out=acc[:], in0=yk[:], scalar=wk_all[:, i * 8 + k:i * 8 + k + 1], in1=acc[:], op0=ALU.mult, op1=ALU.add),
                        R=[r_yk, r_wk, r_acc], W=[r_acc])
                ysb, r_ysb = p_y.next()
                layer_norm(sbp, acc, r_acc, ysb, r_ysb, lng2, r_lng2, lnb2, r_lnb2)
                P.dma("sp", lambda e: e.dma_start(out=out[i * 128:(i + 1) * 128, :], in_=ysb[:]), R=[r_ysb], Wn=[r_out])
            P.wait_all("sp", [r_out, r_dbg])
            P.barrier()
        P.emit()
    return nc


def _t5_bucket(n):
    n = np.maximum(n, 0)
    ratio = np.log(np.maximum(n, 1).astype(np.float32) / np.float32(16)) / np.float32(math.log(8.0))
    large = 16 + (ratio * np.float32(16)).astype(np.int32)
    large = np.minimum(large, 31)
    return np.where(n < 16, n, large)


def _constants():
    c = {}
    c["c_ident"] = np.eye(128, dtype=np.float32)
    H = 4
    lg = np.log(1.0 - 2.0 ** (-5.0 - np.arange(H, dtype=np.float64)))
    idx = np.arange(128, dtype=np.float64)
    qk_scale = 64.0 ** -0.5
    dec = np.zeros((128, H, 128), np.float64)
    for h in range(H):
        d = idx[None, :] - idx[:, None]
        dec[:, h, :] = np.where(d >= 0, np.exp(np.maximum(d, 0) * lg[h]), 0.0) * qk_scale
    c["c_decay"] = dec.reshape(128, 512).astype(np.float32)
    zeta = np.exp((127.0 - idx)[:, None] * lg[None, :]) * qk_scale
    c["c_zeta"] = zeta.astype(np.float32)
    pz = np.zeros((128, NPRE, 4), np.float64)
    for t in range(NPRE):
        pz[:, t, :] = zeta * np.exp(128.0 * lg[None, :] * (NPRE - 1 - t))
    c["c_pz"] = pz.reshape(128, NPRE * 4).astype(np.float32)
    xi = np.exp((idx + 1.0)[None, :] * lg[:, None])
    xil = np.zeros((128, 2, 128), np.float64)
    cdl = np.zeros((128, 4, 128), np.float64)
    for h in range(H):
        po = (h % 2) * 64
        xil[po:po + 64, h // 2, :] = xi[h][None, :]
        cdl[:, h, :] = np.exp(128.0 * lg[h])
    c["c_xi"] = xil.reshape(128, 256).astype(np.float32)
    c["c_cd"] = cdl.reshape(128, 512).astype(np.float32)
    c["c_ltri"] = (np.arange(128)[:, None] < np.arange(128)[None, :]).astype(np.float32)
    c["c_iota"] = np.tile(np.arange(256, dtype=np.float32)[None, :], (128, 1))
    tok = np.zeros((128, NT, 2), np.int32)
    for i in range(NT):
        tok[:, i, :] = (i * 128 + np.arange(128))[:, None]
    c["c_tok"] = tok.reshape(128, NT * 2)
    i_ = np.arange(128)[None, :]
    j_ = np.arange(128)[:, None]
    mask = np.zeros((128, 2, 2, 4, 128), np.float32)
    bidx = np.zeros((128, 2, 128), np.int64)
    for half in range(2):
        dist = i_ + 128 - (j_ + half * 128)
        valid = (dist >= 0) & (dist < 128)
        mask[:, :, half, :, :] = np.where(valid, 0.0, NEG)[:, None, None, :]
        bidx[:, half, :] = _t5_bucket(np.clip(dist, 0, 127))
    c["c_mask"] = mask.reshape(128, 2048)
    return c, bidx


def _prepare_inputs(inp):
    consts, bidx = _constants()
    x = np.ascontiguousarray(inp["x"], dtype=np.float32)
    rel_bias = np.asarray(inp["rel_bias"], np.float32)
    rb = np.zeros((128, 2, 2, 4, 128), np.float32)
    for kv in range(2):
        for g in range(4):
            for half in range(2):
                rb[:, kv, half, g, :] = rel_bias[bidx[:, half, :], kv * 4 + g]
    consts["c_rb"] = rb.reshape(128, 2048)
    inv = 10000.0 ** (-np.arange(32, dtype=np.float32) / np.float32(32))
    shared = {
        "ln_g0": inp["ln_in_g"].reshape(1, 1024), "ln_b0": inp["ln_in_b"].reshape(1, 1024),
        "ln_g1": inp["ln_mix_g"].reshape(1, 1024), "ln_b1": inp["ln_mix_b"].reshape(1, 1024),
        "ln_g2": inp["ln_ffn_g"].reshape(1, 1024), "ln_b2": inp["ln_ffn_b"].reshape(1, 1024),
        "w_in": inp["w_in"][0], "w_out": inp["w_out"][0], "w_router": inp["w_router"][0],
        "rbias": inp["router_bias"].reshape(1, 256),
        "w_gate": inp["w_gate"][0][:max(E_RUN, 1)], "w_up": inp["w_up"][0][:max(E_RUN, 1)], "w_down": inp["w_down"][0][:max(E_RUN, 1)],
        "ws_gate": inp["ws_gate"][0], "ws_up": inp["ws_up"][0], "ws_down": inp["ws_down"][0],
        "sinks": inp["attn_sinks"].reshape(1, 8),
    }
    shared = {k: np.ascontiguousarray(v, dtype=np.float32) for k, v in shared.items()}
    shared.update(consts)
    in_maps = []
    for c in range(NCORES):
        b, q = c // 4, c % 4
        t0 = q * NT * 128
        m = dict(shared)
        m["x_own"] = x[b, t0:t0 + NT * 128]
        xp = np.zeros((NPRE * 128, 1024), np.float32)
        npre = min(t0, NPRE * 128)
        if npre:
            xp[NPRE * 128 - npre:] = x[b, t0 - npre:t0]
        m["x_pre"] = xp
        pm = np.zeros((128, NPRE), np.float32)
        pm[:, NPRE - npre // 128:] = 1.0 if npre else 0.0
        if not npre:
            pm[:] = 0.0
        m["pmask"] = pm
        m["hflag"] = np.full((128, 1), 1.0 if q > 0 else 0.0, np.float32)
        pos = (t0 - NPRE * 128 + np.arange((NPRE + NT) * 128)).astype(np.float32)
        ang = pos[:, None] * inv[None, :]
        rp = np.concatenate([np.cos(ang), np.sin(ang)], axis=1).astype(np.float32)
        m["rope"] = rp.reshape(NPRE + NT, 128, 64)
        in_maps.append(m)
    return in_maps


_NC_CACHE = {}


def kernel(**inputs):
    in_maps = _prepare_inputs(inputs)
    if "nc" not in _NC_CACHE:
        _NC_CACHE["nc"] = build_program()
    res = run_bass_kernel_spmd(_NC_CACHE["nc"], in_maps, core_ids=list(range(NCORES)))
    outs = [np.asarray(r["out"], dtype=np.float32) for r in res.results]
    full = np.stack(outs, 0).reshape(2, 4 * NT * 128, 1024)
    if DEBUG:
        kernel.dbg = res.results
    return full
```

```python
import math
import types
from contextlib import ExitStack

import numpy as np
import concourse.bass as bass
import concourse.mybir as mybir
from concourse.bass_utils import run_bass_kernel_spmd

F32 = mybir.dt.float32
F32R = mybir.dt.float32r
I32 = mybir.dt.int32
U32 = mybir.dt.uint32
AF = mybir.ActivationFunctionType
ALU = mybir.AluOpType

NCORES = 8
NT = 16
NPRE = 48
E = 256
E_RUN = 256
PHASE3 = True
VARIANT = 0
SBUF_ALIGN = 32
STAGE = 7
CAP = 128
ALPHA = 2.0 ** 0.25
NEG = -200.0
CHUNK_DECAY = [float(np.float32((1.0 - 2.0 ** (-5.0 - h)) ** 128)) for h in range(4)]
DEBUG = False


class Res:
    __slots__ = ("name", "w", "rs", "dsem", "dcount")

    def __init__(self, name):
        self.name = name
        self.w = None
        self.rs = []
        self.dsem = None
        self.dcount = 0


def _freeze(fn):
    if fn.__closure__ is None:
        return fn
    cells = []
    for c in fn.__closure__:
        try:
            cells.append(types.CellType(c.cell_contents))
        except ValueError:
            cells.append(c)
    return types.FunctionType(fn.__code__, fn.__globals__, fn.__name__, fn.__defaults__, tuple(cells))


class Prog:
    ENG = ("pe", "dve", "act", "pool", "sp")

    def __init__(self, nc, stack):
        self.nc = nc
        self.stack = stack
        self.stream = {e: [] for e in self.ENG}
        self.seq = {e: 0 for e in self.ENG}
        self.known = {e: {} for e in self.ENG}
        self.esem = {e: stack.enter_context(nc.semaphore("es_" + e)) for e in self.ENG}
        self.used = set()
        self.nd = 0
        self.allres = []
        self._regs = {}

    def reg(self, eng, val):
        key = (id(eng), val)
        if key not in self._regs:
            self._regs[key] = eng.to_reg(val)
        return self._regs[key]

    def res(self, name):
        r = Res(name)
        self.allres.append(r)
        return r

    def _need(self, eng, toks):
        kn = self.known[eng]
        for t in toks:
            if t is None:
                continue
            if t[0] == "e":
                if t[1] == "pe" and eng == "pe":
                    continue
                key = ("e", t[1])
                if kn.get(key, 0) >= t[2]:
                    continue
                kn[key] = t[2]
                self.used.add((t[1], t[2]))
                self.stream[eng].append(("we", t[1], t[2]))
            else:
                key = ("d", id(t[1]))
                if kn.get(key, 0) >= t[2]:
                    continue
                kn[key] = t[2]
                self.stream[eng].append(("wd", t[1], t[2]))

    @staticmethod
    def _deps(R, W):
        toks = []
        for r in R:
            toks.append(r.w)
        for r in W:
            toks.append(r.w)
            toks.extend(r.rs)
        return toks

    def op(self, eng, fn, R=(), W=()):
        self._need(eng, self._deps(R, W))
        self.seq[eng] += 1
        s = self.seq[eng]
        tok = ("e", eng, s)
        self.stream[eng].append(("op", _freeze(fn), s))
        for r in R:
            r.rs.append(tok)
        for r in W:
            r.w = tok
            r.rs = []
        return tok

    def dma(self, q, fn, R=(), W=(), Wn=()):
        self._need(q, self._deps(R, W))
        sr = W[0] if W else Wn[0]
        if sr.dsem is None:
            sr.dsem = self.stack.enter_context(self.nc.semaphore("ds%d" % self.nd))
            self.nd += 1
        sr.dcount += 16
        tok = ("d", sr.dsem, sr.dcount)
        self.stream[q].append(("dma", _freeze(fn), sr.dsem))
        for r in R:
            r.rs.append(tok)
        for r in W:
            r.w = tok
            r.rs = []
        for r in Wn:
            r.w = tok
        return tok

    def wait_all(self, eng, ress):
        best = {}
        for r in ress:
            for t in [r.w] + list(r.rs):
                if t is None:
                    continue
                if t[0] == "e":
                    if t[1] == "pe" and eng == "pe":
                        continue
                    key = ("e", t[1])
                else:
                    key = ("d", id(t[1]))
                if key not in best or best[key][2] < t[2]:
                    best[key] = t
        toks = [t for t in best.values()]
        for i in range(0, len(toks), 3):
            self._need(eng, toks[i:i + 3])
            if i + 3 < len(toks):
                self.seq[eng] += 1
                self.stream[eng].append(("op", (lambda e: e.nop()), self.seq[eng]))

    def barrier(self):
        self.wait_all("sp", self.allres)
        tok = self.op("sp", lambda e: e.nop())
        for e in self.ENG:
            if e != "sp":
                self._need(e, [tok])

    def emit(self):
        nc = self.nc
        val = {}
        for e in self.ENG:
            c = 0
            m = {}
            for it in self.stream[e]:
                if it[0] == "op" and (e, it[2]) in self.used:
                    c += 1
                    m[it[2]] = c
            val[e] = m
        engobj = {"pe": "tensor", "dve": "vector", "act": "scalar", "pool": "gpsimd", "sp": "sync"}
        esem = self.esem
        used = self.used
        with nc.Block() as block:
            for e in self.ENG:
                items = self.stream[e]

                def body(eng, items=items, e=e):
                    for it in items:
                        k = it[0]
                        if k == "we":
                            eng.wait_ge(esem[it[1]], val[it[1]][it[2]])
                        elif k == "wd":
                            eng.wait_ge(it[1], it[2])
                        elif k == "op":
                            ins = it[1](eng)
                            if (e, it[2]) in used:
                                ins.then_inc(esem[e], 1)
                        else:
                            it[1](eng).then_inc(it[2], 16)
                getattr(block, engobj[e])(body)


class TPool:
    def __init__(self, P, alloc, name, shape, dt, n):
        self.tiles = [alloc("%s_%d" % (name, i), shape, dt) for i in range(n)]
        self.ress = [P.res("%s_%d" % (name, i)) for i in range(n)]
        self.i = 0

    def next(self):
        k = self.i % len(self.tiles)
        self.i += 1
        return self.tiles[k], self.ress[k]


def build_program():
    nc = bass.Bass("TRN2", target_bir_lowering=False)

    def din(name, shape, dt=F32):
        return nc.dram_tensor(name, list(shape), dt, kind="ExternalInput").ap()

    x_own = din("x_own", [NT * 128, 1024])
    x_pre = din("x_pre", [NPRE * 128, 1024])
    rope = din("rope", [NPRE + NT, 128, 64])
    pmask = din("pmask", [128, NPRE])
    hflag = din("hflag", [128, 1])
    ln_g = [din("ln_g%d" % i, [1, 1024]) for i in range(3)]
    ln_b = [din("ln_b%d" % i, [1, 1024]) for i in range(3)]
    w_in = din("w_in", [1024, 2304])
    w_out = din("w_out", [1024, 1024])
    w_router = din("w_router", [1024, 256])
    rbias = din("rbias", [1, 256])
    w_gate = din("w_gate", [max(E_RUN, 1), 1024, 256])
    w_up = din("w_up", [max(E_RUN, 1), 1024, 256])
    w_down = din("w_down", [max(E_RUN, 1), 256, 1024])
    ws_gate = din("ws_gate", [1024, 256])
    ws_up = din("ws_up", [1024, 256])
    ws_down = din("ws_down", [256, 1024])
    sinks = din("sinks", [1, 8])
    c_ident = din("c_ident", [128, 128])
    c_decay = din("c_decay", [128, 512])
    c_zeta = din("c_zeta", [128, 4])
    c_pz = din("c_pz", [128, NPRE * 4])
    c_xi = din("c_xi", [128, 256])
    c_cd = din("c_cd", [128, 512])
    c_rb = din("c_rb", [128, 2048])
    c_mask = din("c_mask", [128, 2048])
    c_ltri = din("c_ltri", [128, 128])
    c_iota = din("c_iota", [128, 256])
    c_tok = din("c_tok", [128, NT * 2], I32)

    out = nc.dram_tensor("out", [NT * 128, 1024], F32, kind="ExternalOutput").ap()
    h2_dram = nc.dram_tensor("h2_dram", [NT * 128, 1024], F32, kind="Internal").ap()
    ys_dram = nc.dram_tensor("ys_dram", [(E + NT) * CAP, 1024], F32, kind="Internal").ap()
    list_dram = nc.dram_tensor("list_dram", [E * CAP, 2], I32, kind="Internal").ap()
    if DEBUG:
        dbg_h2 = nc.dram_tensor("dbg_h2", [NT * 128, 1024], F32, kind="ExternalOutput").ap()
        dbg_dest = nc.dram_tensor("dbg_dest", [128, NT * 8], I32, kind="ExternalOutput").ap()
        dbg_wk = nc.dram_tensor("dbg_wk", [128, NT * 8], F32, kind="ExternalOutput").ap()

    with ExitStack() as st0:
        P = Prog(nc, st0)

        cur = [16481]
        npad = [0]
        stack_marks = []

        def mk_alloc(st):
            base_at_entry = cur[0]
            st.callback(lambda: cur.__setitem__(0, base_at_entry))

            def sb(name, shape, dt=F32):
                a32 = (cur[0] + 31) // 32 * 32
                if a32 % SBUF_ALIGN:
                    npad[0] += 1
                    st.enter_context(nc.sbuf_tensor("pad%d" % npad[0], [128, 1], mybir.dt.uint8))
                    a32 += 32
                nbytes = int(np.prod(shape[1:])) * mybir.dt.size(dt)
                cur[0] = a32 + nbytes
                return st.enter_context(nc.sbuf_tensor(name, list(shape), dt))
            return sb

        sb0 = mk_alloc(st0)
        banks = [st0.enter_context(nc.psum_tensor("bank%d" % i, [128, 512], F32)) for i in range(8)]
        bres = [P.res("bank%d" % i) for i in range(8)]
        bctr = [0]

        nbank = [7]

        def bank():
            k = bctr[0] % nbank[0]
            bctr[0] += 1
            return banks[k], bres[k]

        sbank, r_sbank = banks[7], bres[7]

        ident = sb0("ident", [128, 128]); r_ident = P.res("ident")
        destI = sb0("destI", [128, NT * 8], I32); r_destI = P.res("destI")
        wk_all = sb0("wk_all", [128, NT * 8]); r_wk = P.res("wk_all")
        r_h2d = P.res("h2_dram"); r_ys = P.res("ys_dram"); r_list = P.res("list_dram"); r_out = P.res("out")
        r_dbg = P.res("dbg")
        P.dma("sp", lambda e: e.dma_start(out=ident[:], in_=c_ident), W=[r_ident])

        mhalf = sb0("mhalf", [128, 1]); r_mhalf = P.res("mhalf")
        P.op("pool", lambda e: e.memset(mhalf[:], -0.5), W=[r_mhalf])

        def ln_stats(sbp, src, r_src, eps, width=1024, rs_eng="act"):
            stats, r_st = sbp["stats"].next()
            mv, r_mv = sbp["mv"].next()
            rstd, r_rstd = sbp["rstd"].next()
            nchunk = width // 512
            for c in range(nchunk):
                P.op("dve", lambda e, c=c: e.bn_stats(out=stats[:, c, :], in_=src[:, c * 512:(c + 1) * 512]),
                     R=[r_src], W=[r_st])
            P.op("dve", lambda e: e.bn_aggr(out=mv[:], in_=stats[:, 0:nchunk, :].rearrange("p a b -> p (a b)")),
                 R=[r_st], W=[r_mv])
            if rs_eng == "pool":
                P.op("pool", lambda e: e.tensor_scalar(out=rstd[:], in0=mv[:, 1:2], scalar1=eps, scalar2=None, op0=ALU.add),
                     R=[r_mv], W=[r_rstd])
                P.op("pool", lambda e: e.tensor_tensor(out=rstd[:], in0=rstd[:], in1=mhalf[:], op=ALU.pow),
                     R=[r_rstd, r_mhalf], W=[r_rstd])
                return mv, r_mv, rstd, r_rstd
            P.op("act", lambda e: e.activation(out=rstd[:], in_=mv[:, 1:2], func=AF.Sqrt, bias=eps, scale=1.0),
                 R=[r_mv], W=[r_rstd])
            P.op("dve", lambda e: e.reciprocal(out=rstd[:], in_=rstd[:]), R=[r_rstd], W=[r_rstd])
            return mv, r_mv, rstd, r_rstd

        def layer_norm(sbp, src, r_src, dst, r_dst, g_t, r_g, b_t, r_b, g_eng="pool", rs_eng="act", b_eng="pool"):
            mv, r_mv, rstd, r_rstd = ln_stats(sbp, src, r_src, 1e-5, rs_eng=rs_eng)
            P.op("dve", lambda e: e.tensor_scalar(out=dst[:], in0=src[:], scalar1=mv[:, 0:1], scalar2=rstd[:, 0:1],
                                                  op0=ALU.subtract, op1=ALU.mult),
                 R=[r_src, r_mv, r_rstd], W=[r_dst])
            P.op(g_eng, lambda e: e.tensor_tensor(out=dst[:], in0=dst[:], in1=g_t[:], op=ALU.mult),
                 R=[r_dst, r_g], W=[r_dst])
            P.op(b_eng, lambda e: e.tensor_tensor(out=dst[:], in0=dst[:], in1=b_t[:], op=ALU.add),
                 R=[r_dst, r_b], W=[r_dst])

        def transpose_to(src_fn, nchunks, r_src, dstT, r_dstT, rows=128, evac="mix"):
            for b0 in range(0, nchunks, 4):
                bk, r_bk = bank()
                nb = min(4, nchunks - b0)
                for c in range(b0, b0 + nb):
                    P.op("pe", lambda e, c=c, bk=bk, b0=b0: e.transpose(
                        out=bk[0:rows, (c - b0) * 128:(c - b0 + 1) * 128], in_=src_fn(c), identity=ident[:]),
                        R=[r_src, r_ident], W=[r_bk])
                eng = "act" if ((b0 // 4) % 2 == 0 or evac == "act") else "dve"
                if eng == "act":
                    P.op("act", lambda e, bk=bk, b0=b0, nb=nb: e.copy(
                        out=dstT[0:rows, b0:b0 + nb, :].rearrange("p a b -> p (a b)"), in_=bk[0:rows, 0:nb * 128]),
                        R=[r_bk], W=[r_dstT])
                else:
                    P.op("dve", lambda e, bk=bk, b0=b0, nb=nb: e.tensor_copy(
                        out=dstT[0:rows, b0:b0 + nb, :].rearrange("p a b -> p (a b)"), in_=bk[0:rows, 0:nb * 128]),
                        R=[r_bk], W=[r_dstT])

        with ExitStack() as stA:
            sb = mk_alloc(stA)
            w_in_t = sb("w_in_t", [128, 8, 2304], F32R); r_w_in = P.res("w_in")
            w_out_t = sb("w_out_t", [128, 8, 1024], F32R); r_w_out = P.res("w_out")
            w_r_t = sb("w_r_t", [128, 8, 256], F32R); r_w_r = P.res("w_r")
            lng = [sb("lng%d" % i, [128, 1024]) for i in range(2)]
            lnb = [sb("lnb%d" % i, [128, 1024]) for i in range(2)]
            r_lng = [P.res("lng%d" % i) for i in range(2)]
            r_lnb = [P.res("lnb%d" % i) for i in range(2)]
            decay_t = sb("decay_t", [128, 512]); r_decay = P.res("decay")
            zeta_t = sb("zeta_t", [128, 4]); r_zeta = P.res("zeta")
            pz_t = sb("pz_t", [128, NPRE * 4]); r_pz = P.res("pz")
            xi_t = sb("xi_t", [128, 256]); r_xi = P.res("xi")
            btab = sb("btab", [128, 2048]); r_btab = P.res("btab")
            ltri = sb("ltri", [128, 128]); r_ltri = P.res("ltri")
            ones_t = sb("ones_t", [128, 128]); r_ones = P.res("ones")
            iota_t = sb("iota_t", [128, 256]); r_iota = P.res("iota")
            rb_t = sb("rb_t", [128, 256]); r_rb = P.res("rbias")
            tok_t = sb("tok_t", [128, NT * 2], I32); r_tok = P.res("tok")
            pm_t = sb("pm_t", [128, NPRE]); r_pm = P.res("pm")
            hf_t = sb("hf_t", [128, 1]); r_hf = P.res("hf")
            sexp = sb("sexp", [128, 8]); r_sexp = P.res("sexp")
            state = sb("state", [128, 512]); r_state = P.res("state")
            selsum = sb("selsum", [128, 256]); r_selsum = P.res("selsum")

            sbp = {
                "stats": TPool(P, sb, "stats", [128, 4, 6], F32, 2),
                "mv": TPool(P, sb, "mv", [128, 2], F32, 2),
                "rstd": TPool(P, sb, "rstd", [128, 1], F32, 2),
            }
            p_xt = TPool(P, sb, "xt", [128, 1024], F32, 2)
            p_rp = TPool(P, sb, "rp", [128, 64], F32, 2)
            hN, r_hN = sb("hN", [128, 1024]), P.res("hN")
            hT, r_hT = sb("hT", [128, 8, 128], F32R), P.res("hT")
            qk_raw, r_qk_raw = sb("qk_raw", [128, 512]), P.res("qk_raw")
            qk_rot, r_qk_rot = sb("qk_rot", [128, 512]), P.res("qk_rot")
            rt = [sb("rt%d" % i, [128, 256]) for i in range(2)]
            r_rt = [P.res("rt%d" % i) for i in range(2)]
            v_sb, r_v = sb("v_sb", [128, 512]), P.res("v_sb")
            sg, r_sg = sb("sg", [128, 512]), P.res("sg")
            qs_pad, r_qs = sb("qs_pad", [128, 8, 128]), P.res("qs_pad")
            kv_sb, r_ks = sb("kv_sb", [128, 256]), P.res("kv_sb")
            ks_sb = kv_sb[:, 0:128]
            if VARIANT >= 100:
                spacer = sb("spacer", [128, 64])
            p_v1 = TPool(P, sb, "v1", [128, 2, 128], F32, 2)
            qT, r_qT = sb("qT", [128, 2, 128]), P.res("qT")
            krm, r_krm = sb("krm", [128, 4, 128]), P.res("krm")
            kTm, r_kTm = sb("kTm", [128, 4, 128]), P.res("kTm")
            qxT, r_qxT = sb("qxT", [128, 256]), P.res("qxT")
            scT, r_scT = qk_raw, r_qk_raw
            kz, r_kz = sb("kz", [128, 4, 128]), P.res("kz")
            gmv, r_gmv = sb("gmv", [128, 4, 2]), P.res("gmv")
            grstd, r_grstd = sb("grstd", [128, 4]), P.res("grstd")
            concat, r_cc = sb("concat", [128, 1024]), P.res("concat")
            qsT, r_qsT = sb("qsT", [128, 8, 128]), P.res("qsT")
            p_ksT = TPool(P, sb, "ksT", [128, 128], F32, 2)
            p_psb = TPool(P, sb, "psb", [128, 512], F32, 2)
            den, r_den = sb("den", [128, 8]), P.res("den")
            h2, r_h2 = hN, r_hN
            sc, r_sc = sb("sc", [128, 256]), P.res("sc")
            choice, r_choice = sb("choice", [128, 256]), P.res("choice")
            g8, r_g8 = sb("g8", [128, 8, 8]), P.res("g8")
            gs, r_gs = sb("gs", [128, 8]), P.res("gs")
            s8, r_s8 = sb("s8", [128, 8]), P.res("s8")
            pen, r_pen = sb("pen", [128, 8]), P.res("pen")
            m8, r_m8 = sb("m8", [128, 8]), P.res("m8")
            i8, r_i8 = sb("i8", [128, 8], U32), P.res("i8")
            ekf, r_ekf = sb("ekf", [128, 8]), P.res("ekf")
            sel, r_sel = sb("sel", [128, 256]), P.res("sel")
            wn, r_wn = sc, r_sc
            dsum, r_dsum = sb("dsum", [128, 1]), P.res("dsum")
            posd, r_posd = sel, r_sel
            junk, r_junk = choice, r_choice
            destf, r_destf = sb("destf", [128, 8]), P.res("destf")
            lfill, r_lfill = qk_raw[:].bitcast(I32), r_qk_raw

            for c8 in range(8):
                P.dma("pool", lambda e, c8=c8: e.dma_start(out=w_in_t[:, c8, :], in_=w_in[c8 * 128:(c8 + 1) * 128, :], max_dma_last_dim=4096), W=[r_w_in])
            P.dma("pool", lambda e: e.dma_start(out=w_out_t[:], in_=w_out.rearrange("(c p) n -> p c n", p=128)), W=[r_w_out])
            P.dma("pool", lambda e: e.dma_start(out=w_r_t[:], in_=w_router.rearrange("(c p) n -> p c n", p=128)), W=[r_w_r])
            for i in range(2):
                P.dma("sp", lambda e, i=i: e.dma_start(out=lng[i][:], in_=ln_g[i].partition_broadcast(128)), W=[r_lng[i]])
                P.dma("sp", lambda e, i=i: e.dma_start(out=lnb[i][:], in_=ln_b[i].partition_broadcast(128)), W=[r_lnb[i]])
            for (t_, r_, src) in ((decay_t, r_decay, c_decay), (zeta_t, r_zeta, c_zeta), (pz_t, r_pz, c_pz), (xi_t, r_xi, c_xi),
                                  (btab, r_btab, c_rb), (ltri, r_ltri, c_ltri),
                                  (iota_t, r_iota, c_iota), (tok_t, r_tok, c_tok),
                                  (pm_t, r_pm, pmask), (hf_t, r_hf, hflag)):
                P.dma("act", lambda e, t_=t_, src=src: e.dma_start(out=t_[:], in_=src), W=[r_])
            P.dma("act", lambda e: e.dma_start(out=rb_t[:], in_=rbias.partition_broadcast(128)), W=[r_rb])
            P.dma("act", lambda e: e.dma_start(out=sexp[:], in_=sinks.partition_broadcast(128)), W=[r_sexp])
            P.op("act", lambda e: e.activation(out=sexp[:], in_=sexp[:], func=AF.Exp), R=[r_sexp], W=[r_sexp])
            for q4 in range(2):
                mt, r_mt = p_xt.next()
                P.dma("sp", lambda e, mt=mt, q4=q4: e.dma_start(out=mt[:], in_=c_mask[:, q4 * 1024:(q4 + 1) * 1024]), W=[r_mt])
                P.op("pool", lambda e, mt=mt, q4=q4: e.tensor_tensor(out=btab[:, q4 * 1024:(q4 + 1) * 1024],
                                                                     in0=btab[:, q4 * 1024:(q4 + 1) * 1024], in1=mt[:], op=ALU.add),
                     R=[r_mt, r_btab], W=[r_btab])
            P.op("pool", lambda e: e.memset(ones_t[:], 1.0), W=[r_ones])
            P.op("pool", lambda e: e.memset(state[:], 0.0), W=[r_state])
            P.op("pool", lambda e: e.memset(selsum[:], 0.0), W=[r_selsum])
            P.op("pool", lambda e: e.memset(krm[:], 0.0), W=[r_krm])
            P.op("pool", lambda e: e.memset(kz[:], 0.0), W=[r_kz])
            P.op("pool", lambda e: e.memset(qs_pad[:], 0.0), W=[r_qs])
            for i in range(2):
                P.op("pool", lambda e, i=i: e.memset(p_v1.tiles[i][:], 0.0), W=[p_v1.ress[i]])
                P.op("pool", lambda e, i=i: e.memset(p_v1.tiles[i][:, :, 64:65], 1.0), W=[p_v1.ress[i]])
            P.op("pool", lambda e: e.memset(lfill, NT * 128), W=[r_lfill])
            P.dma("pool", lambda e: e.dma_start(out=list_dram.rearrange("(p r) c -> p (r c)", p=128), in_=lfill),
                  R=[r_lfill], W=[r_list])
            P.wait_all("pool", [r_list])

            prev_ksT = [None, None]
            prev_v1 = [None, None]

            def mixer_tile(gt, is_own, i_own, pending=None):
                xsrc = x_own if is_own else x_pre
                ti = i_own if is_own else gt
                xt, r_xt = p_xt.next()
                rp, r_rp = p_rp.next()
                P.dma("sp", lambda e: e.dma_start(out=xt[:], in_=xsrc[ti * 128:(ti + 1) * 128, :]), W=[r_xt])
                P.dma("act", lambda e: e.dma_start(out=rp[:], in_=rope[gt]), W=[r_rp])
                layer_norm(sbp, xt, r_xt, hN, r_hN, lng[0], r_lng[0], lnb[0], r_lnb[0])
                transpose_to(lambda c: hN[:, c * 128:(c + 1) * 128], 8, r_hN, hT, r_hT)
                if STAGE < 2:
                    return
                need_swa_kv = is_own or gt == NPRE - 1

                def proj(col0, ncols):
                    bk, r_bk = bank()
                    for c in range(8):
                        P.op("pe", lambda e, c=c: e.matmul(out=bk[:, 0:ncols], lhsT=hT[:, c, :],
                                                           rhs=w_in_t[:, c, col0:col0 + ncols], start=(c == 0), stop=(c == 7)),
                             R=[r_hT, r_w_in], W=[r_bk])
                    return bk, r_bk

                if is_own:
                    bk, r_bk = proj(0, 512)
                    P.op("act", lambda e: e.copy(out=qk_raw[:], in_=bk[:]), R=[r_bk], W=[r_qk_raw])
                    lo, nh = 0, 8
                else:
                    bk, r_bk = proj(256, 256)
                    P.op("act", lambda e: e.copy(out=qk_raw[:, 256:512], in_=bk[:, 0:256]), R=[r_bk], W=[r_qk_raw])
                    lo, nh = 256, 4
                src4 = qk_raw[:, lo:512].rearrange("p (h t d) -> p h t d", t=2, d=32)
                dst4 = qk_rot[:, lo:512].rearrange("p (h t d) -> p h t d", t=2, d=32)
                cosb = rp[:, 0:32].unsqueeze(1).to_broadcast([128, nh, 32])
                sinb = rp[:, 32:64].unsqueeze(1).to_broadcast([128, nh, 32])
                ta = rt[0][:, 0:nh * 32].rearrange("p (h d) -> p h d", d=32)
                tb = rt[1][:, 0:nh * 32].rearrange("p (h d) -> p h d", d=32)
                P.op("pool", lambda e: e.tensor_tensor(out=ta, in0=src4[:, :, 0, :], in1=cosb, op=ALU.mult), R=[r_qk_raw, r_rp], W=[r_rt[0]])
                P.op("dve", lambda e: e.tensor_tensor(out=tb, in0=src4[:, :, 1, :], in1=sinb, op=ALU.mult), R=[r_qk_raw, r_rp], W=[r_rt[1]])
                P.op("dve", lambda e: e.tensor_tensor(out=dst4[:, :, 0, :], in0=ta, in1=tb, op=ALU.subtract), R=[r_rt[0], r_rt[1]], W=[r_qk_rot])
                P.op("pool", lambda e: e.tensor_tensor(out=ta, in0=src4[:, :, 0, :], in1=sinb, op=ALU.mult), R=[r_qk_raw, r_rp], W=[r_rt[0]])
                P.op("dve", lambda e: e.tensor_tensor(out=tb, in0=src4[:, :, 1, :], in1=cosb, op=ALU.mult), R=[r_qk_raw, r_rp], W=[r_rt[1]])
                P.op("dve", lambda e: e.tensor_tensor(out=dst4[:, :, 1, :], in0=ta, in1=tb, op=ALU.add), R=[r_rt[0], r_rt[1]], W=[r_qk_rot])
                bk, r_bk = proj(512, 512)
                P.op("act", lambda e: e.copy(out=v_sb[:], in_=bk[:]), R=[r_bk], W=[r_v])
                for h in range(4):
                    if is_own:
                        P.op("pool", lambda e, h=h: e.tensor_scalar(out=kz[:, h, (h % 2) * 64:(h % 2 + 1) * 64], in0=qk_rot[:, 256 + h * 64:256 + (h + 1) * 64],
                                                                    scalar1=zeta_t[:, h:h + 1], scalar2=None, op0=ALU.mult),
                             R=[r_qk_rot, r_zeta], W=[r_kz])
                    else:
                        P.op("pool", lambda e, h=h: e.tensor_scalar(out=kz[:, h, (h % 2) * 64:(h % 2 + 1) * 64], in0=qk_rot[:, 256 + h * 64:256 + (h + 1) * 64],
                                                                    scalar1=zeta_t[:, h:h + 1], scalar2=pm_t[:, gt:gt + 1], op0=ALU.mult, op1=ALU.mult),
                             R=[r_qk_rot, r_zeta, r_pm], W=[r_kz])
                if STAGE < 2.1:
                    return
                if is_own:
                    bk, r_bk = proj(1024, 512)
                    P.op("act", lambda e: e.activation(out=sg[:], in_=bk[:], func=AF.Silu), R=[r_bk], W=[r_sg])
                    if STAGE < 2.12:
                        return
                    bk, r_bk = proj(1536, 512)
                    for kv in range(2):
                        P.op("act", lambda e, kv=kv: e.copy(out=qs_pad[:, kv * 4:(kv + 1) * 4, kv * 64:(kv + 1) * 64],
                                                            in_=bk[:, kv * 256:(kv + 1) * 256].rearrange("p (g d) -> p g d", d=64)),
                             R=[r_bk], W=[r_qs])
                if STAGE < 2.13:
                    return
                if need_swa_kv and STAGE >= 2.2:
                    bk, r_bk = proj(2048, 256)
                    v1, r_v1 = p_v1.next()
                    P.op("act", lambda e: e.copy(out=kv_sb[:], in_=bk[:, 0:256]), R=[r_bk], W=[r_ks])
                    P.op("pool", lambda e: e.tensor_copy(out=v1[:, :, 0:64], in_=kv_sb[:, 128:256].rearrange("p (k d) -> p k d", d=64)),
                         R=[r_ks], W=[r_v1])
                    ksT, r_ksT = p_ksT.next()
                    bk, r_bk = bank()
                    if VARIANT == 1:
                        bk, r_bk = bank()
                    if VARIANT != 2:
                        P.op("pe", lambda e: e.transpose(out=bk[:, 0:128], in_=ks_sb, identity=ident[:]), R=[r_ks, r_ident], W=[r_bk])
                    if VARIANT == 3:
                        P.op("dve", lambda e: e.tensor_copy(out=ksT[:], in_=bk[:, 0:128]), R=[r_bk], W=[r_ksT])
                    elif VARIANT != 4:
                        P.op("act", lambda e: e.copy(out=ksT[:], in_=bk[:, 0:128]), R=[r_bk], W=[r_ksT])
                if pending is not None:
                    pending()
                if is_own and STAGE >= 2.3:
                    for hp in range(2):
                        P.op("pool", lambda e, hp=hp: e.tensor_copy(
                            out=krm[:].rearrange("p (a b) c -> p a b c", b=2)[:, :, hp, hp * 64:(hp + 1) * 64],
                            in_=qk_rot[:, 256:512].rearrange("p (a b d) -> p a b d", b=2, d=64)[:, :, hp, :]),
                            R=[r_qk_rot], W=[r_krm])
                    transpose_to(lambda c: qk_rot[:, c * 128:(c + 1) * 128], 2, r_qk_rot, qT, r_qT)
                    transpose_to(lambda c: krm[:, c, :], 4, r_krm, kTm, r_kTm)
                    P.op("pool", lambda e: e.tensor_tensor(out=qxT[:], in0=qT[:].rearrange("p a b -> p (a b)"), in1=xi_t[:], op=ALU.mult),
                         R=[r_qT, r_xi], W=[r_qxT])
                    bk, r_bk = bank()
                    for h in range(4):
                        P.op("pe", lambda e, h=h: e.matmul(out=bk[:, h * 128:(h + 1) * 128], lhsT=kTm[:, h, :],
                                                           rhs=qT[:, h // 2, :], start=True, stop=True),
                             R=[r_kTm, r_qT], W=[r_bk])
                    P.op("dve", lambda e: e.tensor_tensor(out=scT[:], in0=bk[:], in1=decay_t[:], op=ALU.mult), R=[r_bk, r_decay], W=[r_scT])
                    rbk, r_rbk = bank()
                    for h in range(4):
                        P.op("pe", lambda e, h=h: e.matmul(out=rbk[:, h * 128:(h + 1) * 128], lhsT=scT[:, h * 128:(h + 1) * 128],
                                                           rhs=v_sb[:, h * 128:(h + 1) * 128], start=True, stop=False),
                             R=[r_scT, r_v], W=[r_rbk])
                        P.op("pe", lambda e, h=h: e.matmul(out=rbk[:, h * 128:(h + 1) * 128], lhsT=qxT[:, (h // 2) * 128:(h // 2 + 1) * 128],
                                                           rhs=state[:, h * 128:(h + 1) * 128], start=False, stop=True),
                             R=[r_qxT, r_state], W=[r_rbk])
                if STAGE < 2.4:
                    return
                kbk, r_kbk = bank()
                for h in range(4):
                    P.op("pe", lambda e, h=h: e.matmul(out=kbk[:, h * 128:(h + 1) * 128], lhsT=kz[:, h, :],
                                                       rhs=v_sb[:, h * 128:(h + 1) * 128], start=True, stop=True),
                         R=[r_kz, r_v], W=[r_kbk])
                for h in range(4):
                    P.op("pool", lambda e, h=h: e.tensor_scalar(out=state[:, h * 128:(h + 1) * 128], in0=state[:, h * 128:(h + 1) * 128],
                                                                scalar1=CHUNK_DECAY[h], scalar2=None, op0=ALU.mult), R=[r_state], W=[r_state])
                P.op("dve", lambda e: e.tensor_tensor(out=state[:], in0=kbk[:], in1=state[:], op=ALU.add), R=[r_kbk, r_state], W=[r_state])
                if not is_own:
                    if need_swa_kv:
                        prev_ksT[0], prev_ksT[1] = ksT, r_ksT
                        prev_v1[0], prev_v1[1] = v1, r_v1
                    return
                if STAGE < 3:
                    return
                stats, r_st = sbp["stats"].next()
                for h in range(4):
                    P.op("dve", lambda e, h=h: e.bn_stats(out=stats[:, h, :], in_=rbk[:, h * 128:(h + 1) * 128]), R=[r_rbk], W=[r_st])
                for h in range(4):
                    P.op("dve", lambda e, h=h: e.bn_aggr(out=gmv[:, h, :], in_=stats[:, h, :]), R=[r_st], W=[r_gmv])
                P.op("act", lambda e: e.activation(out=grstd[:], in_=gmv[:, :, 1], func=AF.Sqrt, bias=1e-6, scale=1.0), R=[r_gmv], W=[r_grstd])
                P.op("dve", lambda e: e.reciprocal(out=grstd[:], in_=grstd[:]), R=[r_grstd], W=[r_grstd])
                for h in range(4):
                    P.op("dve", lambda e, h=h: e.tensor_scalar(out=concat[:, h * 128:(h + 1) * 128], in0=rbk[:, h * 128:(h + 1) * 128],
                                                               scalar1=gmv[:, h, 0:1], scalar2=grstd[:, h:h + 1], op0=ALU.subtract, op1=ALU.mult),
                         R=[r_rbk, r_gmv, r_grstd], W=[r_cc])
                P.op("pool", lambda e: e.tensor_tensor(out=concat[:, 0:512], in0=concat[:, 0:512], in1=sg[:], op=ALU.mult), R=[r_cc, r_sg], W=[r_cc])
                if STAGE < 4:
                    return
                for b0 in range(2):
                    bk, r_bk = bank()
                    for hh in range(4):
                        hq = b0 * 4 + hh
                        P.op("pe", lambda e, hq=hq, hh=hh, bk=bk: e.transpose(out=bk[:, hh * 128:(hh + 1) * 128], in_=qs_pad[:, hq, :], identity=ident[:]),
                             R=[r_qs, r_ident], W=[r_bk])
                    if b0 == 0:
                        P.op("act", lambda e, bk=bk: e.copy(out=qsT[:, 0:4, :].rearrange("p a b -> p (a b)"), in_=bk[:]), R=[r_bk], W=[r_qsT])
                    else:
                        P.op("dve", lambda e, bk=bk: e.tensor_copy(out=qsT[:, 4:8, :].rearrange("p a b -> p (a b)"), in_=bk[:]), R=[r_bk], W=[r_qsT])
                obk = [bank(), bank()]
                for kv in range(2):
                    psbs = []
                    for half in range(2):
                        kT_, r_kT_ = (prev_ksT[0], prev_ksT[1]) if half == 0 else (ksT, r_ksT)
                        bk, r_bk = bank()
                        P.op("pe", lambda e, kv=kv, kT_=kT_, bk=bk: e.matmul(out=bk[:], lhsT=kT_[:],
                                                                           rhs=qsT[:, kv * 4:(kv + 1) * 4, :].rearrange("p a b -> p (a b)"), start=True, stop=True),
                             R=[r_kT_, r_qsT], W=[r_bk])
                        psb, r_psb = p_psb.next()
                        o0 = (kv * 2 + half) * 512
                        P.op("dve", lambda e, bk=bk, psb=psb, o0=o0: e.scalar_tensor_tensor(out=psb[:], in0=bk[:], scalar=0.125, in1=btab[:, o0:o0 + 512],
                                                                                         op0=ALU.mult, op1=ALU.add),
                             R=[r_bk, r_btab], W=[r_psb])
                        P.op("act", lambda e, psb=psb: e.activation(out=psb[:], in_=psb[:], func=AF.Exp), R=[r_psb], W=[r_psb])
                        if half == 0 and i_own == 0:
                            P.op("pool", lambda e, psb=psb: e.tensor_scalar(out=psb[:], in0=psb[:], scalar1=hf_t[:, 0:1], scalar2=None, op0=ALU.mult),
                                 R=[r_psb, r_hf], W=[r_psb])
                        psbs.append((psb, r_psb))
                    ob, r_ob = obk[kv]
                    for g in range(4):
                        for half in range(2):
                            v1_, r_v1_ = (prev_v1[0], prev_v1[1]) if half == 0 else (v1, r_v1)
                            psb, r_psb = psbs[half]
                            P.op("pe", lambda e, g=g, kv=kv, half=half, v1_=v1_, psb=psb, ob=ob: e.matmul(
                                out=ob[:, g * 128:g * 128 + 66], lhsT=psb[:, g * 128:(g + 1) * 128], rhs=v1_[:, kv, 0:66],
                                start=(half == 0), stop=(half == 1)),
                                R=[r_psb, r_v1_], W=[r_ob])
                for kv in range(2):
                    ob, r_ob = obk[kv]
                    ob3 = ob[:].rearrange("p (g c) -> p g c", c=128)
                    P.op("dve", lambda e, kv=kv, ob3=ob3: e.tensor_tensor(out=den[:, kv * 4:(kv + 1) * 4], in0=ob3[:, :, 64], in1=sexp[:, kv * 4:(kv + 1) * 4], op=ALU.add),
                         R=[r_ob, r_sexp], W=[r_den])
                P.op("dve", lambda e: e.reciprocal(out=den[:], in_=den[:]), R=[r_den], W=[r_den])
                for kv in range(2):
                    ob, r_ob = obk[kv]
                    ob3 = ob[:].rearrange("p (g c) -> p g c", c=128)
                    P.op("dve", lambda e, kv=kv, ob3=ob3: e.tensor_tensor(
                        out=concat[:, 512 + kv * 256:512 + (kv + 1) * 256].rearrange("p (g d) -> p g d", d=64),
                        in0=ob3[:, :, 0:64], in1=den[:, kv * 4:(kv + 1) * 4].unsqueeze(2).to_broadcast([128, 4, 64]), op=ALU.mult),
                        R=[r_ob, r_den], W=[r_cc])
                prev_ksT[0], prev_ksT[1] = ksT, r_ksT
                prev_v1[0], prev_v1[1] = v1, r_v1
                if STAGE < 5:
                    return
                transpose_to(lambda c: concat[:, c * 128:(c + 1) * 128], 8, r_cc, hT, r_hT)
                mb = [bank(), bank()]
                for nb_ in range(2):
                    bk, r_bk = mb[nb_]
                    for c in range(8):
                        P.op("pe", lambda e, c=c, nb_=nb_, bk=bk: e.matmul(out=bk[:], lhsT=hT[:, c, :], rhs=w_out_t[:, c, nb_ * 512:(nb_ + 1) * 512],
                                                                         start=(c == 0), stop=(c == 7)),
                             R=[r_hT, r_w_out], W=[r_bk])
                for nb_ in range(2):
                    bk, r_bk = mb[nb_]
                    P.op("dve", lambda e, nb_=nb_, bk=bk: e.scalar_tensor_tensor(out=concat[:, nb_ * 512:(nb_ + 1) * 512], in0=hN[:, nb_ * 512:(nb_ + 1) * 512],
                                                                               scalar=ALPHA, in1=bk[:], op0=ALU.mult, op1=ALU.add),
                         R=[r_hN, r_bk], W=[r_cc])
                layer_norm(sbp, concat, r_cc, h2, r_h2, lng[1], r_lng[1], lnb[1], r_lnb[1])
                P.dma("sp", lambda e: e.dma_start(out=h2_dram[i_own * 128:(i_own + 1) * 128, :], in_=h2[:]), R=[r_h2], Wn=[r_h2d])
                if DEBUG:
                    P.dma("sp", lambda e: e.dma_start(out=dbg_h2[i_own * 128:(i_own + 1) * 128, :], in_=h2[:]), R=[r_h2], Wn=[r_dbg])
                if STAGE < 6:
                    return
                transpose_to(lambda c: h2[:, c * 128:(c + 1) * 128], 8, r_h2, hT, r_hT)
                lb, r_lb = bank()
                for c in range(8):
                    P.op("pe", lambda e, c=c: e.matmul(out=lb[:, 0:256], lhsT=hT[:, c, :], rhs=w_r_t[:, c, :], start=(c == 0), stop=(c == 7)),
                         R=[r_hT, r_w_r], W=[r_lb])
                P.op("act", lambda e: e.activation(out=sc[:], in_=lb[:, 0:256], func=AF.Sigmoid), R=[r_lb], W=[r_sc])
                P.op("pool", lambda e: e.tensor_tensor(out=choice[:], in0=sc[:], in1=rb_t[:], op=ALU.add), R=[r_sc, r_rb], W=[r_choice])
                def tail():
                    for g in range(8):
                        P.op("dve", lambda e, g=g: e.max(out=g8[:, g, :], in_=choice[:, g * 32:(g + 1) * 32]), R=[r_choice], W=[r_g8])
                    P.op("dve", lambda e: e.tensor_tensor(out=gs[:], in0=g8[:, :, 0], in1=g8[:, :, 1], op=ALU.add), R=[r_g8], W=[r_gs])
                    P.op("dve", lambda e: e.max(out=s8[:], in_=gs[:]), R=[r_gs], W=[r_s8])
                    P.op("dve", lambda e: e.tensor_scalar(out=pen[:], in0=gs[:], scalar1=s8[:, 3:4], scalar2=1e9, op0=ALU.is_ge, op1=ALU.mult), R=[r_gs, r_s8], W=[r_pen])
                    P.op("dve", lambda e: e.tensor_scalar(out=pen[:], in0=pen[:], scalar1=-1e9, scalar2=None, op0=ALU.add), R=[r_pen], W=[r_pen])
                    P.op("dve", lambda e: e.tensor_tensor(out=choice[:].rearrange("p (g j) -> p g j", j=32), in0=choice[:].rearrange("p (g j) -> p g j", j=32),
                                                          in1=pen[:].unsqueeze(2).to_broadcast([128, 8, 32]), op=ALU.add), R=[r_choice, r_pen], W=[r_choice])
                    P.op("dve", lambda e: e.max(out=m8[:], in_=choice[:]), R=[r_choice], W=[r_m8])
                    P.op("dve", lambda e: e.max_index(out=i8[:], in_max=m8[:], in_values=choice[:]), R=[r_choice, r_m8], W=[r_i8])
                    P.op("dve", lambda e: e.tensor_copy(out=ekf[:], in_=i8[:]), R=[r_i8], W=[r_ekf])
                    P.op("dve", lambda e: e.tensor_scalar(out=sel[:], in0=choice[:], scalar1=m8[:, 7:8], scalar2=None, op0=ALU.is_ge), R=[r_choice, r_m8], W=[r_sel])
                    P.op("dve", lambda e: e.tensor_tensor(out=wn[:], in0=sel[:], in1=sc[:], op=ALU.mult), R=[r_sel, r_sc], W=[r_wn])
                    P.op("dve", lambda e: e.reduce_sum(out=dsum[:], in_=wn[:], axis=mybir.AxisListType.X), R=[r_wn], W=[r_dsum])
                    P.op("dve", lambda e: e.reciprocal(out=dsum[:], in_=dsum[:]), R=[r_dsum], W=[r_dsum])
                    P.op("dve", lambda e: e.tensor_scalar(out=wn[:], in0=wn[:], scalar1=dsum[:, 0:1], scalar2=2.5, op0=ALU.mult, op1=ALU.mult), R=[r_wn, r_dsum], W=[r_wn])
                    pb, r_pb = bank()
                    P.op("pe", lambda e: e.matmul(out=pb[:, 0:256], lhsT=ltri[:], rhs=sel[:], start=True, stop=False), R=[r_ltri, r_sel], W=[r_pb])
                    P.op("pe", lambda e: e.matmul(out=pb[:, 0:256], lhsT=ones_t[:], rhs=selsum[:], start=False, stop=True), R=[r_ones, r_selsum], W=[r_pb])
                    P.op("pool", lambda e: e.tensor_tensor(out=selsum[:], in0=selsum[:], in1=sel[:], op=ALU.add), R=[r_selsum, r_sel], W=[r_selsum])
                    P.op("dve", lambda e: e.scalar_tensor_tensor(out=posd[:], in0=iota_t[:], scalar=float(CAP), in1=pb[:, 0:256], op0=ALU.mult, op1=ALU.add), R=[r_pb, r_iota], W=[r_posd])
                    for k in range(8):
                        P.op("dve", lambda e, k=k: e.scalar_tensor_tensor(out=junk[:], in0=iota_t[:], scalar=ekf[:, k:k + 1], in1=wn[:], op0=ALU.is_equal, op1=ALU.mult,
                                                                         accum_out=wk_all[:, i_own * 8 + k:i_own * 8 + k + 1]),
                             R=[r_iota, r_ekf, r_wn], W=[r_junk, r_wk])
                        P.op("dve", lambda e, k=k: e.scalar_tensor_tensor(out=junk[:], in0=iota_t[:], scalar=ekf[:, k:k + 1], in1=posd[:], op0=ALU.is_equal, op1=ALU.mult,
                                                                         accum_out=destf[:, k:k + 1]),
                             R=[r_iota, r_ekf, r_posd], W=[r_junk, r_destf])
                    P.op("dve", lambda e: e.tensor_copy(out=destI[:, i_own * 8:(i_own + 1) * 8], in_=destf[:]), R=[r_destf], W=[r_destI])
                    if STAGE < 7:
                        return
                    for k in range(8):
                        P.dma("pool", lambda e, k=k: e.indirect_dma_start(
                            out=list_dram, out_offset=bass.IndirectOffsetOnAxis(ap=destI[:, i_own * 8 + k:i_own * 8 + k + 1], axis=0),
                            in_=tok_t[:, i_own * 2:i_own * 2 + 2], in_offset=None, bounds_check=P.reg(e, E * CAP - 1), oob_is_err=False),
                            R=[r_destI, r_tok], Wn=[r_list])

                return tail

            def prefix_F(gt):
                xt, r_xt = p_xt.next()
                rp, r_rp = p_rp.next()
                P.dma("sp", lambda e: e.dma_start(out=xt[:], in_=x_pre[gt * 128:(gt + 1) * 128, :]), W=[r_xt])
                P.dma("act", lambda e: e.dma_start(out=rp[:], in_=rope[gt]), W=[r_rp])
                layer_norm(sbp, xt, r_xt, hN, r_hN, lng[0], r_lng[0], lnb[0], r_lnb[0], g_eng="dve", rs_eng="pool")
                transpose_to(lambda c: hN[:, c * 128:(c + 1) * 128], 8, r_hN, hT, r_hT, evac="act")
                if gt % 2 == 0:
                    kraw, r_kraw, vbuf, r_vbuf = qk_raw, r_qk_raw, v_sb, r_v
                else:
                    kraw, r_kraw, vbuf, r_vbuf = sg, r_sg, p_psb.tiles[0], p_psb.ress[0]

                def proj(col0, ncols):
                    bk, r_bk = bank()
                    for c in range(8):
                        P.op("pe", lambda e, c=c: e.matmul(out=bk[:, 0:ncols], lhsT=hT[:, c, :],
                                                           rhs=w_in_t[:, c, col0:col0 + ncols], start=(c == 0), stop=(c == 7)),
                             R=[r_hT, r_w_in], W=[r_bk])
                    return bk, r_bk

                bk, r_bk = proj(256, 256)
                P.op("act", lambda e: e.copy(out=kraw[:, 256:512], in_=bk[:, 0:256]), R=[r_bk], W=[r_kraw])
                bk, r_bk = proj(512, 512)
                P.op("act", lambda e: e.copy(out=vbuf[:], in_=bk[:]), R=[r_bk], W=[r_vbuf])
                if gt == NPRE - 1:
                    bk, r_bk = proj(2048, 256)
                    v1, r_v1 = p_v1.next()
                    P.op("act", lambda e: e.copy(out=kv_sb[:], in_=bk[:, 0:256]), R=[r_bk], W=[r_ks])
                    P.op("pool", lambda e: e.tensor_copy(out=v1[:, :, 0:64], in_=kv_sb[:, 128:256].rearrange("p (k d) -> p k d", d=64)),
                         R=[r_ks], W=[r_v1])
                    ksT, r_ksT = p_ksT.next()
                    bk, r_bk = bank()
                    P.op("pe", lambda e: e.transpose(out=bk[:, 0:128], in_=ks_sb, identity=ident[:]), R=[r_ks, r_ident], W=[r_bk])
                    P.op("act", lambda e: e.copy(out=ksT[:], in_=bk[:, 0:128]), R=[r_bk], W=[r_ksT])
                    prev_ksT[0], prev_ksT[1] = ksT, r_ksT
                    prev_v1[0], prev_v1[1] = v1, r_v1
                return (gt, rp, r_rp, kraw, r_kraw, vbuf, r_vbuf)

            def prefix_G(ctx):
                gt, rp, r_rp, kraw, r_kraw, vbuf, r_vbuf = ctx
                src4 = kraw[:, 256:512].rearrange("p (h t d) -> p h t d", t=2, d=32)
                dst4 = qk_rot[:, 256:512].rearrange("p (h t d) -> p h t d", t=2, d=32)
                cosb = rp[:, 0:32].unsqueeze(1).to_broadcast([128, 4, 32])
                sinb = rp[:, 32:64].unsqueeze(1).to_broadcast([128, 4, 32])
                ta = rt[0][:, 0:128].rearrange("p (h d) -> p h d", d=32)
                tb = rt[1][:, 0:128].rearrange("p (h d) -> p h d", d=32)
                P.op("dve", lambda e: e.tensor_tensor(out=ta, in0=src4[:, :, 0, :], in1=cosb, op=ALU.mult), R=[r_kraw, r_rp], W=[r_rt[0]])
                P.op("dve", lambda e: e.tensor_tensor(out=tb, in0=src4[:, :, 1, :], in1=sinb, op=ALU.mult), R=[r_kraw, r_rp], W=[r_rt[1]])
                P.op("dve", lambda e: e.tensor_tensor(out=dst4[:, :, 0, :], in0=ta, in1=tb, op=ALU.subtract), R=[r_rt[0], r_rt[1]], W=[r_qk_rot])
                P.op("dve", lambda e: e.tensor_tensor(out=ta, in0=src4[:, :, 0, :], in1=sinb, op=ALU.mult), R=[r_kraw, r_rp], W=[r_rt[0]])
                P.op("dve", lambda e: e.tensor_tensor(out=tb, in0=src4[:, :, 1, :], in1=cosb, op=ALU.mult), R=[r_kraw, r_rp], W=[r_rt[1]])
                P.op("dve", lambda e: e.tensor_tensor(out=dst4[:, :, 1, :], in0=ta, in1=tb, op=ALU.add), R=[r_rt[0], r_rt[1]], W=[r_qk_rot])
                for h in range(4):
                    P.op("pool", lambda e, h=h: e.tensor_scalar(out=kz[:, h, (h % 2) * 64:(h % 2 + 1) * 64], in0=qk_rot[:, 256 + h * 64:256 + (h + 1) * 64],
                                                                scalar1=pz_t[:, gt * 4 + h:gt * 4 + h + 1], scalar2=pm_t[:, gt:gt + 1], op0=ALU.mult, op1=ALU.mult),
                         R=[r_qk_rot, r_pz, r_pm], W=[r_kz])
                for h in range(4):
                    P.op("pe", lambda e, h=h: e.matmul(out=sbank[:, h * 128:(h + 1) * 128], lhsT=kz[:, h, :],
                                                       rhs=vbuf[:, h * 128:(h + 1) * 128], start=(gt == 0 and h == 0), stop=(gt == NPRE - 1)),
                         R=[r_kz, r_vbuf], W=[r_sbank])

            ctx_prev = None
            for gt in range(NPRE):
                ctx = prefix_F(gt)
                if ctx_prev is not None:
                    prefix_G(ctx_prev)
                ctx_prev = ctx
            if ctx_prev is not None:
                prefix_G(ctx_prev)
                P.op("act", lambda e: e.copy(out=state[:], in_=sbank[:]), R=[r_sbank], W=[r_state])
            nbank[0] = 8
            pending = None
            for i in range(NT):
                pending = mixer_tile(NPRE + i, True, i, pending)
            if pending is not None:
                pending()
            if DEBUG:
                P.dma("sp", lambda e: e.dma_start(out=dbg_dest, in_=destI[:]), R=[r_destI], Wn=[r_dbg])
                P.dma("sp", lambda e: e.dma_start(out=dbg_wk, in_=wk_all[:]), R=[r_wk], Wn=[r_dbg])
            P.barrier()

        with ExitStack() as stB:
            sb = mk_alloc(stB)
            p_sgu = TPool(P, sb, "sgu", [128, 2, 2048], F32, 3)
            p_sdn = TPool(P, sb, "sdn", [128, 2, 1024], F32, 3)
            p_gu = TPool(P, sb, "wgu", [128, 8, 512], F32R, 2)
            p_dn = TPool(P, sb, "wdn", [128, 2, 1024], F32R, 4)
            p_idx = TPool(P, sb, "idx", [128, 2], I32, 3)
            p_xg = TPool(P, sb, "xg", [128, 1024], F32, 3)
            p_xgT = TPool(P, sb, "xgT", [128, 8, 128], F32R, 2)
            p_sg2 = TPool(P, sb, "sg2", [128, 256], F32, 2)
            p_hh = TPool(P, sb, "hh", [128, 256], F32, 2)
            p_hhT = TPool(P, sb, "hhT", [128, 2, 128], F32R, 2)
            p_y = TPool(P, sb, "ysb", [128, 1024], F32, 2)
            lng2 = sb("lng2", [128, 1024]); r_lng2 = P.res("lng2")
            lnb2 = sb("lnb2", [128, 1024]); r_lnb2 = P.res("lnb2")
            sbp = {
                "stats": TPool(P, sb, "statsB", [128, 4, 6], F32, 2),
                "mv": TPool(P, sb, "mvB", [128, 2], F32, 2),
                "rstd": TPool(P, sb, "rstdB", [128, 1], F32, 2),
            }
            P.dma("act", lambda e: e.dma_start(out=lng2[:], in_=ln_g[2].partition_broadcast(128)), W=[r_lng2])
            P.dma("act", lambda e: e.dma_start(out=lnb2[:], in_=ln_b[2].partition_broadcast(128)), W=[r_lnb2])

            njobs = E_RUN + (NT if PHASE3 else 0)
            jobs = {}
            for i in range(3):
                P.op("pool", lambda e, i=i: e.memset(p_xg.tiles[i][:], 0.0), W=[p_xg.ress[i]])

            def st_L(j):
                c = {}
                jobs[j] = c
                xg, r_xg = p_xg.next()
                c["xg"] = (xg, r_xg)
                sgu, r_sgu = p_sgu.next()
                sdn, r_sdn = p_sdn.next()
                c["sgu"] = (sgu, r_sgu)
                c["sdn"] = (sdn, r_sdn)
                if j < E_RUN:
                    wg_, wu_, wd_ = w_gate[j], w_up[j], w_down[j]
                else:
                    wg_, wu_, wd_ = ws_gate, ws_up, ws_down
                P.dma("sp", lambda e: e.dma_start(out=sgu[:, 0, :], in_=wg_.rearrange("(p c) n -> p (c n)", c=8)), W=[r_sgu])
                P.dma("sp", lambda e: e.dma_start(out=sgu[:, 1, :], in_=wu_.rearrange("(p c) n -> p (c n)", c=8)), W=[r_sgu])
                P.dma("sp", lambda e: e.dma_start(out=sdn[:].rearrange("p c n -> p (c n)"), in_=wd_.rearrange("(p c) n -> p (c n)", c=2)), W=[r_sdn])
                if j < E_RUN:
                    idx, r_idx = p_idx.next()
                    P.dma("pool", lambda e: e.dma_start(out=idx[:], in_=list_dram[j * CAP:(j + 1) * CAP, :]), R=[r_list], W=[r_idx])
                    P.dma("pool", lambda e: e.indirect_dma_start(out=xg[:], out_offset=None, in_=h2_dram,
                                                                 in_offset=bass.IndirectOffsetOnAxis(ap=idx[:, 0:1], axis=0),
                                                                 bounds_check=P.reg(e, NT * 128 - 1), oob_is_err=False),
                          R=[r_h2d, r_idx], W=[r_xg])
                else:
                    i = j - E_RUN
                    P.dma("act", lambda e: e.dma_start(out=xg[:], in_=h2_dram[i * 128:(i + 1) * 128, :]), R=[r_h2d], W=[r_xg])

            def st_R(j):
                c = jobs[j]
                sgu, r_sgu = c["sgu"]
                sdn, r_sdn = c["sdn"]
                gu, r_gu = p_gu.next()
                dn, r_dn = p_dn.next()
                c["gu"] = (gu, r_gu)
                c["dn"] = (dn, r_dn)
                P.op("act", lambda e: e.copy(out=gu[:, :, 0:256], in_=sgu[:, 0, :].rearrange("p (c n) -> p c n", n=256)), R=[r_sgu], W=[r_gu])
                P.op("dve", lambda e: e.tensor_copy(out=gu[:, :, 256:512], in_=sgu[:, 1, :].rearrange("p (c n) -> p c n", n=256)), R=[r_sgu], W=[r_gu])
                P.op("act", lambda e: e.copy(out=dn[:, 0, :], in_=sdn[:, 0, :]), R=[r_sdn], W=[r_dn])
                P.op("dve", lambda e: e.tensor_copy(out=dn[:, 1, :], in_=sdn[:, 1, :]), R=[r_sdn], W=[r_dn])

            def st_A(j):
                c = jobs[j]
                xg, r_xg = c["xg"]
                xgT, r_xgT = p_xgT.next()
                c["xgT"] = (xgT, r_xgT)
                xv = xg[:].rearrange("s (p c) -> s c p", c=8)
                transpose_to(lambda cc: xv[:, cc, :], 8, r_xg, xgT, r_xgT)

            def st_B(j):
                c = jobs[j]
                xgT, r_xgT = c["xgT"]
                gu, r_gu = c["gu"]
                gb, r_gb = bank()
                for cc in range(8):
                    P.op("pe", lambda e, cc=cc: e.matmul(out=gb[:], lhsT=xgT[:, cc, :], rhs=gu[:, cc, :], start=(cc == 0), stop=(cc == 7)),
                         R=[r_xgT, r_gu], W=[r_gb])
                sg2, r_sg2 = p_sg2.next()
                hh, r_hh = p_hh.next()
                P.op("act", lambda e: e.activation(out=sg2[:], in_=gb[:, 0:256], func=AF.Silu), R=[r_gb], W=[r_sg2])
                P.op("dve", lambda e: e.tensor_tensor(out=hh[:], in0=gb[:, 256:512], in1=sg2[:], op=ALU.mult), R=[r_gb, r_sg2], W=[r_hh])
                c["hh"] = (hh, r_hh)

            def st_C(j):
                c = jobs[j]
                hh, r_hh = c["hh"]
                hhT, r_hhT = p_hhT.next()
                c["hhT"] = (hhT, r_hhT)
                hv = hh[:].rearrange("s (p c) -> s c p", c=2)
                transpose_to(lambda cc: hv[:, cc, :], 2, r_hh, hhT, r_hhT)

            def st_D(j):
                c = jobs[j]
                hhT, r_hhT = c["hhT"]
                dn, r_dn = c["dn"]
                yb = [bank(), bank()]
                for nb_ in range(2):
                    bk, r_bk = yb[nb_]
                    for cc in range(2):
                        P.op("pe", lambda e, cc=cc, nb_=nb_, bk=bk: e.matmul(out=bk[:], lhsT=hhT[:, cc, :], rhs=dn[:, cc, nb_ * 512:(nb_ + 1) * 512],
                                                                           start=(cc == 0), stop=(cc == 1)),
                             R=[r_hhT, r_dn], W=[r_bk])
                ysb, r_ysb = p_y.next()
                P.op("act", lambda e: e.copy(out=ysb[:, 0:512], in_=yb[0][0][:]), R=[yb[0][1]], W=[r_ysb])
                P.op("dve", lambda e: e.tensor_copy(out=ysb[:, 512:1024], in_=yb[1][0][:]), R=[yb[1][1]], W=[r_ysb])
                P.dma("pool", lambda e: e.dma_start(out=ys_dram[j * CAP:(j + 1) * CAP, :], in_=ysb[:]), R=[r_ysb], Wn=[r_ys])
                del jobs[j]

            for it in range(-4, njobs):
                for (fn, off) in ((st_L, 4), (st_R, 3), (st_A, 3), (st_B, 2), (st_C, 1), (st_D, 0)):
                    j = it + off
                    if 0 <= j < njobs:
                        fn(j)

            p_acc = TPool(P, sb, "acc", [128, 1024], F32, 2)
            p_yk = TPool(P, sb, "yk", [128, 1024], F32, 3)
            for i in range(NT if PHASE3 else 0):
                xg, r_xg = p_xg.next()
                ysh, r_ysh = p_xg.next()
                P.dma("act", lambda e: e.dma_start(out=xg[:], in_=h2_dram[i * 128:(i + 1) * 128, :]), R=[r_h2d], W=[r_xg])
                P.dma("act", lambda e: e.dma_start(out=ysh[:], in_=ys_dram[(E_RUN + i) * CAP:(E_RUN + i + 1) * CAP, :]), R=[r_ys], W=[r_ysh])
                acc, r_acc = p_acc.next()
                P.op("dve", lambda e: e.scalar_tensor_tensor(out=acc[:], in0=xg[:], scalar=ALPHA, in1=ysh[:], op0=ALU.mult, op1=ALU.add),
                     R=[r_xg, r_ysh], W=[r_acc])
                for k in range(8):
                    yk, r_yk = p_yk.next()
                    P.dma("pool", lambda e, k=k: e.indirect_dma_start(
                        out=yk[:], out_offset=None, in_=ys_dram,
                        in_offset=bass.IndirectOffsetOnAxis(ap=destI[:, i * 8 + k:i * 8 + k + 1], axis=0),
                        bounds_check=P.reg(e, E * CAP - 1), oob_is_err=False),
                        R=[r_ys, r_destI], W=[r_yk])
                    P.op("dve", lambda e, k=k: e.scalar_tensor_tensor(
                        out=acc[:], in0=yk[:], scalar=wk_all[:, i * 8 + k:i * 8 + k + 1], in1=acc[:], op0=ALU.mult, op1=ALU.add),
                        R=[r_yk, r_wk, r_acc], W=[r_acc])
                ysb, r_ysb = p_y.next()
                layer_norm(sbp, acc, r_acc, ysb, r_ysb, lng2, r_lng2, lnb2, r_lnb2, g_eng="dve", b_eng="dve")
                P.dma("sp", lambda e: e.dma_start(out=out[i * 128:(i + 1) * 128, :], in_=ysb[:]), R=[r_ysb], Wn=[r_out])
            P.wait_all("sp", [r_out, r_dbg])
            P.barrier()
        P.emit()
    return nc


def _t5_bucket(n):
    n = np.maximum(n, 0)
    ratio = np.log(np.maximum(n, 1).astype(np.float32) / np.float32(16)) / np.float32(math.log(8.0))
    large = 16 + (ratio * np.float32(16)).astype(np.int32)
    large = np.minimum(large, 31)
    return np.where(n < 16, n, large)


def _constants():
    c = {}
    c["c_ident"] = np.eye(128, dtype=np.float32)
    H = 4
    lg = np.log(1.0 - 2.0 ** (-5.0 - np.arange(H, dtype=np.float64)))
    idx = np.arange(128, dtype=np.float64)
    qk_scale = 64.0 ** -0.5
    dec = np.zeros((128, H, 128), np.float64)
    for h in range(H):
        d = idx[None, :] - idx[:, None]
        dec[:, h, :] = np.where(d >= 0, np.exp(np.maximum(d, 0) * lg[h]), 0.0) * qk_scale
    c["c_decay"] = dec.reshape(128, 512).astype(np.float32)
    zeta = np.exp((127.0 - idx)[:, None] * lg[None, :]) * qk_scale
    c["c_zeta"] = zeta.astype(np.float32)
    pz = np.zeros((128, NPRE, 4), np.float64)
    for t in range(NPRE):
        pz[:, t, :] = zeta * np.exp(128.0 * lg[None, :] * (NPRE - 1 - t))
    c["c_pz"] = pz.reshape(128, NPRE * 4).astype(np.float32)
    xi = np.exp((idx + 1.0)[None, :] * lg[:, None])
    xil = np.zeros((128, 2, 128), np.float64)
    cdl = np.zeros((128, 4, 128), np.float64)
    for h in range(H):
        po = (h % 2) * 64
        xil[po:po + 64, h // 2, :] = xi[h][None, :]
        cdl[:, h, :] = np.exp(128.0 * lg[h])
    c["c_xi"] = xil.reshape(128, 256).astype(np.float32)
    c["c_cd"] = cdl.reshape(128, 512).astype(np.float32)
    c["c_ltri"] = (np.arange(128)[:, None] < np.arange(128)[None, :]).astype(np.float32)
    c["c_iota"] = np.tile(np.arange(256, dtype=np.float32)[None, :], (128, 1))
    tok = np.zeros((128, NT, 2), np.int32)
    for i in range(NT):
        tok[:, i, :] = (i * 128 + np.arange(128))[:, None]
    c["c_tok"] = tok.reshape(128, NT * 2)
    i_ = np.arange(128)[None, :]
    j_ = np.arange(128)[:, None]
    mask = np.zeros((128, 2, 2, 4, 128), np.float32)
    bidx = np.zeros((128, 2, 128), np.int64)
    for half in range(2):
        dist = i_ + 128 - (j_ + half * 128)
        valid = (dist >= 0) & (dist < 128)
        mask[:, :, half, :, :] = np.where(valid, 0.0, NEG)[:, None, None, :]
        bidx[:, half, :] = _t5_bucket(np.clip(dist, 0, 127))
    c["c_mask"] = mask.reshape(128, 2048)
    return c, bidx


def _prepare_inputs(inp):
    consts, bidx = _constants()
    x = np.ascontiguousarray(inp["x"], dtype=np.float32)
    rel_bias = np.asarray(inp["rel_bias"], np.float32)
    rb = np.zeros((128, 2, 2, 4, 128), np.float32)
    for kv in range(2):
        for g in range(4):
            for half in range(2):
                rb[:, kv, half, g, :] = rel_bias[bidx[:, half, :], kv * 4 + g]
    consts["c_rb"] = rb.reshape(128, 2048)
    inv = 10000.0 ** (-np.arange(32, dtype=np.float32) / np.float32(32))
    shared = {
        "ln_g0": inp["ln_in_g"].reshape(1, 1024), "ln_b0": inp["ln_in_b"].reshape(1, 1024),
        "ln_g1": inp["ln_mix_g"].reshape(1, 1024), "ln_b1": inp["ln_mix_b"].reshape(1, 1024),
        "ln_g2": inp["ln_ffn_g"].reshape(1, 1024), "ln_b2": inp["ln_ffn_b"].reshape(1, 1024),
        "w_in": inp["w_in"][0], "w_out": inp["w_out"][0], "w_router": inp["w_router"][0],
        "rbias": inp["router_bias"].reshape(1, 256),
        "w_gate": inp["w_gate"][0][:max(E_RUN, 1)], "w_up": inp["w_up"][0][:max(E_RUN, 1)], "w_down": inp["w_down"][0][:max(E_RUN, 1)],
        "ws_gate": inp["ws_gate"][0], "ws_up": inp["ws_up"][0], "ws_down": inp["ws_down"][0],
        "sinks": inp["attn_sinks"].reshape(1, 8),
    }
    shared = {k: np.ascontiguousarray(v, dtype=np.float32) for k, v in shared.items()}
    shared.update(consts)
    in_maps = []
    for c in range(NCORES):
        b, q = c // 4, c % 4
        t0 = q * NT * 128
        m = dict(shared)
        m["x_own"] = x[b, t0:t0 + NT * 128]
        xp = np.zeros((NPRE * 128, 1024), np.float32)
        npre = min(t0, NPRE * 128)
        if npre:
            xp[NPRE * 128 - npre:] = x[b, t0 - npre:t0]
        m["x_pre"] = xp
        pm = np.zeros((128, NPRE), np.float32)
        pm[:, NPRE - npre // 128:] = 1.0 if npre else 0.0
        if not npre:
            pm[:] = 0.0
        m["pmask"] = pm
        m["hflag"] = np.full((128, 1), 1.0 if q > 0 else 0.0, np.float32)
        pos = (t0 - NPRE * 128 + np.arange((NPRE + NT) * 128)).astype(np.float32)
        ang = pos[:, None] * inv[None, :]
        rp = np.concatenate([np.cos(ang), np.sin(ang)], axis=1).astype(np.float32)
        m["rope"] = rp.reshape(NPRE + NT, 128, 64)
        in_maps.append(m)
    return in_maps


_NC_CACHE = {}


def kernel(**inputs):
    in_maps = _prepare_inputs(inputs)
    if "nc" not in _NC_CACHE:
        _NC_CACHE["nc"] = build_program()
    res = run_bass_kernel_spmd(_NC_CACHE["nc"], in_maps, core_ids=list(range(NCORES)))
    outs = [np.asarray(r["out"], dtype=np.float32) for r in res.results]
    full = np.stack(outs, 0).reshape(2, 4 * NT * 128, 1024)
    if DEBUG:
        kernel.dbg = res.results
    return full
```

```python
import math
import types
from contextlib import ExitStack

import numpy as np
import concourse.bass as bass
import concourse.mybir as mybir
from concourse.bass_utils import run_bass_kernel_spmd

F32 = mybir.dt.float32
F32R = mybir.dt.float32r
I32 = mybir.dt.int32
U32 = mybir.dt.uint32
AF = mybir.ActivationFunctionType
ALU = mybir.AluOpType

NCORES = 8
NT = 16
NPRE = 48
E = 256
E_RUN = 256
PHASE3 = True
VARIANT = 0
SBUF_ALIGN = 32
STAGE = 7
CAP = 128
ALPHA = 2.0 ** 0.25
NEG = -200.0
CHUNK_DECAY = [float(np.float32((1.0 - 2.0 ** (-5.0 - h)) ** 128)) for h in range(4)]
DEBUG = False


class Res:
    __slots__ = ("name", "w", "rs", "dsem", "dcount")

    def __init__(self, name):
        self.name = name
        self.w = None
        self.rs = []
        self.dsem = None
        self.dcount = 0


def _freeze(fn):
    if fn.__closure__ is None:
        return fn
    cells = []
    for c in fn.__closure__:
        try:
            cells.append(types.CellType(c.cell_contents))
        except ValueError:
            cells.append(c)
    return types.FunctionType(fn.__code__, fn.__globals__, fn.__name__, fn.__defaults__, tuple(cells))


class Prog:
    ENG = ("pe", "dve", "act", "pool", "sp")

    def __init__(self, nc, stack):
        self.nc = nc
        self.stack = stack
        self.stream = {e: [] for e in self.ENG}
        self.seq = {e: 0 for e in self.ENG}
        self.known = {e: {} for e in self.ENG}
        self.esem = {e: stack.enter_context(nc.semaphore("es_" + e)) for e in self.ENG}
        self.used = set()
        self.nd = 0
        self.allres = []
        self._regs = {}

    def reg(self, eng, val):
        key = (id(eng), val)
        if key not in self._regs:
            self._regs[key] = eng.to_reg(val)
        return self._regs[key]

    def res(self, name):
        r = Res(name)
        self.allres.append(r)
        return r

    def _need(self, eng, toks):
        kn = self.known[eng]
        for t in toks:
            if t is None:
                continue
            if t[0] == "e":
                if t[1] == "pe" and eng == "pe":
                    continue
                key = ("e", t[1])
                if kn.get(key, 0) >= t[2]:
                    continue
                kn[key] = t[2]
                self.used.add((t[1], t[2]))
                self.stream[eng].append(("we", t[1], t[2]))
            else:
                key = ("d", id(t[1]))
                if kn.get(key, 0) >= t[2]:
                    continue
                kn[key] = t[2]
                self.stream[eng].append(("wd", t[1], t[2]))

    @staticmethod
    def _deps(R, W):
        toks = []
        for r in R:
            toks.append(r.w)
        for r in W:
            toks.append(r.w)
            toks.extend(r.rs)
        return toks

    def op(self, eng, fn, R=(), W=()):
        self._need(eng, self._deps(R, W))
        self.seq[eng] += 1
        s = self.seq[eng]
        tok = ("e", eng, s)
        self.stream[eng].append(("op", _freeze(fn), s))
        for r in R:
            r.rs.append(tok)
        for r in W:
            r.w = tok
            r.rs = []
        return tok

    def dma(self, q, fn, R=(), W=(), Wn=()):
        self._need(q, self._deps(R, W))
        sr = W[0] if W else Wn[0]
        if sr.dsem is None:
            sr.dsem = self.stack.enter_context(self.nc.semaphore("ds%d" % self.nd))
            self.nd += 1
        sr.dcount += 16
        tok = ("d", sr.dsem, sr.dcount)
        self.stream[q].append(("dma", _freeze(fn), sr.dsem))
        for r in R:
            r.rs.append(tok)
        for r in W:
            r.w = tok
            r.rs = []
        for r in Wn:
            r.w = tok
        return tok

    def wait_all(self, eng, ress):
        best = {}
        for r in ress:
            for t in [r.w] + list(r.rs):
                if t is None:
                    continue
                if t[0] == "e":
                    if t[1] == "pe" and eng == "pe":
                        continue
                    key = ("e", t[1])
                else:
                    key = ("d", id(t[1]))
                if key not in best or best[key][2] < t[2]:
                    best[key] = t
        toks = [t for t in best.values()]
        for i in range(0, len(toks), 3):
            self._need(eng, toks[i:i + 3])
            if i + 3 < len(toks):
                self.seq[eng] += 1
                self.stream[eng].append(("op", (lambda e: e.nop()), self.seq[eng]))

    def barrier(self):
        self.wait_all("sp", self.allres)
        tok = self.op("sp", lambda e: e.nop())
        for e in self.ENG:
            if e != "sp":
                self._need(e, [tok])

    def emit(self):
        nc = self.nc
        val = {}
        for e in self.ENG:
            c = 0
            m = {}
            for it in self.stream[e]:
                if it[0] == "op" and (e, it[2]) in self.used:
                    c += 1
                    m[it[2]] = c
            val[e] = m
        engobj = {"pe": "tensor", "dve": "vector", "act": "scalar", "pool": "gpsimd", "sp": "sync"}
        esem = self.esem
        used = self.used
        with nc.Block() as block:
            for e in self.ENG:
                items = self.stream[e]

                def body(eng, items=items, e=e):
                    for it in items:
                        k = it[0]
                        if k == "we":
                            eng.wait_ge(esem[it[1]], val[it[1]][it[2]])
                        elif k == "wd":
                            eng.wait_ge(it[1], it[2])
                        elif k == "op":
                            ins = it[1](eng)
                            if (e, it[2]) in used:
                                ins.then_inc(esem[e], 1)
                        else:
                            it[1](eng).then_inc(it[2], 16)
                getattr(block, engobj[e])(body)


class TPool:
    def __init__(self, P, alloc, name, shape, dt, n):
        self.tiles = [alloc("%s_%d" % (name, i), shape, dt) for i in range(n)]
        self.ress = [P.res("%s_%d" % (name, i)) for i in range(n)]
        self.i = 0

    def next(self):
        k = self.i % len(self.tiles)
        self.i += 1
        return self.tiles[k], self.ress[k]


def build_program():
    nc = bass.Bass("TRN2", target_bir_lowering=False)

    def din(name, shape, dt=F32):
        return nc.dram_tensor(name, list(shape), dt, kind="ExternalInput").ap()

    x_own = din("x_own", [NT * 128, 1024])
    x_pre = din("x_pre", [NPRE * 128, 1024])
    rope = din("rope", [NPRE + NT, 128, 64])
    pmask = din("pmask", [128, NPRE])
    hflag = din("hflag", [128, 1])
    ln_g = [din("ln_g%d" % i, [1, 1024]) for i in range(3)]
    ln_b = [din("ln_b%d" % i, [1, 1024]) for i in range(3)]
    w_in = din("w_in", [1024, 2304])
    w_out = din("w_out", [1024, 1024])
    w_router = din("w_router", [1024, 256])
    rbias = din("rbias", [1, 256])
    w_gate = din("w_gate", [max(E_RUN, 1), 1024, 256])
    w_up = din("w_up", [max(E_RUN, 1), 1024, 256])
    w_down = din("w_down", [max(E_RUN, 1), 256, 1024])
    ws_gate = din("ws_gate", [1024, 256])
    ws_up = din("ws_up", [1024, 256])
    ws_down = din("ws_down", [256, 1024])
    sinks = din("sinks", [1, 8])
    c_ident = din("c_ident", [128, 128])
    c_decay = din("c_decay", [128, 512])
    c_zeta = din("c_zeta", [128, 4])
    c_pz = din("c_pz", [128, NPRE * 4])
    c_xi = din("c_xi", [128, 256])
    c_cd = din("c_cd", [128, 512])
    c_rb = din("c_rb", [128, 2048])
    c_mask = din("c_mask", [128, 2048])
    c_ltri = din("c_ltri", [128, 128])
    c_iota = din("c_iota", [128, 256])
    c_tok = din("c_tok", [128, NT * 2], I32)

    out = nc.dram_tensor("out", [NT * 128, 1024], F32, kind="ExternalOutput").ap()
    h2_dram = nc.dram_tensor("h2_dram", [NT * 128, 1024], F32, kind="Internal").ap()
    ys_dram = nc.dram_tensor("ys_dram", [(E + NT) * CAP, 1024], F32, kind="Internal").ap()
    list_dram = nc.dram_tensor("list_dram", [E * CAP, 2], I32, kind="Internal").ap()
    if DEBUG:
        dbg_h2 = nc.dram_tensor("dbg_h2", [NT * 128, 1024], F32, kind="ExternalOutput").ap()
        dbg_dest = nc.dram_tensor("dbg_dest", [128, NT * 8], I32, kind="ExternalOutput").ap()
        dbg_wk = nc.dram_tensor("dbg_wk", [128, NT * 8], F32, kind="ExternalOutput").ap()

    with ExitStack() as st0:
        P = Prog(nc, st0)

        cur = [16481]
        npad = [0]
        stack_marks = []

        def mk_alloc(st):
            base_at_entry = cur[0]
            st.callback(lambda: cur.__setitem__(0, base_at_entry))

            def sb(name, shape, dt=F32):
                a32 = (cur[0] + 31) // 32 * 32
                if a32 % SBUF_ALIGN:
                    npad[0] += 1
                    st.enter_context(nc.sbuf_tensor("pad%d" % npad[0], [128, 1], mybir.dt.uint8))
                    a32 += 32
                nbytes = int(np.prod(shape[1:])) * mybir.dt.size(dt)
                cur[0] = a32 + nbytes
                return st.enter_context(nc.sbuf_tensor(name, list(shape), dt))
            return sb

        sb0 = mk_alloc(st0)
        banks = [st0.enter_context(nc.psum_tensor("bank%d" % i, [128, 512], F32)) for i in range(8)]
        bres = [P.res("bank%d" % i) for i in range(8)]
        bctr = [0]

        nbank = [7]

        def bank():
            k = bctr[0] % nbank[0]
            bctr[0] += 1
            return banks[k], bres[k]

        sbank, r_sbank = banks[7], bres[7]

        ident = sb0("ident", [128, 128]); r_ident = P.res("ident")
        destI = sb0("destI", [128, NT * 8], I32); r_destI = P.res("destI")
        wk_all = sb0("wk_all", [128, NT * 8]); r_wk = P.res("wk_all")
        r_h2d = P.res("h2_dram"); r_ys = P.res("ys_dram"); r_list = P.res("list_dram"); r_out = P.res("out")
        r_dbg = P.res("dbg")
        P.dma("sp", lambda e: e.dma_start(out=ident[:], in_=c_ident), W=[r_ident])

        mhalf = sb0("mhalf", [128, 1]); r_mhalf = P.res("mhalf")
        P.op("pool", lambda e: e.memset(mhalf[:], -0.5), W=[r_mhalf])

        def ln_stats(sbp, src, r_src, eps, width=1024, rs_eng="act"):
            stats, r_st = sbp["stats"].next()
            mv, r_mv = sbp["mv"].next()
            rstd, r_rstd = sbp["rstd"].next()
            nchunk = width // 512
            for c in range(nchunk):
                P.op("dve", lambda e, c=c: e.bn_stats(out=stats[:, c, :], in_=src[:, c * 512:(c + 1) * 512]),
                     R=[r_src], W=[r_st])
            P.op("dve", lambda e: e.bn_aggr(out=mv[:], in_=stats[:, 0:nchunk, :].rearrange("p a b -> p (a b)")),
                 R=[r_st], W=[r_mv])
            if rs_eng == "pool":
                P.op("pool", lambda e: e.tensor_scalar(out=rstd[:], in0=mv[:, 1:2], scalar1=eps, scalar2=None, op0=ALU.add),
                     R=[r_mv], W=[r_rstd])
                P.op("pool", lambda e: e.tensor_tensor(out=rstd[:], in0=rstd[:], in1=mhalf[:], op=ALU.pow),
                     R=[r_rstd, r_mhalf], W=[r_rstd])
                return mv, r_mv, rstd, r_rstd
            P.op("act", lambda e: e.activation(out=rstd[:], in_=mv[:, 1:2], func=AF.Sqrt, bias=eps, scale=1.0),
                 R=[r_mv], W=[r_rstd])
            P.op("dve", lambda e: e.reciprocal(out=rstd[:], in_=rstd[:]), R=[r_rstd], W=[r_rstd])
            return mv, r_mv, rstd, r_rstd

        def layer_norm(sbp, src, r_src, dst, r_dst, g_t, r_g, b_t, r_b, g_eng="pool", rs_eng="act"):
            mv, r_mv, rstd, r_rstd = ln_stats(sbp, src, r_src, 1e-5, rs_eng=rs_eng)
            P.op("dve", lambda e: e.tensor_scalar(out=dst[:], in0=src[:], scalar1=mv[:, 0:1], scalar2=rstd[:, 0:1],
                                                  op0=ALU.subtract, op1=ALU.mult),
                 R=[r_src, r_mv, r_rstd], W=[r_dst])
            P.op(g_eng, lambda e: e.tensor_tensor(out=dst[:], in0=dst[:], in1=g_t[:], op=ALU.mult),
                 R=[r_dst, r_g], W=[r_dst])
            P.op("pool", lambda e: e.tensor_tensor(out=dst[:], in0=dst[:], in1=b_t[:], op=ALU.add),
                 R=[r_dst, r_b], W=[r_dst])

        def transpose_to(src_fn, nchunks, r_src, dstT, r_dstT, rows=128, evac="mix"):
            for b0 in range(0, nchunks, 4):
                bk, r_bk = bank()
                nb = min(4, nchunks - b0)
                for c in range(b0, b0 + nb):
                    P.op("pe", lambda e, c=c, bk=bk, b0=b0: e.transpose(
                        out=bk[0:rows, (c - b0) * 128:(c - b0 + 1) * 128], in_=src_fn(c), identity=ident[:]),
                        R=[r_src, r_ident], W=[r_bk])
                eng = "act" if ((b0 // 4) % 2 == 0 or evac == "act") else "dve"
                if eng == "act":
                    P.op("act", lambda e, bk=bk, b0=b0, nb=nb: e.copy(
                        out=dstT[0:rows, b0:b0 + nb, :].rearrange("p a b -> p (a b)"), in_=bk[0:rows, 0:nb * 128]),
                        R=[r_bk], W=[r_dstT])
                else:
                    P.op("dve", lambda e, bk=bk, b0=b0, nb=nb: e.tensor_copy(
                        out=dstT[0:rows, b0:b0 + nb, :].rearrange("p a b -> p (a b)"), in_=bk[0:rows, 0:nb * 128]),
                        R=[r_bk], W=[r_dstT])

        with ExitStack() as stA:
            sb = mk_alloc(stA)
            w_in_t = sb("w_in_t", [128, 8, 2304], F32R); r_w_in = P.res("w_in")
            w_out_t = sb("w_out_t", [128, 8, 1024], F32R); r_w_out = P.res("w_out")
            w_r_t = sb("w_r_t", [128, 8, 256], F32R); r_w_r = P.res("w_r")
            lng = [sb("lng%d" % i, [128, 1024]) for i in range(2)]
            lnb = [sb("lnb%d" % i, [128, 1024]) for i in range(2)]
            r_lng = [P.res("lng%d" % i) for i in range(2)]
            r_lnb = [P.res("lnb%d" % i) for i in range(2)]
            decay_t = sb("decay_t", [128, 512]); r_decay = P.res("decay")
            zeta_t = sb("zeta_t", [128, 4]); r_zeta = P.res("zeta")
            pz_t = sb("pz_t", [128, NPRE * 4]); r_pz = P.res("pz")
            xi_t = sb("xi_t", [128, 256]); r_xi = P.res("xi")
            btab = sb("btab", [128, 2048]); r_btab = P.res("btab")
            ltri = sb("ltri", [128, 128]); r_ltri = P.res("ltri")
            ones_t = sb("ones_t", [128, 128]); r_ones = P.res("ones")
            iota_t = sb("iota_t", [128, 256]); r_iota = P.res("iota")
            rb_t = sb("rb_t", [128, 256]); r_rb = P.res("rbias")
            tok_t = sb("tok_t", [128, NT * 2], I32); r_tok = P.res("tok")
            pm_t = sb("pm_t", [128, NPRE]); r_pm = P.res("pm")
            hf_t = sb("hf_t", [128, 1]); r_hf = P.res("hf")
            sexp = sb("sexp", [128, 8]); r_sexp = P.res("sexp")
            state = sb("state", [128, 512]); r_state = P.res("state")
            selsum = sb("selsum", [128, 256]); r_selsum = P.res("selsum")

            sbp = {
                "stats": TPool(P, sb, "stats", [128, 4, 6], F32, 2),
                "mv": TPool(P, sb, "mv", [128, 2], F32, 2),
                "rstd": TPool(P, sb, "rstd", [128, 1], F32, 2),
            }
            p_xt = TPool(P, sb, "xt", [128, 1024], F32, 2)
            p_rp = TPool(P, sb, "rp", [128, 64], F32, 2)
            hN, r_hN = sb("hN", [128, 1024]), P.res("hN")
            hT, r_hT = sb("hT", [128, 8, 128], F32R), P.res("hT")
            qk_raw, r_qk_raw = sb("qk_raw", [128, 512]), P.res("qk_raw")
            qk_rot, r_qk_rot = sb("qk_rot", [128, 512]), P.res("qk_rot")
            rt = [sb("rt%d" % i, [128, 256]) for i in range(2)]
            r_rt = [P.res("rt%d" % i) for i in range(2)]
            v_sb, r_v = sb("v_sb", [128, 512]), P.res("v_sb")
            sg, r_sg = sb("sg", [128, 512]), P.res("sg")
            qs_pad, r_qs = sb("qs_pad", [128, 8, 128]), P.res("qs_pad")
            kv_sb, r_ks = sb("kv_sb", [128, 256]), P.res("kv_sb")
            ks_sb = kv_sb[:, 0:128]
            if VARIANT >= 100:
                spacer = sb("spacer", [128, 64])
            p_v1 = TPool(P, sb, "v1", [128, 2, 128], F32, 2)
            qT, r_qT = sb("qT", [128, 2, 128]), P.res("qT")
            krm, r_krm = sb("krm", [128, 4, 128]), P.res("krm")
            kTm, r_kTm = sb("kTm", [128, 4, 128]), P.res("kTm")
            qxT, r_qxT = sb("qxT", [128, 256]), P.res("qxT")
            scT, r_scT = qk_raw, r_qk_raw
            kz, r_kz = sb("kz", [128, 4, 128]), P.res("kz")
            gmv, r_gmv = sb("gmv", [128, 4, 2]), P.res("gmv")
            grstd, r_grstd = sb("grstd", [128, 4]), P.res("grstd")
            concat, r_cc = sb("concat", [128, 1024]), P.res("concat")
            qsT, r_qsT = sb("qsT", [128, 8, 128]), P.res("qsT")
            p_ksT = TPool(P, sb, "ksT", [128, 128], F32, 2)
            p_psb = TPool(P, sb, "psb", [128, 512], F32, 2)
            den, r_den = sb("den", [128, 8]), P.res("den")
            h2, r_h2 = hN, r_hN
            sc, r_sc = sb("sc", [128, 256]), P.res("sc")
            choice, r_choice = sb("choice", [128, 256]), P.res("choice")
            g8, r_g8 = sb("g8", [128, 8, 8]), P.res("g8")
            gs, r_gs = sb("gs", [128, 8]), P.res("gs")
            s8, r_s8 = sb("s8", [128, 8]), P.res("s8")
            pen, r_pen = sb("pen", [128, 8]), P.res("pen")
            m8, r_m8 = sb("m8", [128, 8]), P.res("m8")
            i8, r_i8 = sb("i8", [128, 8], U32), P.res("i8")
            ekf, r_ekf = sb("ekf", [128, 8]), P.res("ekf")
            sel, r_sel = sb("sel", [128, 256]), P.res("sel")
            wn, r_wn = sc, r_sc
            dsum, r_dsum = sb("dsum", [128, 1]), P.res("dsum")
            posd, r_posd = sel, r_sel
            junk, r_junk = choice, r_choice
            destf, r_destf = sb("destf", [128, 8]), P.res("destf")
            lfill, r_lfill = qk_raw[:].bitcast(I32), r_qk_raw

            for c8 in range(8):
                P.dma("pool", lambda e, c8=c8: e.dma_start(out=w_in_t[:, c8, :], in_=w_in[c8 * 128:(c8 + 1) * 128, :], max_dma_last_dim=4096), W=[r_w_in])
            P.dma("pool", lambda e: e.dma_start(out=w_out_t[:], in_=w_out.rearrange("(c p) n -> p c n", p=128)), W=[r_w_out])
            P.dma("pool", lambda e: e.dma_start(out=w_r_t[:], in_=w_router.rearrange("(c p) n -> p c n", p=128)), W=[r_w_r])
            for i in range(2):
                P.dma("sp", lambda e, i=i: e.dma_start(out=lng[i][:], in_=ln_g[i].partition_broadcast(128)), W=[r_lng[i]])
                P.dma("sp", lambda e, i=i: e.dma_start(out=lnb[i][:], in_=ln_b[i].partition_broadcast(128)), W=[r_lnb[i]])
            for (t_, r_, src) in ((decay_t, r_decay, c_decay), (zeta_t, r_zeta, c_zeta), (pz_t, r_pz, c_pz), (xi_t, r_xi, c_xi),
                                  (btab, r_btab, c_rb), (ltri, r_ltri, c_ltri),
                                  (iota_t, r_iota, c_iota), (tok_t, r_tok, c_tok),
                                  (pm_t, r_pm, pmask), (hf_t, r_hf, hflag)):
                P.dma("act", lambda e, t_=t_, src=src: e.dma_start(out=t_[:], in_=src), W=[r_])
            P.dma("act", lambda e: e.dma_start(out=rb_t[:], in_=rbias.partition_broadcast(128)), W=[r_rb])
            P.dma("act", lambda e: e.dma_start(out=sexp[:], in_=sinks.partition_broadcast(128)), W=[r_sexp])
            P.op("act", lambda e: e.activation(out=sexp[:], in_=sexp[:], func=AF.Exp), R=[r_sexp], W=[r_sexp])
            for q4 in range(2):
                mt, r_mt = p_xt.next()
                P.dma("sp", lambda e, mt=mt, q4=q4: e.dma_start(out=mt[:], in_=c_mask[:, q4 * 1024:(q4 + 1) * 1024]), W=[r_mt])
                P.op("pool", lambda e, mt=mt, q4=q4: e.tensor_tensor(out=btab[:, q4 * 1024:(q4 + 1) * 1024],
                                                                     in0=btab[:, q4 * 1024:(q4 + 1) * 1024], in1=mt[:], op=ALU.add),
                     R=[r_mt, r_btab], W=[r_btab])
            P.op("pool", lambda e: e.memset(ones_t[:], 1.0), W=[r_ones])
            P.op("pool", lambda e: e.memset(state[:], 0.0), W=[r_state])
            P.op("pool", lambda e: e.memset(selsum[:], 0.0), W=[r_selsum])
            P.op("pool", lambda e: e.memset(krm[:], 0.0), W=[r_krm])
            P.op("pool", lambda e: e.memset(kz[:], 0.0), W=[r_kz])
            P.op("pool", lambda e: e.memset(qs_pad[:], 0.0), W=[r_qs])
            for i in range(2):
                P.op("pool", lambda e, i=i: e.memset(p_v1.tiles[i][:], 0.0), W=[p_v1.ress[i]])
                P.op("pool", lambda e, i=i: e.memset(p_v1.tiles[i][:, :, 64:65], 1.0), W=[p_v1.ress[i]])
            P.op("pool", lambda e: e.memset(lfill, NT * 128), W=[r_lfill])
            P.dma("pool", lambda e: e.dma_start(out=list_dram.rearrange("(p r) c -> p (r c)", p=128), in_=lfill),
                  R=[r_lfill], W=[r_list])
            P.wait_all("pool", [r_list])

            prev_ksT = [None, None]
            prev_v1 = [None, None]

            def mixer_tile(gt, is_own, i_own, pending=None):
                xsrc = x_own if is_own else x_pre
                ti = i_own if is_own else gt
                xt, r_xt = p_xt.next()
                rp, r_rp = p_rp.next()
                P.dma("sp", lambda e: e.dma_start(out=xt[:], in_=xsrc[ti * 128:(ti + 1) * 128, :]), W=[r_xt])
                P.dma("act", lambda e: e.dma_start(out=rp[:], in_=rope[gt]), W=[r_rp])
                layer_norm(sbp, xt, r_xt, hN, r_hN, lng[0], r_lng[0], lnb[0], r_lnb[0])
                transpose_to(lambda c: hN[:, c * 128:(c + 1) * 128], 8, r_hN, hT, r_hT)
                if STAGE < 2:
                    return
                need_swa_kv = is_own or gt == NPRE - 1

                def proj(col0, ncols):
                    bk, r_bk = bank()
                    for c in range(8):
                        P.op("pe", lambda e, c=c: e.matmul(out=bk[:, 0:ncols], lhsT=hT[:, c, :],
                                                           rhs=w_in_t[:, c, col0:col0 + ncols], start=(c == 0), stop=(c == 7)),
                             R=[r_hT, r_w_in], W=[r_bk])
                    return bk, r_bk

                if is_own:
                    bk, r_bk = proj(0, 512)
                    P.op("act", lambda e: e.copy(out=qk_raw[:], in_=bk[:]), R=[r_bk], W=[r_qk_raw])
                    lo, nh = 0, 8
                else:
                    bk, r_bk = proj(256, 256)
                    P.op("act", lambda e: e.copy(out=qk_raw[:, 256:512], in_=bk[:, 0:256]), R=[r_bk], W=[r_qk_raw])
                    lo, nh = 256, 4
                src4 = qk_raw[:, lo:512].rearrange("p (h t d) -> p h t d", t=2, d=32)
                dst4 = qk_rot[:, lo:512].rearrange("p (h t d) -> p h t d", t=2, d=32)
                cosb = rp[:, 0:32].unsqueeze(1).to_broadcast([128, nh, 32])
                sinb = rp[:, 32:64].unsqueeze(1).to_broadcast([128, nh, 32])
                ta = rt[0][:, 0:nh * 32].rearrange("p (h d) -> p h d", d=32)
                tb = rt[1][:, 0:nh * 32].rearrange("p (h d) -> p h d", d=32)
                P.op("pool", lambda e: e.tensor_tensor(out=ta, in0=src4[:, :, 0, :], in1=cosb, op=ALU.mult), R=[r_qk_raw, r_rp], W=[r_rt[0]])
                P.op("dve", lambda e: e.tensor_tensor(out=tb, in0=src4[:, :, 1, :], in1=sinb, op=ALU.mult), R=[r_qk_raw, r_rp], W=[r_rt[1]])
                P.op("dve", lambda e: e.tensor_tensor(out=dst4[:, :, 0, :], in0=ta, in1=tb, op=ALU.subtract), R=[r_rt[0], r_rt[1]], W=[r_qk_rot])
                P.op("pool", lambda e: e.tensor_tensor(out=ta, in0=src4[:, :, 0, :], in1=sinb, op=ALU.mult), R=[r_qk_raw, r_rp], W=[r_rt[0]])
                P.op("dve", lambda e: e.tensor_tensor(out=tb, in0=src4[:, :, 1, :], in1=cosb, op=ALU.mult), R=[r_qk_raw, r_rp], W=[r_rt[1]])
                P.op("dve", lambda e: e.tensor_tensor(out=dst4[:, :, 1, :], in0=ta, in1=tb, op=ALU.add), R=[r_rt[0], r_rt[1]], W=[r_qk_rot])
                bk, r_bk = proj(512, 512)
                P.op("act", lambda e: e.copy(out=v_sb[:], in_=bk[:]), R=[r_bk], W=[r_v])
                for h in range(4):
                    if is_own:
                        P.op("pool", lambda e, h=h: e.tensor_scalar(out=kz[:, h, (h % 2) * 64:(h % 2 + 1) * 64], in0=qk_rot[:, 256 + h * 64:256 + (h + 1) * 64],
                                                                    scalar1=zeta_t[:, h:h + 1], scalar2=None, op0=ALU.mult),
                             R=[r_qk_rot, r_zeta], W=[r_kz])
                    else:
                        P.op("pool", lambda e, h=h: e.tensor_scalar(out=kz[:, h, (h % 2) * 64:(h % 2 + 1) * 64], in0=qk_rot[:, 256 + h * 64:256 + (h + 1) * 64],
                                                                    scalar1=zeta_t[:, h:h + 1], scalar2=pm_t[:, gt:gt + 1], op0=ALU.mult, op1=ALU.mult),
                             R=[r_qk_rot, r_zeta, r_pm], W=[r_kz])
                if STAGE < 2.1:
                    return
                if is_own:
                    bk, r_bk = proj(1024, 512)
                    P.op("act", lambda e: e.activation(out=sg[:], in_=bk[:], func=AF.Silu), R=[r_bk], W=[r_sg])
                    if STAGE < 2.12:
                        return
                    bk, r_bk = proj(1536, 512)
                    for kv in range(2):
                        P.op("act", lambda e, kv=kv: e.copy(out=qs_pad[:, kv * 4:(kv + 1) * 4, kv * 64:(kv + 1) * 64],
                                                            in_=bk[:, kv * 256:(kv + 1) * 256].rearrange("p (g d) -> p g d", d=64)),
                             R=[r_bk], W=[r_qs])
                if STAGE < 2.13:
                    return
                if need_swa_kv and STAGE >= 2.2:
                    bk, r_bk = proj(2048, 256)
                    v1, r_v1 = p_v1.next()
                    P.op("act", lambda e: e.copy(out=kv_sb[:], in_=bk[:, 0:256]), R=[r_bk], W=[r_ks])
                    P.op("pool", lambda e: e.tensor_copy(out=v1[:, :, 0:64], in_=kv_sb[:, 128:256].rearrange("p (k d) -> p k d", d=64)),
                         R=[r_ks], W=[r_v1])
                    ksT, r_ksT = p_ksT.next()
                    bk, r_bk = bank()
                    if VARIANT == 1:
                        bk, r_bk = bank()
                    if VARIANT != 2:
                        P.op("pe", lambda e: e.transpose(out=bk[:, 0:128], in_=ks_sb, identity=ident[:]), R=[r_ks, r_ident], W=[r_bk])
                    if VARIANT == 3:
                        P.op("dve", lambda e: e.tensor_copy(out=ksT[:], in_=bk[:, 0:128]), R=[r_bk], W=[r_ksT])
                    elif VARIANT != 4:
                        P.op("act", lambda e: e.copy(out=ksT[:], in_=bk[:, 0:128]), R=[r_bk], W=[r_ksT])
                if pending is not None:
                    pending()
                if is_own and STAGE >= 2.3:
                    for hp in range(2):
                        P.op("pool", lambda e, hp=hp: e.tensor_copy(
                            out=krm[:].rearrange("p (a b) c -> p a b c", b=2)[:, :, hp, hp * 64:(hp + 1) * 64],
                            in_=qk_rot[:, 256:512].rearrange("p (a b d) -> p a b d", b=2, d=64)[:, :, hp, :]),
                            R=[r_qk_rot], W=[r_krm])
                    transpose_to(lambda c: qk_rot[:, c * 128:(c + 1) * 128], 2, r_qk_rot, qT, r_qT)
                    transpose_to(lambda c: krm[:, c, :], 4, r_krm, kTm, r_kTm)
                    P.op("pool", lambda e: e.tensor_tensor(out=qxT[:], in0=qT[:].rearrange("p a b -> p (a b)"), in1=xi_t[:], op=ALU.mult),
                         R=[r_qT, r_xi], W=[r_qxT])
                    bk, r_bk = bank()
                    for h in range(4):
                        P.op("pe", lambda e, h=h: e.matmul(out=bk[:, h * 128:(h + 1) * 128], lhsT=kTm[:, h, :],
                                                           rhs=qT[:, h // 2, :], start=True, stop=True),
                             R=[r_kTm, r_qT], W=[r_bk])
                    P.op("dve", lambda e: e.tensor_tensor(out=scT[:], in0=bk[:], in1=decay_t[:], op=ALU.mult), R=[r_bk, r_decay], W=[r_scT])
                    rbk, r_rbk = bank()
                    for h in range(4):
                        P.op("pe", lambda e, h=h: e.matmul(out=rbk[:, h * 128:(h + 1) * 128], lhsT=scT[:, h * 128:(h + 1) * 128],
                                                           rhs=v_sb[:, h * 128:(h + 1) * 128], start=True, stop=False),
                             R=[r_scT, r_v], W=[r_rbk])
                        P.op("pe", lambda e, h=h: e.matmul(out=rbk[:, h * 128:(h + 1) * 128], lhsT=qxT[:, (h // 2) * 128:(h // 2 + 1) * 128],
                                                           rhs=state[:, h * 128:(h + 1) * 128], start=False, stop=True),
                             R=[r_qxT, r_state], W=[r_rbk])
                if STAGE < 2.4:
                    return
                kbk, r_kbk = bank()
                for h in range(4):
                    P.op("pe", lambda e, h=h: e.matmul(out=kbk[:, h * 128:(h + 1) * 128], lhsT=kz[:, h, :],
                                                       rhs=v_sb[:, h * 128:(h + 1) * 128], start=True, stop=True),
                         R=[r_kz, r_v], W=[r_kbk])
                for h in range(4):
                    P.op("pool", lambda e, h=h: e.tensor_scalar(out=state[:, h * 128:(h + 1) * 128], in0=state[:, h * 128:(h + 1) * 128],
                                                                scalar1=CHUNK_DECAY[h], scalar2=None, op0=ALU.mult), R=[r_state], W=[r_state])
                P.op("dve", lambda e: e.tensor_tensor(out=state[:], in0=kbk[:], in1=state[:], op=ALU.add), R=[r_kbk, r_state], W=[r_state])
                if not is_own:
                    if need_swa_kv:
                        prev_ksT[0], prev_ksT[1] = ksT, r_ksT
                        prev_v1[0], prev_v1[1] = v1, r_v1
                    return
                if STAGE < 3:
                    return
                stats, r_st = sbp["stats"].next()
                for h in range(4):
                    P.op("dve", lambda e, h=h: e.bn_stats(out=stats[:, h, :], in_=rbk[:, h * 128:(h + 1) * 128]), R=[r_rbk], W=[r_st])
                for h in range(4):
                    P.op("dve", lambda e, h=h: e.bn_aggr(out=gmv[:, h, :], in_=stats[:, h, :]), R=[r_st], W=[r_gmv])
                P.op("act", lambda e: e.activation(out=grstd[:], in_=gmv[:, :, 1], func=AF.Sqrt, bias=1e-6, scale=1.0), R=[r_gmv], W=[r_grstd])
                P.op("dve", lambda e: e.reciprocal(out=grstd[:], in_=grstd[:]), R=[r_grstd], W=[r_grstd])
                for h in range(4):
                    P.op("dve", lambda e, h=h: e.tensor_scalar(out=concat[:, h * 128:(h + 1) * 128], in0=rbk[:, h * 128:(h + 1) * 128],
                                                               scalar1=gmv[:, h, 0:1], scalar2=grstd[:, h:h + 1], op0=ALU.subtract, op1=ALU.mult),
                         R=[r_rbk, r_gmv, r_grstd], W=[r_cc])
                P.op("pool", lambda e: e.tensor_tensor(out=concat[:, 0:512], in0=concat[:, 0:512], in1=sg[:], op=ALU.mult), R=[r_cc, r_sg], W=[r_cc])
                if STAGE < 4:
                    return
                for b0 in range(2):
                    bk, r_bk = bank()
                    for hh in range(4):
                        hq = b0 * 4 + hh
                        P.op("pe", lambda e, hq=hq, hh=hh, bk=bk: e.transpose(out=bk[:, hh * 128:(hh + 1) * 128], in_=qs_pad[:, hq, :], identity=ident[:]),
                             R=[r_qs, r_ident], W=[r_bk])
                    if b0 == 0:
                        P.op("act", lambda e, bk=bk: e.copy(out=qsT[:, 0:4, :].rearrange("p a b -> p (a b)"), in_=bk[:]), R=[r_bk], W=[r_qsT])
                    else:
                        P.op("dve", lambda e, bk=bk: e.tensor_copy(out=qsT[:, 4:8, :].rearrange("p a b -> p (a b)"), in_=bk[:]), R=[r_bk], W=[r_qsT])
                obk = [bank(), bank()]
                for kv in range(2):
                    psbs = []
                    for half in range(2):
                        kT_, r_kT_ = (prev_ksT[0], prev_ksT[1]) if half == 0 else (ksT, r_ksT)
                        bk, r_bk = bank()
                        P.op("pe", lambda e, kv=kv, kT_=kT_, bk=bk: e.matmul(out=bk[:], lhsT=kT_[:],
                                                                           rhs=qsT[:, kv * 4:(kv + 1) * 4, :].rearrange("p a b -> p (a b)"), start=True, stop=True),
                             R=[r_kT_, r_qsT], W=[r_bk])
                        psb, r_psb = p_psb.next()
                        o0 = (kv * 2 + half) * 512
                        P.op("dve", lambda e, bk=bk, psb=psb, o0=o0: e.scalar_tensor_tensor(out=psb[:], in0=bk[:], scalar=0.125, in1=btab[:, o0:o0 + 512],
                                                                                         op0=ALU.mult, op1=ALU.add),
                             R=[r_bk, r_btab], W=[r_psb])
                        P.op("act", lambda e, psb=psb: e.activation(out=psb[:], in_=psb[:], func=AF.Exp), R=[r_psb], W=[r_psb])
                        if half == 0 and i_own == 0:
                            P.op("pool", lambda e, psb=psb: e.tensor_scalar(out=psb[:], in0=psb[:], scalar1=hf_t[:, 0:1], scalar2=None, op0=ALU.mult),
                                 R=[r_psb, r_hf], W=[r_psb])
                        psbs.append((psb, r_psb))
                    ob, r_ob = obk[kv]
                    for g in range(4):
                        for half in range(2):
                            v1_, r_v1_ = (prev_v1[0], prev_v1[1]) if half == 0 else (v1, r_v1)
                            psb, r_psb = psbs[half]
                            P.op("pe", lambda e, g=g, kv=kv, half=half, v1_=v1_, psb=psb, ob=ob: e.matmul(
                                out=ob[:, g * 128:g * 128 + 66], lhsT=psb[:, g * 128:(g + 1) * 128], rhs=v1_[:, kv, 0:66],
                                start=(half == 0), stop=(half == 1)),
                                R=[r_psb, r_v1_], W=[r_ob])
                for kv in range(2):
                    ob, r_ob = obk[kv]
                    ob3 = ob[:].rearrange("p (g c) -> p g c", c=128)
                    P.op("dve", lambda e, kv=kv, ob3=ob3: e.tensor_tensor(out=den[:, kv * 4:(kv + 1) * 4], in0=ob3[:, :, 64], in1=sexp[:, kv * 4:(kv + 1) * 4], op=ALU.add),
                         R=[r_ob, r_sexp], W=[r_den])
                P.op("dve", lambda e: e.reciprocal(out=den[:], in_=den[:]), R=[r_den], W=[r_den])
                for kv in range(2):
                    ob, r_ob = obk[kv]
                    ob3 = ob[:].rearrange("p (g c) -> p g c", c=128)
                    P.op("dve", lambda e, kv=kv, ob3=ob3: e.tensor_tensor(
                        out=concat[:, 512 + kv * 256:512 + (kv + 1) * 256].rearrange("p (g d) -> p g d", d=64),
                        in0=ob3[:, :, 0:64], in1=den[:, kv * 4:(kv + 1) * 4].unsqueeze(2).to_broadcast([128, 4, 64]), op=ALU.mult),
                        R=[r_ob, r_den], W=[r_cc])
                prev_ksT[0], prev_ksT[1] = ksT, r_ksT
                prev_v1[0], prev_v1[1] = v1, r_v1
                if STAGE < 5:
                    return
                transpose_to(lambda c: concat[:, c * 128:(c + 1) * 128], 8, r_cc, hT, r_hT)
                mb = [bank(), bank()]
                for nb_ in range(2):
                    bk, r_bk = mb[nb_]
                    for c in range(8):
                        P.op("pe", lambda e, c=c, nb_=nb_, bk=bk: e.matmul(out=bk[:], lhsT=hT[:, c, :], rhs=w_out_t[:, c, nb_ * 512:(nb_ + 1) * 512],
                                                                         start=(c == 0), stop=(c == 7)),
                             R=[r_hT, r_w_out], W=[r_bk])
                for nb_ in range(2):
                    bk, r_bk = mb[nb_]
                    P.op("dve", lambda e, nb_=nb_, bk=bk: e.scalar_tensor_tensor(out=concat[:, nb_ * 512:(nb_ + 1) * 512], in0=hN[:, nb_ * 512:(nb_ + 1) * 512],
                                                                               scalar=ALPHA, in1=bk[:], op0=ALU.mult, op1=ALU.add),
                         R=[r_hN, r_bk], W=[r_cc])
                layer_norm(sbp, concat, r_cc, h2, r_h2, lng[1], r_lng[1], lnb[1], r_lnb[1])
                P.dma("sp", lambda e: e.dma_start(out=h2_dram[i_own * 128:(i_own + 1) * 128, :], in_=h2[:]), R=[r_h2], Wn=[r_h2d])
                if DEBUG:
                    P.dma("sp", lambda e: e.dma_start(out=dbg_h2[i_own * 128:(i_own + 1) * 128, :], in_=h2[:]), R=[r_h2], Wn=[r_dbg])
                if STAGE < 6:
                    return
                transpose_to(lambda c: h2[:, c * 128:(c + 1) * 128], 8, r_h2, hT, r_hT)
                lb, r_lb = bank()
                for c in range(8):
                    P.op("pe", lambda e, c=c: e.matmul(out=lb[:, 0:256], lhsT=hT[:, c, :], rhs=w_r_t[:, c, :], start=(c == 0), stop=(c == 7)),
                         R=[r_hT, r_w_r], W=[r_lb])
                P.op("act", lambda e: e.activation(out=sc[:], in_=lb[:, 0:256], func=AF.Sigmoid), R=[r_lb], W=[r_sc])
                P.op("pool", lambda e: e.tensor_tensor(out=choice[:], in0=sc[:], in1=rb_t[:], op=ALU.add), R=[r_sc, r_rb], W=[r_choice])
                def tail():
                    for g in range(8):
                        P.op("dve", lambda e, g=g: e.max(out=g8[:, g, :], in_=choice[:, g * 32:(g + 1) * 32]), R=[r_choice], W=[r_g8])
                    P.op("dve", lambda e: e.tensor_tensor(out=gs[:], in0=g8[:, :, 0], in1=g8[:, :, 1], op=ALU.add), R=[r_g8], W=[r_gs])
                    P.op("dve", lambda e: e.max(out=s8[:], in_=gs[:]), R=[r_gs], W=[r_s8])
                    P.op("dve", lambda e: e.tensor_scalar(out=pen[:], in0=gs[:], scalar1=s8[:, 3:4], scalar2=1e9, op0=ALU.is_ge, op1=ALU.mult), R=[r_gs, r_s8], W=[r_pen])
                    P.op("dve", lambda e: e.tensor_scalar(out=pen[:], in0=pen[:], scalar1=-1e9, scalar2=None, op0=ALU.add), R=[r_pen], W=[r_pen])
                    P.op("dve", lambda e: e.tensor_tensor(out=choice[:].rearrange("p (g j) -> p g j", j=32), in0=choice[:].rearrange("p (g j) -> p g j", j=32),
                                                          in1=pen[:].unsqueeze(2).to_broadcast([128, 8, 32]), op=ALU.add), R=[r_choice, r_pen], W=[r_choice])
                    P.op("dve", lambda e: e.max(out=m8[:], in_=choice[:]), R=[r_choice], W=[r_m8])
                    P.op("dve", lambda e: e.max_index(out=i8[:], in_max=m8[:], in_values=choice[:]), R=[r_choice, r_m8], W=[r_i8])
                    P.op("dve", lambda e: e.tensor_copy(out=ekf[:], in_=i8[:]), R=[r_i8], W=[r_ekf])
                    P.op("dve", lambda e: e.tensor_scalar(out=sel[:], in0=choice[:], scalar1=m8[:, 7:8], scalar2=None, op0=ALU.is_ge), R=[r_choice, r_m8], W=[r_sel])
                    P.op("dve", lambda e: e.tensor_tensor(out=wn[:], in0=sel[:], in1=sc[:], op=ALU.mult), R=[r_sel, r_sc], W=[r_wn])
                    P.op("dve", lambda e: e.reduce_sum(out=dsum[:], in_=wn[:], axis=mybir.AxisListType.X), R=[r_wn], W=[r_dsum])
                    P.op("dve", lambda e: e.reciprocal(out=dsum[:], in_=dsum[:]), R=[r_dsum], W=[r_dsum])
                    P.op("dve", lambda e: e.tensor_scalar(out=wn[:], in0=wn[:], scalar1=dsum[:, 0:1], scalar2=2.5, op0=ALU.mult, op1=ALU.mult), R=[r_wn, r_dsum], W=[r_wn])
                    pb, r_pb = bank()
                    P.op("pe", lambda e: e.matmul(out=pb[:, 0:256], lhsT=ltri[:], rhs=sel[:], start=True, stop=False), R=[r_ltri, r_sel], W=[r_pb])
                    P.op("pe", lambda e: e.matmul(out=pb[:, 0:256], lhsT=ones_t[:], rhs=selsum[:], start=False, stop=True), R=[r_ones, r_selsum], W=[r_pb])
                    P.op("pool", lambda e: e.tensor_tensor(out=selsum[:], in0=selsum[:], in1=sel[:], op=ALU.add), R=[r_selsum, r_sel], W=[r_selsum])
                    P.op("dve", lambda e: e.scalar_tensor_tensor(out=posd[:], in0=iota_t[:], scalar=float(CAP), in1=pb[:, 0:256], op0=ALU.mult, op1=ALU.add), R=[r_pb, r_iota], W=[r_posd])
                    for k in range(8):
                        P.op("dve", lambda e, k=k: e.scalar_tensor_tensor(out=junk[:], in0=iota_t[:], scalar=ekf[:, k:k + 1], in1=wn[:], op0=ALU.is_equal, op1=ALU.mult,
                                                                         accum_out=wk_all[:, i_own * 8 + k:i_own * 8 + k + 1]),
                             R=[r_iota, r_ekf, r_wn], W=[r_junk, r_wk])
                        P.op("dve", lambda e, k=k: e.scalar_tensor_tensor(out=junk[:], in0=iota_t[:], scalar=ekf[:, k:k + 1], in1=posd[:], op0=ALU.is_equal, op1=ALU.mult,
                                                                         accum_out=destf[:, k:k + 1]),
                             R=[r_iota, r_ekf, r_posd], W=[r_junk, r_destf])
                    P.op("dve", lambda e: e.tensor_copy(out=destI[:, i_own * 8:(i_own + 1) * 8], in_=destf[:]), R=[r_destf], W=[r_destI])
                    if STAGE < 7:
                        return
                    for k in range(8):
                        P.dma("pool", lambda e, k=k: e.indirect_dma_start(
                            out=list_dram, out_offset=bass.IndirectOffsetOnAxis(ap=destI[:, i_own * 8 + k:i_own * 8 + k + 1], axis=0),
                            in_=tok_t[:, i_own * 2:i_own * 2 + 2], in_offset=None, bounds_check=P.reg(e, E * CAP - 1), oob_is_err=False),
                            R=[r_destI, r_tok], Wn=[r_list])

                return tail

            def prefix_F(gt):
                xt, r_xt = p_xt.next()
                rp, r_rp = p_rp.next()
                P.dma("sp", lambda e: e.dma_start(out=xt[:], in_=x_pre[gt * 128:(gt + 1) * 128, :]), W=[r_xt])
                P.dma("act", lambda e: e.dma_start(out=rp[:], in_=rope[gt]), W=[r_rp])
                layer_norm(sbp, xt, r_xt, hN, r_hN, lng[0], r_lng[0], lnb[0], r_lnb[0], g_eng="dve", rs_eng="pool")
                transpose_to(lambda c: hN[:, c * 128:(c + 1) * 128], 8, r_hN, hT, r_hT, evac="act")
                if gt % 2 == 0:
                    kraw, r_kraw, vbuf, r_vbuf = qk_raw, r_qk_raw, v_sb, r_v
                else:
                    kraw, r_kraw, vbuf, r_vbuf = sg, r_sg, p_psb.tiles[0], p_psb.ress[0]

                def proj(col0, ncols):
                    bk, r_bk = bank()
                    for c in range(8):
                        P.op("pe", lambda e, c=c: e.matmul(out=bk[:, 0:ncols], lhsT=hT[:, c, :],
                                                           rhs=w_in_t[:, c, col0:col0 + ncols], start=(c == 0), stop=(c == 7)),
                             R=[r_hT, r_w_in], W=[r_bk])
                    return bk, r_bk

                bk, r_bk = proj(256, 256)
                P.op("act", lambda e: e.copy(out=kraw[:, 256:512], in_=bk[:, 0:256]), R=[r_bk], W=[r_kraw])
                bk, r_bk = proj(512, 512)
                P.op("act", lambda e: e.copy(out=vbuf[:], in_=bk[:]), R=[r_bk], W=[r_vbuf])
                if gt == NPRE - 1:
                    bk, r_bk = proj(2048, 256)
                    v1, r_v1 = p_v1.next()
                    P.op("act", lambda e: e.copy(out=kv_sb[:], in_=bk[:, 0:256]), R=[r_bk], W=[r_ks])
                    P.op("pool", lambda e: e.tensor_copy(out=v1[:, :, 0:64], in_=kv_sb[:, 128:256].rearrange("p (k d) -> p k d", d=64)),
                         R=[r_ks], W=[r_v1])
                    ksT, r_ksT = p_ksT.next()
                    bk, r_bk = bank()
                    P.op("pe", lambda e: e.transpose(out=bk[:, 0:128], in_=ks_sb, identity=ident[:]), R=[r_ks, r_ident], W=[r_bk])
                    P.op("act", lambda e: e.copy(out=ksT[:], in_=bk[:, 0:128]), R=[r_bk], W=[r_ksT])
                    prev_ksT[0], prev_ksT[1] = ksT, r_ksT
                    prev_v1[0], prev_v1[1] = v1, r_v1
                return (gt, rp, r_rp, kraw, r_kraw, vbuf, r_vbuf)

            def prefix_G(ctx):
                gt, rp, r_rp, kraw, r_kraw, vbuf, r_vbuf = ctx
                src4 = kraw[:, 256:512].rearrange("p (h t d) -> p h t d", t=2, d=32)
                dst4 = qk_rot[:, 256:512].rearrange("p (h t d) -> p h t d", t=2, d=32)
                cosb = rp[:, 0:32].unsqueeze(1).to_broadcast([128, 4, 32])
                sinb = rp[:, 32:64].unsqueeze(1).to_broadcast([128, 4, 32])
                ta = rt[0][:, 0:128].rearrange("p (h d) -> p h d", d=32)
                tb = rt[1][:, 0:128].rearrange("p (h d) -> p h d", d=32)
                P.op("dve", lambda e: e.tensor_tensor(out=ta, in0=src4[:, :, 0, :], in1=cosb, op=ALU.mult), R=[r_kraw, r_rp], W=[r_rt[0]])
                P.op("dve", lambda e: e.tensor_tensor(out=tb, in0=src4[:, :, 1, :], in1=sinb, op=ALU.mult), R=[r_kraw, r_rp], W=[r_rt[1]])
                P.op("dve", lambda e: e.tensor_tensor(out=dst4[:, :, 0, :], in0=ta, in1=tb, op=ALU.subtract), R=[r_rt[0], r_rt[1]], W=[r_qk_rot])
                P.op("dve", lambda e: e.tensor_tensor(out=ta, in0=src4[:, :, 0, :], in1=sinb, op=ALU.mult), R=[r_kraw, r_rp], W=[r_rt[0]])
                P.op("dve", lambda e: e.tensor_tensor(out=tb, in0=src4[:, :, 1, :], in1=cosb, op=ALU.mult), R=[r_kraw, r_rp], W=[r_rt[1]])
                P.op("dve", lambda e: e.tensor_tensor(out=dst4[:, :, 1, :], in0=ta, in1=tb, op=ALU.add), R=[r_rt[0], r_rt[1]], W=[r_qk_rot])
                for h in range(4):
                    P.op("pool", lambda e, h=h: e.tensor_scalar(out=kz[:, h, (h % 2) * 64:(h % 2 + 1) * 64], in0=qk_rot[:, 256 + h * 64:256 + (h + 1) * 64],
                                                                scalar1=pz_t[:, gt * 4 + h:gt * 4 + h + 1], scalar2=pm_t[:, gt:gt + 1], op0=ALU.mult, op1=ALU.mult),
                         R=[r_qk_rot, r_pz, r_pm], W=[r_kz])
                for h in range(4):
                    P.op("pe", lambda e, h=h: e.matmul(out=sbank[:, h * 128:(h + 1) * 128], lhsT=kz[:, h, :],
                                                       rhs=vbuf[:, h * 128:(h + 1) * 128], start=(gt == 0 and h == 0), stop=(gt == NPRE - 1)),
                         R=[r_kz, r_vbuf], W=[r_sbank])

            ctx_prev = None
            for gt in range(NPRE):
                ctx = prefix_F(gt)
                if ctx_prev is not None:
                    prefix_G(ctx_prev)
                ctx_prev = ctx
            if ctx_prev is not None:
                prefix_G(ctx_prev)
                P.op("act", lambda e: e.copy(out=state[:], in_=sbank[:]), R=[r_sbank], W=[r_state])
            nbank[0] = 8
            pending = None
            for i in range(NT):
                pending = mixer_tile(NPRE + i, True, i, pending)
            if pending is not None:
                pending()
            if DEBUG:
                P.dma("sp", lambda e: e.dma_start(out=dbg_dest, in_=destI[:]), R=[r_destI], Wn=[r_dbg])
                P.dma("sp", lambda e: e.dma_start(out=dbg_wk, in_=wk_all[:]), R=[r_wk], Wn=[r_dbg])
            P.barrier()

        with ExitStack() as stB:
            sb = mk_alloc(stB)
            p_sgu = TPool(P, sb, "sgu", [128, 2, 2048], F32, 3)
            p_sdn = TPool(P, sb, "sdn", [128, 2, 1024], F32, 3)
            p_gu = TPool(P, sb, "wgu", [128, 8, 512], F32R, 2)
            p_dn = TPool(P, sb, "wdn", [128, 2, 1024], F32R, 4)
            p_idx = TPool(P, sb, "idx", [128, 2], I32, 3)
            p_xg = TPool(P, sb, "xg", [128, 1024], F32, 3)
            p_xgT = TPool(P, sb, "xgT", [128, 8, 128], F32R, 2)
            p_sg2 = TPool(P, sb, "sg2", [128, 256], F32, 2)
            p_hh = TPool(P, sb, "hh", [128, 256], F32, 2)
            p_hhT = TPool(P, sb, "hhT", [128, 2, 128], F32R, 2)
            p_y = TPool(P, sb, "ysb", [128, 1024], F32, 2)
            lng2 = sb("lng2", [128, 1024]); r_lng2 = P.res("lng2")
            lnb2 = sb("lnb2", [128, 1024]); r_lnb2 = P.res("lnb2")
            sbp = {
                "stats": TPool(P, sb, "statsB", [128, 4, 6], F32, 2),
                "mv": TPool(P, sb, "mvB", [128, 2], F32, 2),
                "rstd": TPool(P, sb, "rstdB", [128, 1], F32, 2),
            }
            P.dma("act", lambda e: e.dma_start(out=lng2[:], in_=ln_g[2].partition_broadcast(128)), W=[r_lng2])
            P.dma("act", lambda e: e.dma_start(out=lnb2[:], in_=ln_b[2].partition_broadcast(128)), W=[r_lnb2])

            njobs = E_RUN + (NT if PHASE3 else 0)
            jobs = {}
            for i in range(3):
                P.op("pool", lambda e, i=i: e.memset(p_xg.tiles[i][:], 0.0), W=[p_xg.ress[i]])

            def st_L(j):
                c = {}
                jobs[j] = c
                xg, r_xg = p_xg.next()
                c["xg"] = (xg, r_xg)
                sgu, r_sgu = p_sgu.next()
                sdn, r_sdn = p_sdn.next()
                c["sgu"] = (sgu, r_sgu)
                c["sdn"] = (sdn, r_sdn)
                if j < E_RUN:
                    wg_, wu_, wd_ = w_gate[j], w_up[j], w_down[j]
                else:
                    wg_, wu_, wd_ = ws_gate, ws_up, ws_down
                P.dma("sp", lambda e: e.dma_start(out=sgu[:, 0, :], in_=wg_.rearrange("(p c) n -> p (c n)", c=8)), W=[r_sgu])
                P.dma("sp", lambda e: e.dma_start(out=sgu[:, 1, :], in_=wu_.rearrange("(p c) n -> p (c n)", c=8)), W=[r_sgu])
                P.dma("sp", lambda e: e.dma_start(out=sdn[:].rearrange("p c n -> p (c n)"), in_=wd_.rearrange("(p c) n -> p (c n)", c=2)), W=[r_sdn])
                if j < E_RUN:
                    idx, r_idx = p_idx.next()
                    P.dma("pool", lambda e: e.dma_start(out=idx[:], in_=list_dram[j * CAP:(j + 1) * CAP, :]), R=[r_list], W=[r_idx])
                    P.dma("pool", lambda e: e.indirect_dma_start(out=xg[:], out_offset=None, in_=h2_dram,
                                                                 in_offset=bass.IndirectOffsetOnAxis(ap=idx[:, 0:1], axis=0),
                                                                 bounds_check=P.reg(e, NT * 128 - 1), oob_is_err=False),
                          R=[r_h2d, r_idx], W=[r_xg])
                else:
                    i = j - E_RUN
                    P.dma("act", lambda e: e.dma_start(out=xg[:], in_=h2_dram[i * 128:(i + 1) * 128, :]), R=[r_h2d], W=[r_xg])

            def st_R(j):
                c = jobs[j]
                sgu, r_sgu = c["sgu"]
                sdn, r_sdn = c["sdn"]
                gu, r_gu = p_gu.next()
                dn, r_dn = p_dn.next()
                c["gu"] = (gu, r_gu)
                c["dn"] = (dn, r_dn)
                P.op("act", lambda e: e.copy(out=gu[:, :, 0:256], in_=sgu[:, 0, :].rearrange("p (c n) -> p c n", n=256)), R=[r_sgu], W=[r_gu])
                P.op("dve", lambda e: e.tensor_copy(out=gu[:, :, 256:512], in_=sgu[:, 1, :].rearrange("p (c n) -> p c n", n=256)), R=[r_sgu], W=[r_gu])
                P.op("act", lambda e: e.copy(out=dn[:, 0, :], in_=sdn[:, 0, :]), R=[r_sdn], W=[r_dn])
                P.op("dve", lambda e: e.tensor_copy(out=dn[:, 1, :], in_=sdn[:, 1, :]), R=[r_sdn], W=[r_dn])

            def st_A(j):
                c = jobs[j]
                xg, r_xg = c["xg"]
                xgT, r_xgT = p_xgT.next()
                c["xgT"] = (xgT, r_xgT)
                xv = xg[:].rearrange("s (p c) -> s c p", c=8)
                transpose_to(lambda cc: xv[:, cc, :], 8, r_xg, xgT, r_xgT)

            def st_B(j):
                c = jobs[j]
                xgT, r_xgT = c["xgT"]
                gu, r_gu = c["gu"]
                gb, r_gb = bank()
                for cc in range(8):
                    P.op("pe", lambda e, cc=cc: e.matmul(out=gb[:], lhsT=xgT[:, cc, :], rhs=gu[:, cc, :], start=(cc == 0), stop=(cc == 7)),
                         R=[r_xgT, r_gu], W=[r_gb])
                sg2, r_sg2 = p_sg2.next()
                hh, r_hh = p_hh.next()
                P.op("act", lambda e: e.activation(out=sg2[:], in_=gb[:, 0:256], func=AF.Silu), R=[r_gb], W=[r_sg2])
                P.op("dve", lambda e: e.tensor_tensor(out=hh[:], in0=gb[:, 256:512], in1=sg2[:], op=ALU.mult), R=[r_gb, r_sg2], W=[r_hh])
                c["hh"] = (hh, r_hh)

            def st_C(j):
                c = jobs[j]
                hh, r_hh = c["hh"]
                hhT, r_hhT = p_hhT.next()
                c["hhT"] = (hhT, r_hhT)
                hv = hh[:].rearrange("s (p c) -> s c p", c=2)
                transpose_to(lambda cc: hv[:, cc, :], 2, r_hh, hhT, r_hhT)

            def st_D(j):
                c = jobs[j]
                hhT, r_hhT = c["hhT"]
                dn, r_dn = c["dn"]
                yb = [bank(), bank()]
                for nb_ in range(2):
                    bk, r_bk = yb[nb_]
                    for cc in range(2):
                        P.op("pe", lambda e, cc=cc, nb_=nb_, bk=bk: e.matmul(out=bk[:], lhsT=hhT[:, cc, :], rhs=dn[:, cc, nb_ * 512:(nb_ + 1) * 512],
                                                                           start=(cc == 0), stop=(cc == 1)),
                             R=[r_hhT, r_dn], W=[r_bk])
                ysb, r_ysb = p_y.next()
                P.op("act", lambda e: e.copy(out=ysb[:, 0:512], in_=yb[0][0][:]), R=[yb[0][1]], W=[r_ysb])
                P.op("dve", lambda e: e.tensor_copy(out=ysb[:, 512:1024], in_=yb[1][0][:]), R=[yb[1][1]], W=[r_ysb])
                P.dma("pool", lambda e: e.dma_start(out=ys_dram[j * CAP:(j + 1) * CAP, :], in_=ysb[:]), R=[r_ysb], Wn=[r_ys])
                del jobs[j]

            for it in range(-4, njobs):
                for (fn, off) in ((st_L, 4), (st_R, 3), (st_A, 3), (st_B, 2), (st_C, 1), (st_D, 0)):
                    j = it + off
                    if 0 <= j < njobs:
                        fn(j)

            p_acc = TPool(P, sb, "acc", [128, 1024], F32, 2)
            p_yk = TPool(P, sb, "yk", [128, 1024], F32, 3)
            def combine_front(i):
                xg, r_xg = p_xg.next()
                ysh, r_ysh = p_xg.next()
                P.dma("act", lambda e: e.dma_start(out=xg[:], in_=h2_dram[i * 128:(i + 1) * 128, :]), R=[r_h2d], W=[r_xg])
                P.dma("act", lambda e: e.dma_start(out=ysh[:], in_=ys_dram[(E_RUN + i) * CAP:(E_RUN + i + 1) * CAP, :]), R=[r_ys], W=[r_ysh])
                acc, r_acc = p_acc.next()
                P.op("dve", lambda e: e.scalar_tensor_tensor(out=acc[:], in0=xg[:], scalar=ALPHA, in1=ysh[:], op0=ALU.mult, op1=ALU.add),
                     R=[r_xg, r_ysh], W=[r_acc])
                for k in range(8):
                    yk, r_yk = p_yk.next()
                    P.dma("pool", lambda e, k=k: e.indirect_dma_start(
                        out=yk[:], out_offset=None, in_=ys_dram,
                        in_offset=bass.IndirectOffsetOnAxis(ap=destI[:, i * 8 + k:i * 8 + k + 1], axis=0),
                        bounds_check=P.reg(e, E * CAP - 1), oob_is_err=False),
                        R=[r_ys, r_destI], W=[r_yk])
                    P.op("dve", lambda e, k=k: e.scalar_tensor_tensor(
                        out=acc[:], in0=yk[:], scalar=wk_all[:, i * 8 + k:i * 8 + k + 1], in1=acc[:], op0=ALU.mult, op1=ALU.add),
                        R=[r_yk, r_wk, r_acc], W=[r_acc])
                return (i, acc, r_acc)

            def combine_back(c):
                i, acc, r_acc = c
                ysb, r_ysb = p_y.next()
                layer_norm(sbp, acc, r_acc, ysb, r_ysb, lng2, r_lng2, lnb2, r_lnb2)
                P.dma("sp", lambda e: e.dma_start(out=out[i * 128:(i + 1) * 128, :], in_=ysb[:]), R=[r_ysb], Wn=[r_out])

            cprev = None
            for i in range(NT if PHASE3 else 0):
                c = combine_front(i)
                if cprev is not None:
                    combine_back(cprev)
                cprev = c
            if cprev is not None:
                combine_back(cprev)
            P.wait_all("sp", [r_out, r_dbg])
            P.barrier()
        P.emit()
    return nc


def _t5_bucket(n):
    n = np.maximum(n, 0)
    ratio = np.log(np.maximum(n, 1).astype(np.float32) / np.float32(16)) / np.float32(math.log(8.0))
    large = 16 + (ratio * np.float32(16)).astype(np.int32)
    large = np.minimum(large, 31)
    return np.where(n < 16, n, large)


def _constants():
    c = {}
    c["c_ident"] = np.eye(128, dtype=np.float32)
    H = 4
    lg = np.log(1.0 - 2.0 ** (-5.0 - np.arange(H, dtype=np.float64)))
    idx = np.arange(128, dtype=np.float64)
    qk_scale = 64.0 ** -0.5
    dec = np.zeros((128, H, 128), np.float64)
    for h in range(H):
        d = idx[None, :] - idx[:, None]
        dec[:, h, :] = np.where(d >= 0, np.exp(np.maximum(d, 0) * lg[h]), 0.0) * qk_scale
    c["c_decay"] = dec.reshape(128, 512).astype(np.float32)
    zeta = np.exp((127.0 - idx)[:, None] * lg[None, :]) * qk_scale
    c["c_zeta"] = zeta.astype(np.float32)
    pz = np.zeros((128, NPRE, 4), np.float64)
    for t in range(NPRE):
        pz[:, t, :] = zeta * np.exp(128.0 * lg[None, :] * (NPRE - 1 - t))
    c["c_pz"] = pz.reshape(128, NPRE * 4).astype(np.float32)
    xi = np.exp((idx + 1.0)[None, :] * lg[:, None])
    xil = np.zeros((128, 2, 128), np.float64)
    cdl = np.zeros((128, 4, 128), np.float64)
    for h in range(H):
        po = (h % 2) * 64
        xil[po:po + 64, h // 2, :] = xi[h][None, :]
        cdl[:, h, :] = np.exp(128.0 * lg[h])
    c["c_xi"] = xil.reshape(128, 256).astype(np.float32)
    c["c_cd"] = cdl.reshape(128, 512).astype(np.float32)
    c["c_ltri"] = (np.arange(128)[:, None] < np.arange(128)[None, :]).astype(np.float32)
    c["c_iota"] = np.tile(np.arange(256, dtype=np.float32)[None, :], (128, 1))
    tok = np.zeros((128, NT, 2), np.int32)
    for i in range(NT):
        tok[:, i, :] = (i * 128 + np.arange(128))[:, None]
    c["c_tok"] = tok.reshape(128, NT * 2)
    i_ = np.arange(128)[None, :]
    j_ = np.arange(128)[:, None]
    mask = np.zeros((128, 2, 2, 4, 128), np.float32)
    bidx = np.zeros((128, 2, 128), np.int64)
    for half in range(2):
        dist = i_ + 128 - (j_ + half * 128)
        valid = (dist >= 0) & (dist < 128)
        mask[:, :, half, :, :] = np.where(valid, 0.0, NEG)[:, None, None, :]
        bidx[:, half, :] = _t5_bucket(np.clip(dist, 0, 127))
    c["c_mask"] = mask.reshape(128, 2048)
    return c, bidx


def _prepare_inputs(inp):
    consts, bidx = _constants()
    x = np.ascontiguousarray(inp["x"], dtype=np.float32)
    rel_bias = np.asarray(inp["rel_bias"], np.float32)
    rb = np.zeros((128, 2, 2, 4, 128), np.float32)
    for kv in range(2):
        for g in range(4):
            for half in range(2):
                rb[:, kv, half, g, :] = rel_bias[bidx[:, half, :], kv * 4 + g]
    consts["c_rb"] = rb.reshape(128, 2048)
    inv = 10000.0 ** (-np.arange(32, dtype=np.float32) / np.float32(32))
    shared = {
        "ln_g0": inp["ln_in_g"].reshape(1, 1024), "ln_b0": inp["ln_in_b"].reshape(1, 1024),
        "ln_g1": inp["ln_mix_g"].reshape(1, 1024), "ln_b1": inp["ln_mix_b"].reshape(1, 1024),
        "ln_g2": inp["ln_ffn_g"].reshape(1, 1024), "ln_b2": inp["ln_ffn_b"].reshape(1, 1024),
        "w_in": inp["w_in"][0], "w_out": inp["w_out"][0], "w_router": inp["w_router"][0],
        "rbias": inp["router_bias"].reshape(1, 256),
        "w_gate": inp["w_gate"][0][:max(E_RUN, 1)], "w_up": inp["w_up"][0][:max(E_RUN, 1)], "w_down": inp["w_down"][0][:max(E_RUN, 1)],
        "ws_gate": inp["ws_gate"][0], "ws_up": inp["ws_up"][0], "ws_down": inp["ws_down"][0],
        "sinks": inp["attn_sinks"].reshape(1, 8),
    }
    shared = {k: np.ascontiguousarray(v, dtype=np.float32) for k, v in shared.items()}
    shared.update(consts)
    in_maps = []
    for c in range(NCORES):
        b, q = c // 4, c % 4
        t0 = q * NT * 128
        m = dict(shared)
        m["x_own"] = x[b, t0:t0 + NT * 128]
        xp = np.zeros((NPRE * 128, 1024), np.float32)
        npre = min(t0, NPRE * 128)
        if npre:
            xp[NPRE * 128 - npre:] = x[b, t0 - npre:t0]
        m["x_pre"] = xp
        pm = np.zeros((128, NPRE), np.float32)
        pm[:, NPRE - npre // 128:] = 1.0 if npre else 0.0
        if not npre:
            pm[:] = 0.0
        m["pmask"] = pm
        m["hflag"] = np.full((128, 1), 1.0 if q > 0 else 0.0, np.float32)
        pos = (t0 - NPRE * 128 + np.arange((NPRE + NT) * 128)).astype(np.float32)
        ang = pos[:, None] * inv[None, :]
        rp = np.concatenate([np.cos(ang), np.sin(ang)], axis=1).astype(np.float32)
        m["rope"] = rp.reshape(NPRE + NT, 128, 64)
        in_maps.append(m)
    return in_maps


_NC_CACHE = {}


def kernel(**inputs):
    in_maps = _prepare_inputs(inputs)
    if "nc" not in _NC_CACHE:
        _NC_CACHE["nc"] = build_program()
    res = run_bass_kernel_spmd(_NC_CACHE["nc"], in_maps, core_ids=list(range(NCORES)))
    outs = [np.asarray(r["out"], dtype=np.float32) for r in res.results]
    full = np.stack(outs, 0).reshape(2, 4 * NT * 128, 1024)
    if DEBUG:
        kernel.dbg = res.results
    return full
```
